# Optimizing a Trainium2 kernel written in Bass

```python
import jax, jax.numpy as jnp
from jax import lax
import numpy as np

D_MODEL = 1024
BATCH = 4
SEQ = 4096
DEPTH = 4

N_MIXERS = 4
N_HEADS = 16
HEAD_DIM = D_MODEL // N_HEADS
BLOCK_Q = 128
CHUNK = 128
GMLP_GROUPS = 8
GMLP_DIM = D_MODEL
LORA_W = 64
LORA_A = 64
LORA_G = 128
N_EXPERTS = 16
N_GROUPS = 4
EXPERTS_PER_GROUP = N_EXPERTS // N_GROUPS
TOP_K = 2
D_EXPERT = 512
ALPHA = (2 * DEPTH) ** 0.25
BETA = (8 * DEPTH) ** -0.25
LN_EPS = 1e-5
GN_EPS = 64e-5

kernel_name = "hybrid_fox_gmlp_stickbreak_rwkv7_grouped_moe"


def _layer_norm(x, g, b, eps=LN_EPS):
    xf = x.astype(jnp.float32)
    mu = jnp.mean(xf, axis=-1, keepdims=True)
    var = jnp.mean(jnp.square(xf - mu), axis=-1, keepdims=True)
    return ((xf - mu) * lax.rsqrt(var + eps)).astype(x.dtype) * g + b


def _split_heads(t):
    B, S, _ = t.shape
    return t.reshape(B, S, N_HEADS, HEAD_DIM)


def _query_blocks(t):
    B, S, H, Dh = t.shape
    return t.reshape(B, S // BLOCK_Q, BLOCK_Q, H, Dh).transpose(1, 0, 2, 3, 4)


def _merge_blocks(o):
    nb, B, BQ, H, Dh = o.shape
    return o.transpose(1, 0, 2, 3, 4).reshape(B, nb * BQ, H * Dh)


def forgetting_attention(h, w_in, b_f, w_out):
    B, S, _ = h.shape
    proj = h @ w_in
    q, k, v, f_logit, o_logit = jnp.split(
        proj, [D_MODEL, 2 * D_MODEL, 3 * D_MODEL, 3 * D_MODEL + N_HEADS], axis=-1)
    q, k, v = _split_heads(q), _split_heads(k), _split_heads(v)
    log_f = jax.nn.log_sigmoid((f_logit + b_f).astype(jnp.float32))
    c = jnp.cumsum(log_f, axis=1).transpose(0, 2, 1)
    c_blocks = c.reshape(B, N_HEADS, S // BLOCK_Q, BLOCK_Q).transpose(2, 0, 1, 3)
    key_pos = jnp.arange(S)
    scale = HEAD_DIM ** -0.5

    def block(args):
        i, q_i, c_i = args
        q_pos = i * BLOCK_Q + jnp.arange(BLOCK_Q)
        logits = (jnp.einsum('bqhd,bkhd->bhqk', q_i, k).astype(jnp.float32) * scale
                  + c_i[..., :, None] - c[:, :, None, :])
        logits = jnp.where(key_pos[None, :] <= q_pos[:, None], logits, -jnp.inf)
        p = jax.nn.softmax(logits, axis=-1).astype(v.dtype)
        return jnp.einsum('bhqk,bkhd->bqhd', p, v)

    o = lax.map(block, (jnp.arange(S // BLOCK_Q), _query_blocks(q), c_blocks))
    o = _merge_blocks(o) * jax.nn.sigmoid(o_logit)
    return o @ w_out


def chunked_spatial_gating(h, w_in, b_in, ln_g, ln_b, w_s, b_s, w_out):
    B, S, _ = h.shape
    z = jax.nn.gelu(h @ w_in + b_in)
    u, v = jnp.split(z, 2, axis=-1)
    v = _layer_norm(v, ln_g, ln_b)
    v = v.reshape(B, S // CHUNK, CHUNK, GMLP_GROUPS, GMLP_DIM // GMLP_GROUPS)
    causal = jnp.tril(jnp.ones((CHUNK, CHUNK), dtype=bool))
    w_causal = jnp.where(causal, w_s, 0.0).astype(v.dtype)
    sv = jnp.einsum('gts,bnsgc->bntgc', w_causal, v) + b_s.T[:, :, None]
    y = u * sv.reshape(B, S, GMLP_DIM)
    return y @ w_out


def stick_breaking_attention(h, w_in, w_out):
    B, S, _ = h.shape
    q, k, v = jnp.split(h @ w_in, 3, axis=-1)
    q, k, v = _split_heads(q), _split_heads(k), _split_heads(v)
    key_pos = jnp.arange(S)
    scale = HEAD_DIM ** -0.5

    def block(args):
        i, q_i = args
        q_pos = i * BLOCK_Q + jnp.arange(BLOCK_Q)
        z = jnp.einsum('bqhd,bkhd->bhqk', q_i, k).astype(jnp.float32) * scale
        strict = key_pos[None, :] < q_pos[:, None]
        log_not = jnp.where(strict, jax.nn.log_sigmoid(-z), 0.0)
        log_rest = lax.cumsum(log_not, axis=3, reverse=True) - log_not
        a = jnp.where(strict, jnp.exp(jax.nn.log_sigmoid(z) + log_rest), 0.0).astype(v.dtype)
        return jnp.einsum('bhqk,bkhd->bqhd', a, v)

    o = lax.map(block, (jnp.arange(S // BLOCK_Q), _query_blocks(q)))
    return _merge_blocks(o) @ w_out


def rwkv7_time_mix(h, mu, w_rkv, w0, w1, w2, a0, a1, a2, g1, g2, k_k, k_a, r_k, gn_g, gn_b, w_out):
    B, S, _ = h.shape
    f32 = jnp.float32
    h_prev = jnp.pad(h, ((0, 0), (1, 0), (0, 0)))[:, :-1]
    hm = h[None] + (h_prev - h)[None] * mu[:, None, None, :]
    r, k, v = jnp.einsum('cbsd,cde->cbse', hm[:3], w_rkv)
    x_w, x_a, x_g = hm[3], hm[4], hm[5]
    d = (w0 + jnp.tanh(x_w @ w1) @ w2).astype(f32)
    log_w = -jnp.exp(-jax.nn.softplus(-d) - 0.5)
    a = jax.nn.sigmoid(a0 + (x_a @ a1) @ a2)
    g = jax.nn.sigmoid(x_g @ g1) @ g2
    kk = _split_heads(k * k_k).astype(f32)
    kk = kk / jnp.maximum(jnp.linalg.norm(kk, axis=-1, keepdims=True), 1e-12)
    k = k * (1 + (a - 1) * k_a)

    def heads_t(t):
        return _split_heads(t).astype(f32).transpose(1, 0, 2, 3)

    def step(state, inp):
        r_t, w_t, k_t, v_t, kk_t, a_t = inp
        sa = jnp.einsum('bhij,bhj->bhi', state, -kk_t)
        state = (state * w_t[:, :, None, :]
                 + sa[..., None] * (kk_t * a_t)[:, :, None, :]
                 + v_t[..., None] * k_t[:, :, None, :])
        return state, jnp.einsum('bhij,bhj->bhi', state, r_t)

    state0 = jnp.zeros((B, N_HEADS, HEAD_DIM, HEAD_DIM), f32)
    xs = (heads_t(r), heads_t(jnp.exp(log_w)), heads_t(k), heads_t(v),
          kk.transpose(1, 0, 2, 3), heads_t(a))
    _, y = lax.scan(step, state0, xs)
    y = y.transpose(1, 0, 2, 3)
    mean = jnp.mean(y, axis=-1, keepdims=True)
    var = jnp.mean(jnp.square(y - mean), axis=-1, keepdims=True)
    y = ((y - mean) * lax.rsqrt(var + GN_EPS)).reshape(B, S, D_MODEL) * gn_g + gn_b
    r_h, k_h, v_h = (_split_heads(t).astype(f32) for t in (r, k, v))
    bonus = jnp.sum(r_h * k_h * r_k, axis=-1, keepdims=True) * v_h
    y = (y + bonus.reshape(B, S, D_MODEL)).astype(h.dtype)
    return (y * g) @ w_out


def grouped_moe(h, router_w, router_b, w_gate, w_up, w_down):
    B, S, D = h.shape
    T = B * S
    hf = h.reshape(T, D)
    scores = jax.nn.sigmoid((hf @ router_w).astype(jnp.float32))
    sel = (scores + router_b.astype(jnp.float32)).reshape(T, N_GROUPS, EXPERTS_PER_GROUP)
    group_score = jnp.sum(lax.top_k(sel, TOP_K)[0], axis=-1)
    g_idx = jnp.argmax(group_score, axis=-1)
    in_group = jnp.take_along_axis(sel, g_idx[:, None, None], axis=1)[:, 0]
    _, local = lax.top_k(in_group, TOP_K)
    e_idx = g_idx[:, None] * EXPERTS_PER_GROUP + local
    gate = jnp.take_along_axis(scores, e_idx, axis=-1)
    gate = gate / jnp.sum(gate, axis=-1, keepdims=True)
    combine = jnp.sum(jax.nn.one_hot(e_idx, N_EXPERTS, dtype=jnp.float32) * gate[..., None], axis=1)
    combine = combine.astype(h.dtype)
    y = jnp.zeros_like(hf)
    for e in range(N_EXPERTS):
        he = jax.nn.silu(hf @ w_gate[e]) * (hf @ w_up[e])
        y = y + combine[:, e:e + 1] * (he @ w_down[e])
    return y.reshape(B, S, D)


def setup_inputs(seed: int = 0) -> dict:
    key = jax.random.key(seed)
    keys = iter(jax.random.split(key, 64))

    def normal(shape, scale):
        return scale * jax.random.normal(next(keys), shape, jnp.float32)

    D, H, N = D_MODEL, N_HEADS, HEAD_DIM
    n_fox, n_gm, n_sb, n_rw = [len(range(m, DEPTH, N_MIXERS)) for m in range(N_MIXERS)]
    s_in = D ** -0.5
    inp = {}
    inp["x"] = normal((BATCH, SEQ, D), 1.0)
    inp["ln1_g"] = 1.0 + normal((DEPTH, D), 0.02)
    inp["ln1_b"] = normal((DEPTH, D), 0.02)
    inp["ln2_g"] = 1.0 + normal((DEPTH, D), 0.02)
    inp["ln2_b"] = normal((DEPTH, D), 0.02)
    inp["fox_w_in"] = jnp.concatenate([
        normal((n_fox, D, 2 * D), s_in), normal((n_fox, D, D), s_in * BETA),
        normal((n_fox, D, H), s_in), normal((n_fox, D, D), s_in)], axis=-1)
    inp["fox_b_f"] = 3.0 + normal((n_fox, H), 0.1)
    inp["fox_w_out"] = normal((n_fox, D, D), s_in * BETA)
    inp["gm_w_in"] = normal((n_gm, D, 2 * GMLP_DIM), s_in)
    inp["gm_b_in"] = normal((n_gm, 2 * GMLP_DIM), 0.02)
    inp["gm_ln_g"] = 1.0 + normal((n_gm, GMLP_DIM), 0.02)
    inp["gm_ln_b"] = normal((n_gm, GMLP_DIM), 0.02)
    inp["gm_w_s"] = normal((n_gm, GMLP_GROUPS, CHUNK, CHUNK), CHUNK ** -0.5)
    inp["gm_b_s"] = 1.0 + normal((n_gm, GMLP_GROUPS, CHUNK), 0.02)
    inp["gm_w_out"] = normal((n_gm, GMLP_DIM, D), GMLP_DIM ** -0.5 * BETA)
    inp["sb_w_in"] = jnp.concatenate([
        normal((n_sb, D, 2 * D), s_in), normal((n_sb, D, D), s_in * BETA)], axis=-1)
    inp["sb_w_out"] = normal((n_sb, D, D), s_in * BETA)
    inp["rw_mu"] = jax.random.uniform(next(keys), (n_rw, 6, D), jnp.float32)
    inp["rw_w_rkv"] = jnp.concatenate([
        normal((n_rw, 2, D, D), s_in), normal((n_rw, 1, D, D), s_in * BETA)], axis=1)
    inp["rw_w0"] = normal((n_rw, D), 0.5)
    inp["rw_w1"] = normal((n_rw, D, LORA_W), s_in)
    inp["rw_w2"] = normal((n_rw, LORA_W, D), LORA_W ** -0.5)
    inp["rw_a0"] = normal((n_rw, D), 0.1)
    inp["rw_a1"] = normal((n_rw, D, LORA_A), s_in)
    inp["rw_a2"] = normal((n_rw, LORA_A, D), LORA_A ** -0.5)
    inp["rw_g1"] = normal((n_rw, D, LORA_G), s_in)
    inp["rw_g2"] = normal((n_rw, LORA_G, D), LORA_G ** -0.5)
    inp["rw_k_k"] = 0.85 + normal((n_rw, D), 0.1)
    inp["rw_k_a"] = 1.0 + normal((n_rw, D), 0.1)
    inp["rw_r_k"] = normal((n_rw, H, N), 0.1)
    inp["rw_gn_g"] = 1.0 + normal((n_rw, D), 0.02)
    inp["rw_gn_b"] = normal((n_rw, D), 0.02)
    inp["rw_w_out"] = normal((n_rw, D, D), s_in * BETA)
    inp["router_w"] = normal((D, N_EXPERTS), s_in)
    inp["router_b"] = normal((N_EXPERTS,), 0.01)
    inp["moe_w_gate"] = normal((DEPTH, N_EXPERTS, D, D_EXPERT), s_in)
    inp["moe_w_up"] = normal((DEPTH, N_EXPERTS, D, D_EXPERT), s_in)
    inp["moe_w_down"] = normal((DEPTH, N_EXPERTS, D_EXPERT, D), D_EXPERT ** -0.5 * BETA)
    return inp


def reference(x, ln1_g, ln1_b, ln2_g, ln2_b,
              fox_w_in, fox_b_f, fox_w_out,
              gm_w_in, gm_b_in, gm_ln_g, gm_ln_b, gm_w_s, gm_b_s, gm_w_out,
              sb_w_in, sb_w_out,
              rw_mu, rw_w_rkv, rw_w0, rw_w1, rw_w2, rw_a0, rw_a1, rw_a2, rw_g1, rw_g2,
              rw_k_k, rw_k_a, rw_r_k, rw_gn_g, rw_gn_b, rw_w_out,
              router_w, router_b, moe_w_gate, moe_w_up, moe_w_down):
    h = x
    for i in range(DEPTH):
        kind, j = i % N_MIXERS, i // N_MIXERS
        if kind == 0:
            mix = forgetting_attention(h, fox_w_in[j], fox_b_f[j], fox_w_out[j])
        elif kind == 1:
            mix = chunked_spatial_gating(h, gm_w_in[j], gm_b_in[j], gm_ln_g[j], gm_ln_b[j],
                                         gm_w_s[j], gm_b_s[j], gm_w_out[j])
        elif kind == 2:
            mix = stick_breaking_attention(h, sb_w_in[j], sb_w_out[j])
        else:
            mix = rwkv7_time_mix(h, rw_mu[j], rw_w_rkv[j], rw_w0[j], rw_w1[j], rw_w2[j],
                                 rw_a0[j], rw_a1[j], rw_a2[j], rw_g1[j], rw_g2[j],
                                 rw_k_k[j], rw_k_a[j], rw_r_k[j], rw_gn_g[j], rw_gn_b[j],
                                 rw_w_out[j])
        h = _layer_norm(ALPHA * h + mix, ln1_g[i], ln1_b[i])
        ffn = grouped_moe(h, router_w, router_b, moe_w_gate[i], moe_w_up[i], moe_w_down[i])
        h = _layer_norm(ALPHA * h + ffn, ln2_g[i], ln2_b[i])
    return h
```

```python
import numpy as np
import concourse.bass as bass
import concourse.mybir as mybir
from contextlib import ExitStack

F32 = mybir.dt.float32
BF16 = mybir.dt.bfloat16
AF = mybir.ActivationFunctionType
ALU = mybir.AluOpType
AX = mybir.AxisListType

ENGS = ["pe", "dve", "act", "pool", "sp"]
_WRITE_KEYS = ("out", "accum_out", "out_max", "out_indices")


class Tile:
    def __init__(self, tensor, name):
        self.tensor = tensor
        self.name = name
        self.is_psum = False
        self.last_w = None
        self.readers = {}

    def __getitem__(self, idx):
        return View(self, self.tensor[idx])

    def v(self):
        return View(self, self.tensor[:])


class View:
    def __init__(self, tile, ap):
        if isinstance(tile, View):
            tile = tile.tile
        self.tile = tile
        self.ap = ap

    @property
    def tensor(self):
        return self.ap

    def v(self):
        return self

    def __getitem__(self, idx):
        return View(self.tile, self.ap[idx])

    def rearrange(self, s, **kw):
        return View(self.tile, self.ap.rearrange(s, **kw))

    def bitcast(self, dt):
        return View(self.tile, self.ap.bitcast(dt))

    def to_broadcast(self, shape):
        return View(self.tile, self.ap.to_broadcast(shape))

    def broadcast_to(self, shape):
        return View(self.tile, self.ap.broadcast_to(shape))

    def unsqueeze(self, a):
        return View(self.tile, self.ap.unsqueeze(a))

    def partition_broadcast(self, n):
        return View(self.tile, self.ap.partition_broadcast(n))


class Prog:
    def __init__(self, nc, same_engine_sync=True, n_dma_sems=12):
        self.nc = nc
        self.es = ExitStack()
        self.ops = {e: [] for e in ENGS}
        self.count = {e: 0 for e in ENGS}
        self.waited = {e: {} for e in ENGS}
        self.same_engine_sync = same_engine_sync
        self.sems = {}
        for e in ENGS:
            self.sems[("eng", e)] = self.es.enter_context(nc.semaphore("s_" + e))
        self.n_dma_sems = n_dma_sems
        self.dma_k = {}
        for lane in ("sp", "pool", "act"):
            self.dma_k[lane] = 0
            for i in range(n_dma_sems):
                self.sems[("dma", lane, i)] = self.es.enter_context(
                    nc.semaphore("d_%s_%d" % (lane, i)))
        self.n_tiles = 0
        self.final_tokens = []
        self.ses = None
        self.stage_id = 0

    def begin_stage(self):
        self.ses = ExitStack()
        self.stage_id += 1

    def end_stage(self, last=False):
        self.barrier()
        self.emit(last=last)
        self.ses.close()
        self.ses = None

    def sbuf(self, shape, dt, name=None):
        self.n_tiles += 1
        name = name or ("t%d" % self.n_tiles)
        if self.ses is not None:
            name = "s%d_%s" % (self.stage_id, name)
        t = (self.ses or self.es).enter_context(self.nc.sbuf_tensor(name, list(shape), dt))
        return Tile(t, name)

    def psum(self, shape, dt, name=None):
        self.n_tiles += 1
        name = name or ("p%d" % self.n_tiles)
        if self.ses is not None:
            name = "s%d_%s" % (self.stage_id, name)
        t = (self.ses or self.es).enter_context(self.nc.psum_tensor(name, list(shape), dt))
        tl = Tile(t, name)
        tl.is_psum = True
        return tl

    def dram(self, name, shape, dt, kind):
        t = self.nc.dram_tensor(name, list(shape), dt, kind=kind)
        return Tile(t.ap(), name)

    def subtile(self, tile, idx, name=None):
        return Tile(tile.tensor[idx], name or (tile.name + "_sub"))

    def _deps(self, eng, reads, writes, skip_same=False):
        deps = {}

        def add(tok):
            if tok is None:
                return
            k, v = tok
            if deps.get(k, 0) < v:
                deps[k] = v

        for t in reads:
            add(t.last_w)
        for t in writes:
            add(t.last_w)
            for k, v in t.readers.items():
                add((k, v))
        out = []
        for k, v in deps.items():
            if k == ("eng", eng):
                if skip_same:
                    continue
            if self.waited[eng].get(k, 0) >= v:
                continue
            self.waited[eng][k] = v
            out.append((k, v))
        return out

    def _commit(self, tok, reads, writes):
        k, v = tok
        for t in writes:
            t.last_w = tok
            t.readers = {}
        for t in reads:
            if t in writes:
                continue
            if t.readers.get(k, 0) < v:
                t.readers[k] = v

    def _split(self, kwargs):
        reads, writes, real = [], [], {}
        for k, a in kwargs.items():
            if isinstance(a, View):
                (writes if k in _WRITE_KEYS else reads).append(a.tile)
                real[k] = a.ap
            else:
                real[k] = a
        return reads, writes, real

    def op(self, eng, method, extra_reads=(), extra_writes=(), **kwargs):
        reads, writes, real = self._split(kwargs)
        reads += [v.tile if isinstance(v, View) else v for v in extra_reads]
        writes += [v.tile if isinstance(v, View) else v for v in extra_writes]
        if eng != "pe":
            writes += [t for t in reads if t.is_psum and t not in writes]
        waits = self._deps(eng, reads, writes,
                           skip_same=(eng == "pe" or not self.same_engine_sync))
        self.count[eng] += 1
        tok = (("eng", eng), self.count[eng])
        self.ops[eng].append((waits, method, real, tok, 1))
        self._commit(tok, reads, writes)
        return tok

    def dve(self, method, **kw):
        return self.op("dve", method, **kw)

    def act(self, method, **kw):
        return self.op("act", method, **kw)

    def pool(self, method, **kw):
        return self.op("pool", method, **kw)

    def pe(self, method, **kw):
        return self.op("pe", method, **kw)

    def mm(self, out, lhsT, rhs, start=True, stop=True, **kw):
        return self.op("pe", "matmul", out=out, lhsT=lhsT, rhs=rhs, start=start, stop=stop, **kw)

    def dma(self, out, in_, lane="sp", final=False, **kw):
        reads, writes = [in_.tile], [out.tile]
        k = self.dma_k[lane]
        self.dma_k[lane] += 1
        si = k % self.n_dma_sems
        semkey = ("dma", lane, si)
        waits = self._deps(lane, reads, writes)
        prev = 16 * (k // self.n_dma_sems)
        if prev > 0 and self.waited[lane].get(semkey, 0) < prev:
            self.waited[lane][semkey] = prev
            waits.append((semkey, prev))
        tok = (semkey, prev + 16)
        real = dict(out=out.ap, in_=in_.ap, **kw)
        self.ops[lane].append((waits, "dma_start", real, tok, 16))
        self._commit(tok, reads, writes)
        if final:
            self.final_tokens.append(tok)
        return tok

    def barrier(self):
        allk = {}
        for e in ENGS:
            if self.count[e] > 0:
                allk[("eng", e)] = self.count[e]
        for lane in ("sp", "pool", "act"):
            k = self.dma_k[lane]
            for i in range(self.n_dma_sems):
                n = (k - i + self.n_dma_sems - 1) // self.n_dma_sems if k > i else 0
                if n > 0:
                    allk[("dma", lane, i)] = 16 * n
        for e in ENGS:
            waits = []
            for k, v in allk.items():
                if self.waited[e].get(k, 0) >= v:
                    continue
                self.waited[e][k] = v
                waits.append((k, v))
            if waits:
                self.ops[e].append((waits, None, None, None, 0))

    def emit(self, last=True):
        nc = self.nc
        fin = []
        seen = {}
        for k, v in self.final_tokens:
            if seen.get(k, 0) < v:
                seen[k] = v
        fin = list(seen.items())
        engobj = {"pe": "tensor", "dve": "vector", "act": "scalar", "pool": "gpsimd", "sp": "sync"}
        with nc.Block() as block:
            for e in ENGS:
                ops = self.ops[e]
                extra = fin if (e == "sp" and last) else []

                def body(eng, ops=ops, extra=extra):
                    for waits, method, real, tok, inc in ops:
                        for k, v in waits:
                            eng.wait_ge(self.sems[k], v)
                        if method is None:
                            continue
                        ins = getattr(eng, method)(**real)
                        ins.then_inc(self.sems[tok[0]], inc)
                    for k, v in extra:
                        eng.wait_ge(self.sems[k], v)

                if not ops and not extra:
                    continue
                getattr(block, engobj[e])(body)
        self.ops = {e: [] for e in ENGS}

    def close(self):
        self.es.close()


D = 1024
NE = 16
DE = 512
ALPHA = 8 ** 0.25
LN_EPS = 1e-5


def layer_norm_tile(P, x, out, g_t, b_t, stats, mv, rstd, tmp, eps=LN_EPS):
    for c in range(2):
        P.dve("bn_stats", out=stats[:, c, :], in_=x[:, c * 512:(c + 1) * 512])
    P.dve("bn_aggr", out=mv.v(), in_=stats.v())
    P.dve("tensor_scalar", out=rstd.v(), in0=mv[:, 1:2], scalar1=eps, scalar2=None, op0=ALU.add)
    P.act("activation", out=rstd.v(), in_=rstd.v(), func=AF.Sqrt)
    P.dve("reciprocal", out=rstd.v(), in_=rstd.v())
    P.dve("tensor_scalar", out=out, in0=x, scalar1=mv[:, 0:1], scalar2=rstd[:, 0:1],
          op0=ALU.subtract, op1=ALU.mult)
    P.pool("tensor_tensor", out=out, in0=out, in1=g_t, op=ALU.mult)
    P.pool("tensor_tensor", out=out, in0=out, in1=b_t, op=ALU.add)


def body_T(P, io, TOK=2048, stop=99, sub=99):
    nc = P.nc
    NT = TOK // 128
    NC4 = TOK // 512
    hin, xT, wout, rw, rb = io["hin"], io["xT"], io["wout"], io["rw"], io["rb"]
    wg, wu, wd, hout = io["wg"], io["wu"], io["wd"], io["hout"]
    lnrows = io["ln"]
    houtT = io.get("houtT")

    acc = P.sbuf([128, NT, D], F32, "acc")
    acc_t = [P.subtile(acc, (slice(None), t, slice(None)), "acc%d" % t) for t in range(NT)]
    h1T = P.sbuf([128, 8, TOK], BF16, "h1T")
    arena = P.sbuf([128, 24576], BF16, "arena")
    lnt = [P.sbuf([128, D], F32, "lnt%d" % i) for i in range(4)]
    rwt = P.sbuf([128, 8, NE], F32, "rwt")
    rbt = P.sbuf([128, NE], F32, "rbt")
    ident = P.sbuf([128, 128], F32, "ident")
    hin_t = [P.sbuf([128, D], F32, "hin%d" % i) for i in range(2)]
    h1_t = [P.sbuf([128, D], F32, "h1_%d" % i) for i in range(2)]
    h1Tf = [P.sbuf([128, 8, 128], F32, "h1Tf%d" % i) for i in range(2)]
    tmp_t = [None, None]
    stats = [P.sbuf([128, 2, 6], F32, "st%d" % i) for i in range(2)]
    mv = [P.sbuf([128, 2], F32, "mv%d" % i) for i in range(2)]
    rstd = [P.sbuf([128, 1], F32, "rstd%d" % i) for i in range(2)]
    scores = P.sbuf([128, NT, NE], F32, "scores")
    comb = P.sbuf([128, NT, NE], F32, "comb")
    r_sel = P.sbuf([128, NT, NE], F32, "r_sel")
    r_cnt = P.sbuf([128, NT, NE], F32, "r_cnt")
    r_tmp = P.sbuf([128, NT, NE], F32, "r_tmp")
    r_gs = P.sbuf([128, NT, 4], F32, "r_gs")
    r_gm = P.sbuf([128, NT], F32, "r_gm")
    r_gmask = P.sbuf([128, NT, 4], F32, "r_gmask")
    r_den = P.sbuf([128, NT], F32, "r_den")
    heT = [[P.sbuf([128, 512], BF16, "heT%d_%d" % (b, f)) for f in range(4)] for b in range(2)]
    sg = [P.sbuf([128, 512], F32, "sg%d" % i) for i in range(2)]
    ps = [P.psum([128, 512], F32, "ps%d" % i) for i in range(8)]

    ar = arena.tensor
    woutb = Tile(ar[:, 0:8192].rearrange("p (k f) -> p k f", k=8), "woutb")
    xTb = Tile(ar[:, 8192:8192 + 8 * TOK].rearrange("p (k t) -> p k t", k=8), "xTb")
    wbuf = []
    for b in range(2):
        o = b * 12288
        wbuf.append(dict(
            g=Tile(ar[:, o:o + 4096].rearrange("p (k f) -> p k f", k=8), "wg%d" % b),
            u=Tile(ar[:, o + 4096:o + 8192].rearrange("p (k f) -> p k f", k=8), "wu%d" % b),
            d=Tile(ar[:, o + 8192:o + 12288].rearrange("p (k f) -> p k f", k=4), "wd%d" % b)))

    for i in range(4):
        P.dma(lnt[i].v(), View(lnrows[i], lnrows[i].tensor.broadcast_to([128, D])), lane="sp")
    P.dma(rwt.v(), rw.v().rearrange("(k p) e -> p k e", p=128), lane="sp")
    P.dma(rbt.v(), View(rb, rb.tensor[0:1, :].broadcast_to([128, NE])), lane="sp")
    P.op("pool", "memset", extra_writes=[ident], ap=ident.v().ap, constant=1.0)
    P.pool("affine_select", out=ident.v(), in_=ident.v(), pattern=[[-1, 128]],
           compare_op=ALU.is_equal, fill=0.0, base=0, channel_multiplier=1)
    P.dma(woutb.v(), wout.v().rearrange("(k p) f -> p k f", p=128), lane="pool")
    for k in range(8):
        P.dma(xTb[:, k, :], xT[k * 128:(k + 1) * 128, :], lane="pool")

    for tt in range(NT):
        b = tt % 2
        if sub < 99 and tt > 0:
            break
        P.dma(hin_t[b].v(), hin[tt * 128:(tt + 1) * 128, :], lane="sp")
        pm = [ps[(2 * tt) % 4], ps[(2 * tt) % 4 + 1]]
        for half in range(2):
            for k in range(8):
                P.mm(pm[half].v(), xTb[:, k, tt * 128:(tt + 1) * 128],
                     woutb[:, k, half * 512:(half + 1) * 512], start=(k == 0), stop=(k == 7))
        if sub == 1:
            break
        for half in range(2):
            P.dve("scalar_tensor_tensor", out=hin_t[b][:, half * 512:(half + 1) * 512],
                  in0=hin_t[b][:, half * 512:(half + 1) * 512], scalar=ALPHA, in1=pm[half].v(),
                  op0=ALU.mult, op1=ALU.add)
        if sub == 2:
            break
        layer_norm_tile(P, hin_t[b].v(), h1_t[b].v(), lnt[0].v(), lnt[1].v(),
                        stats[b], mv[b], rstd[b], tmp_t[b])
        if sub == 3:
            break
        P.act("mul", out=acc_t[tt].v(), in_=h1_t[b].v(), mul=ALPHA)
        pt = [ps[4 + (2 * tt) % 4], ps[4 + (2 * tt) % 4 + 1]]
        for k in range(8):
            P.pe("transpose", out=pt[k // 4][:, (k % 4) * 128:(k % 4 + 1) * 128],
                 in_=h1_t[b][:, k * 128:(k + 1) * 128], identity=ident.v())
        if sub == 4:
            break
        for hf in range(2):
            P.act("copy", out=h1T[:, hf * 4:(hf + 1) * 4, tt * 128:(tt + 1) * 128],
                  in_=pt[hf].v().rearrange("p (k t) -> p k t", k=4))
            P.dve("tensor_copy", out=h1Tf[b][:, hf * 4:(hf + 1) * 4, :],
                  in_=pt[hf].v().rearrange("p (k t) -> p k t", k=4))
        if sub == 5:
            break
        pr = pm[0]
        for k in range(8):
            P.mm(pr[:, 0:NE], h1Tf[b][:, k, :], rwt[:, k, :], start=(k == 0), stop=(k == 7))
        P.act("activation", out=scores[:, tt, :], in_=pr[:, 0:NE], func=AF.Sigmoid)

    def v4(t):
        return t.v().rearrange("p t (g i) -> p t g i", g=4)
    P.dve("tensor_tensor", out=r_sel.v(), in0=scores.v(),
          in1=View(rbt, rbt.tensor[:, :].unsqueeze(1).to_broadcast([128, NT, NE])), op=ALU.add)
    P.op("dve", "memset", extra_writes=[r_cnt], ap=r_cnt.v().ap, constant=0.0)
    for j in range(4):
        selj = View(r_sel, v4(r_sel).ap[:, :, :, j:j + 1].to_broadcast([128, NT, 4, 4]))
        P.dve("tensor_tensor", out=v4(r_tmp), in0=selj, in1=v4(r_sel), op=ALU.is_gt)
        P.dve("tensor_tensor", out=r_cnt.v(), in0=r_cnt.v(), in1=r_tmp.v(), op=ALU.add)
    P.dve("tensor_single_scalar", out=r_cnt.v(), in_=r_cnt.v(), scalar=1.5, op=ALU.is_lt)
    P.dve("tensor_tensor", out=r_tmp.v(), in0=r_sel.v(), in1=r_cnt.v(), op=ALU.mult)
    P.dve("tensor_reduce", out=r_gs.v(), in_=v4(r_tmp), axis=AX.X, op=ALU.add)
    P.dve("tensor_reduce", out=r_gm.v(), in_=r_gs.v(), axis=AX.X, op=ALU.max)
    P.dve("tensor_tensor", out=r_gmask.v(), in0=r_gs.v(),
          in1=View(r_gm, r_gm.tensor[:, :].unsqueeze(2).to_broadcast([128, NT, 4])), op=ALU.is_ge)
    P.dve("tensor_tensor", out=v4(r_cnt), in0=v4(r_cnt),
          in1=View(r_gmask, r_gmask.tensor[:, :, :].unsqueeze(3).to_broadcast([128, NT, 4, 4])),
          op=ALU.mult)
    P.dve("tensor_tensor", out=r_tmp.v(), in0=scores.v(), in1=r_cnt.v(), op=ALU.mult)
    P.dve("tensor_reduce", out=r_den.v(), in_=r_tmp.v(), axis=AX.X, op=ALU.add)
    P.dve("reciprocal", out=r_den.v(), in_=r_den.v())
    P.dve("tensor_tensor", out=comb.v(), in0=r_tmp.v(),
          in1=View(r_den, r_den.tensor[:, :].unsqueeze(2).to_broadcast([128, NT, NE])), op=ALU.mult)

    P.barrier()

    it = 0
    for e in range(NE):
        wb = wbuf[e % 2]
        P.dma(wb["g"].v(), wg[e].rearrange("(k p) f -> p k f", p=128), lane="pool")
        P.dma(wb["u"].v(), wu[e].rearrange("(k p) f -> p k f", p=128), lane="pool")
        P.dma(wb["d"].v(), wd[e].rearrange("(k p) f -> p k f", p=128), lane="pool")
        for tc in range(NC4):
            hb = heT[it % 2]
            for ft in range(4):
                pg = ps[(2 * (it * 4 + ft)) % 4]
                pu = ps[(2 * (it * 4 + ft)) % 4 + 1]
                for k in range(8):
                    P.mm(pg.v(), wb["g"][:, k, ft * 128:(ft + 1) * 128],
                         h1T[:, k, tc * 512:(tc + 1) * 512], start=(k == 0), stop=(k == 7))
                for k in range(8):
                    P.mm(pu.v(), wb["u"][:, k, ft * 128:(ft + 1) * 128],
                         h1T[:, k, tc * 512:(tc + 1) * 512], start=(k == 0), stop=(k == 7))
                s_ = sg[(it * 4 + ft) % 2]
                P.act("activation", out=s_.v(), in_=pg.v(), func=AF.Silu)
                P.dve("tensor_tensor", out=hb[ft].v(), in0=s_.v(), in1=pu.v(), op=ALU.mult)
            for t4 in range(4):
                tt = tc * 4 + t4
                for half in range(2):
                    py = ps[4 + (2 * (it * 4 + t4)) % 4 + half]
                    for ft in range(4):
                        P.mm(py.v(), hb[ft][:, t4 * 128:(t4 + 1) * 128],
                             wb["d"][:, ft, half * 512:(half + 1) * 512], start=(ft == 0), stop=(ft == 3))
                    P.dve("scalar_tensor_tensor", out=acc_t[tt][:, half * 512:(half + 1) * 512],
                          in0=py.v(), scalar=comb[:, tt, e:e + 1],
                          in1=acc_t[tt][:, half * 512:(half + 1) * 512], op0=ALU.mult, op1=ALU.add)
            it += 1

    for tt in range(NT):
        b = tt % 2
        layer_norm_tile(P, acc_t[tt].v(), h1_t[b].v(), lnt[2].v(), lnt[3].v(),
                        stats[b], mv[b], rstd[b], tmp_t[b])
        P.dma(hout[tt * 128:(tt + 1) * 128, :], h1_t[b].v(), lane="sp", final=True)
        if houtT is not None:
            ptt = [ps[(2 * tt) % 4], ps[(2 * tt) % 4 + 1]]
            for k in range(8):
                P.pe("transpose", out=ptt[k // 4][:, (k % 4) * 128:(k % 4 + 1) * 128],
                     in_=h1_t[b][:, k * 128:(k + 1) * 128], identity=ident.v())
            for hf in range(2):
                P.act("copy", out=h1Tf[b][:, hf * 4:(hf + 1) * 4, :],
                      in_=ptt[hf].v().rearrange("p (k t) -> p k t", k=4))
            P.dma(houtT[:, tt * 128:(tt + 1) * 128].rearrange("(k p) t -> p k t", p=128), h1Tf[b].v(),
                  lane="sp", final=True)


def build_T(nc, TOK=2048):
    P = Prog(nc)
    io = dict(hin=P.dram("hin", [TOK, D], F32, "ExternalInput"), xT=P.dram("xT", [D, TOK], F32, "ExternalInput"),
              wout=P.dram("wout", [D, D], F32, "ExternalInput"), rw=P.dram("rw", [D, NE], F32, "ExternalInput"),
              rb=P.dram("rb", [1, NE], F32, "ExternalInput"), wg=P.dram("wg", [NE, D, DE], F32, "ExternalInput"),
              wu=P.dram("wu", [NE, D, DE], F32, "ExternalInput"), wd=P.dram("wd", [NE, DE, D], F32, "ExternalInput"),
              hout=P.dram("hout", [TOK, D], F32, "ExternalOutput"))
    lnp = P.dram("lnp", [4, D], F32, "ExternalInput")
    io["ln"] = [lnp[i:i + 1, :] for i in range(4)]
    body_T(P, io, TOK)
    P.emit()
    P.close()
    return nc


D = 1024


def body_GM(P, io, TOK=2048):
    NT = TOK // 128
    hT, w_in, b_in, wsT, bsT = io["hT"], io["w_in"], io["b_in"], io["wsT"], io["bsT"]
    lnrows = io["ln"]

    hTb = P.sbuf([128, 8, TOK], BF16, "hTb")
    winb = P.sbuf([128, 8, 2 * D], BF16, "winb")
    bint = P.sbuf([128, 2 * D], F32, "bint")
    lnt = [P.sbuf([128, D], F32, "lnt%d" % i) for i in range(2)]
    wst = P.sbuf([128, 8, 128], F32, "wst")
    wsb = P.sbuf([128, 8, 128], BF16, "wsb")
    bst = P.sbuf([128, 8], F32, "bst")
    zt = [P.sbuf([128, 2 * D], F32, "z%d" % i) for i in range(2)]
    vn = [P.sbuf([128, D], F32, "vn%d" % i) for i in range(2)]
    vnb = [P.sbuf([128, D], BF16, "vnb%d" % i) for i in range(2)]
    yt = [P.sbuf([128, D], F32, "y%d" % i) for i in range(2)]
    stats = [P.sbuf([128, 2, 6], F32, "st%d" % i) for i in range(2)]
    mv = [P.sbuf([128, 2], F32, "mv%d" % i) for i in range(2)]
    rstd = [P.sbuf([128, 1], F32, "rstd%d" % i) for i in range(2)]
    ps = [P.psum([128, 512], F32, "ps%d" % i) for i in range(8)]
    identf = P.sbuf([128, 128], F32, "identf")
    yT = [P.sbuf([128, 8, 128], F32, "yT%d" % i) for i in range(2)]
    P.op("pool", "memset", extra_writes=[identf], ap=identf.v().ap, constant=1.0)
    P.pool("affine_select", out=identf.v(), in_=identf.v(), pattern=[[-1, 128]],
           compare_op=ALU.is_equal, fill=0.0, base=0, channel_multiplier=1)

    for k in range(8):
        for c2 in range(max(1, TOK // 2048)):
            w_ = min(TOK, 2048)
            P.dma(hTb[:, k, c2 * w_:(c2 + 1) * w_], hT[k * 128:(k + 1) * 128, c2 * w_:(c2 + 1) * w_], lane="pool")
        P.dma(winb[:, k, 0:1024], w_in[k * 128:(k + 1) * 128, 0:1024], lane="pool")
        P.dma(winb[:, k, 1024:2048], w_in[k * 128:(k + 1) * 128, 1024:2048], lane="pool")
    P.dma(bint.v(), View(b_in, b_in.tensor[0:1, :].broadcast_to([128, 2 * D])), lane="sp")
    for i in range(2):
        P.dma(lnt[i].v(), View(lnrows[i], lnrows[i].tensor.broadcast_to([128, D])), lane="sp")
    P.dma(wst.v(), wsT.v(), lane="sp")
    P.dma(bst.v(), bsT.v(), lane="sp")
    P.pool("affine_select", out=wst.v(), in_=wst.v(), pattern=[[0, 8], [1, 128]],
           compare_op=ALU.is_ge, fill=0.0, base=0, channel_multiplier=-1)
    P.pool("tensor_copy", out=wsb.v(), in_=wst.v())

    for tt in range(NT):
        b = tt % 2
        for cb in range(4):
            pz = ps[cb]
            for k in range(8):
                P.mm(pz.v(), hTb[:, k, tt * 128:(tt + 1) * 128], winb[:, k, cb * 512:(cb + 1) * 512],
                     start=(k == 0), stop=(k == 7))
            P.dve("tensor_tensor", out=zt[b][:, cb * 512:(cb + 1) * 512], in0=pz.v(),
                  in1=bint[:, cb * 512:(cb + 1) * 512], op=ALU.add)
        P.act("activation", out=zt[b].v(), in_=zt[b].v(), func=AF.Gelu)
        layer_norm_tile(P, zt[b][:, D:2 * D], vn[b].v(), lnt[0].v(), lnt[1].v(), stats[b], mv[b], rstd[b], None)
        P.act("copy", out=vnb[b].v(), in_=vn[b].v())
        for g in range(8):
            psv = ps[4 + g // 4]
            P.mm(psv[:, (g % 4) * 128:(g % 4 + 1) * 128], wsb[:, g, :], vnb[b][:, g * 128:(g + 1) * 128])
        for g in range(8):
            psv = ps[4 + g // 4]
            P.dve("scalar_tensor_tensor", out=yt[b][:, g * 128:(g + 1) * 128],
                  in0=psv[:, (g % 4) * 128:(g % 4 + 1) * 128], scalar=bst[:, g:g + 1],
                  in1=zt[b][:, g * 128:(g + 1) * 128], op0=ALU.add, op1=ALU.mult)
        if "X" in io:
            P.dma(io["X"][tt * 128:(tt + 1) * 128, :], yt[b].v(), lane="sp", final=True)
        else:
            for k in range(8):
                P.pe("transpose", out=ps[6 + k // 4][:, (k % 4) * 128:(k % 4 + 1) * 128],
                     in_=yt[b][:, k * 128:(k + 1) * 128], identity=identf.v())
            for hf in range(2):
                P.act("copy", out=yT[b][:, hf * 4:(hf + 1) * 4, :],
                      in_=ps[6 + hf].v().rearrange("p (k t) -> p k t", k=4))
            P.dma(io["XT"][:, tt * 128:(tt + 1) * 128].rearrange("(k p) t -> p k t", p=128), yT[b].v(),
                  lane="sp", final=True)


def build_GM(nc, TOK=2048):
    P = Prog(nc)
    lnp = P.dram("lnp", [2, D], F32, "ExternalInput")
    io = dict(hT=P.dram("hT", [D, TOK], F32, "ExternalInput").v(), w_in=P.dram("w_in", [D, 2 * D], F32, "ExternalInput").v(),
              b_in=P.dram("b_in", [1, 2 * D], F32, "ExternalInput").v(), ln=[lnp[i:i + 1, :] for i in range(2)],
              wsT=P.dram("wsT", [128, 8, 128], F32, "ExternalInput").v(), bsT=P.dram("bsT", [128, 8], F32, "ExternalInput").v(),
              X=P.dram("X", [TOK, D], F32, "ExternalOutput").v())
    body_GM(P, io, TOK)
    P.emit()
    P.close()
    return nc


D = 1024
S = 4096
NB = S // 128
NQ = S // 512
NH = 8


def body_FOX(P, hT, groups):
    hTb = P.sbuf([128, 8, S], BF16, "hTb")
    wqb = P.sbuf([128, 8, 512], BF16, "wqb")
    wkb = P.sbuf([128, 8, 512], BF16, "wkb")
    wvb = P.sbuf([128, 8, 512], BF16, "wvb")
    wogb = P.sbuf([128, 8, 512], BF16, "wogb")
    wfb = P.sbuf([128, 8, NH], BF16, "wfb")
    bft = P.sbuf([128, NH], F32, "bft")
    Vall = P.sbuf([128, NB, 512], BF16, "Vall")
    QT = [P.sbuf([64, S], BF16, "QT%d" % i) for i in range(2)]
    KT = [P.sbuf([64, S], BF16, "KT%d" % i) for i in range(2)]
    OG = [P.sbuf([64, S], BF16, "OG%d" % i) for i in range(2)]
    triU = P.sbuf([128, 128], F32, "triU")
    triUb = P.sbuf([128, 128], BF16, "triUb")
    onesf = P.sbuf([128, 128], F32, "onesf")
    onesb = P.sbuf([128, 64], BF16, "onesb")
    logf = P.sbuf([128, NB, NH], F32, "logf")
    negcin = P.sbuf([128, NB, NH], F32, "negcin")
    Rb = P.sbuf([128, NB + 1, NH], F32, "Rb")
    negR = P.sbuf([128, NB + 1, NH], F32, "negR")
    biasq = [P.sbuf([128, NB], F32, "biasq%d" % i) for i in range(2)]
    Pt = [P.sbuf([128, 512], BF16, "P%d" % i) for i in range(3)]
    rden = [P.sbuf([64, 512], F32, "rden%d" % i) for i in range(2)]
    ot = [P.sbuf([64, 512], F32, "ot%d" % i) for i in range(2)]
    ps = [P.psum([128, 512], F32, "ps%d" % i) for i in range(8)]

    for k in range(8):
        for c4 in range(4):
            P.dma(hTb[:, k, c4 * 1024:(c4 + 1) * 1024], hT[k * 128:(k + 1) * 128, c4 * 1024:(c4 + 1) * 1024], lane="pool")
    P.op("pool", "memset", extra_writes=[triU], ap=triU.v().ap, constant=1.0)
    P.pool("affine_select", out=triU.v(), in_=triU.v(), pattern=[[1, 128]],
           compare_op=ALU.is_ge, fill=0.0, base=0, channel_multiplier=-1)
    P.pool("tensor_copy", out=triUb.v(), in_=triU.v())
    P.op("pool", "memset", extra_writes=[onesf], ap=onesf.v().ap, constant=1.0)
    P.op("pool", "memset", extra_writes=[onesb], ap=onesb.v().ap, constant=1.0)

    unit = 0
    sc = 0
    for grp in groups:
        wq, wk, wv, wog, wf, bf, XT = (grp[n] for n in ("wq", "wk", "wv", "wog", "wf", "bf", "XT"))
        for wsrc, wdst in ((wq, wqb), (wk, wkb), (wv, wvb), (wog, wogb)):
            P.dma(wdst.v(), wsrc.rearrange("(k p) f -> p k f", p=128), lane="pool")
        P.dma(wfb.v(), wf.rearrange("(k p) f -> p k f", p=128), lane="pool")
        P.dma(bft.v(), View(bf, bf.tensor.broadcast_to([128, NH])), lane="sp")
        pf = ps[0]
        for kb in range(NB):
            for k in range(8):
                P.mm(pf[:, kb * NH:(kb + 1) * NH], hTb[:, k, kb * 128:(kb + 1) * 128], wfb[:, k, :],
                     start=(k == 0), stop=(k == 7))
        lf2 = logf.v().rearrange("p b h -> p (b h)")
        P.dve("tensor_tensor", out=logf.v(), in0=pf[:, 0:NB * NH].rearrange("p (b h) -> p b h", h=NH),
              in1=View(bft, bft.tensor[:, :].unsqueeze(1).to_broadcast([128, NB, NH])), op=ALU.add)
        P.act("activation", out=lf2, in_=lf2, func=AF.Exp, scale=-1.0)
        P.act("activation", out=lf2, in_=lf2, func=AF.Ln, bias=1.0)
        P.dve("tensor_scalar", out=lf2, in0=lf2, scalar1=-1.0, scalar2=None, op0=ALU.mult)
        pc = ps[1]
        P.mm(pc[:, 0:NB * NH], triU.v(), lf2)
        P.dve("tensor_scalar", out=negcin.v().rearrange("p b h -> p (b h)"), in0=pc[:, 0:NB * NH],
              scalar1=-1.0, scalar2=None, op0=ALU.mult)
        pT = ps[2]
        P.mm(pT[:, 0:NB * NH], onesf.v(), lf2)
        P.op("dve", "memset", extra_writes=[Rb], ap=Rb[:, 0, :].ap, constant=0.0)
        for m in range(NB):
            P.dve("tensor_tensor", out=Rb[:, m + 1, :], in0=Rb[:, m, :], in1=pT[:, m * NH:(m + 1) * NH], op=ALU.add)
        P.dve("tensor_scalar", out=negR.v(), in0=Rb.v(), scalar1=-1.0, scalar2=None, op0=ALU.mult)

        for kb in range(NB):
            pv = ps[4 + kb % 4]
            for k in range(8):
                P.mm(pv.v(), hTb[:, k, kb * 128:(kb + 1) * 128], wvb[:, k, :], start=(k == 0), stop=(k == 7))
            if kb % 2 == 0:
                P.dve("tensor_copy", out=Vall[:, kb, :], in_=pv.v())
            else:
                P.act("copy", out=Vall[:, kb, :], in_=pv.v())

        for h in range(NH):
            hb = h % 2
            for j in range(NQ):
                for (wsrc, dst, kind) in ((wqb, QT[hb], 0), (wkb, KT[hb], 0), (wogb, OG[hb], 1)):
                    pp = ps[sc % 4]
                    sc += 1
                    for k in range(8):
                        P.mm(pp[0:64, :], wsrc[:, k, h * 64:(h + 1) * 64], hTb[:, k, j * 512:(j + 1) * 512],
                             start=(k == 0), stop=(k == 7))
                    if kind == 0:
                        P.dve("tensor_copy", out=dst[:, j * 512:(j + 1) * 512], in_=pp[0:64, :])
                    else:
                        P.act("activation", out=dst[:, j * 512:(j + 1) * 512], in_=pp[0:64, :], func=AF.Sigmoid)
            for j in range(NQ):
                ub = unit % 2
                unit += 1
                nk = 4 * j + 4
                bq = biasq[ub]
                P.dve("scalar_tensor_tensor", out=bq[:, 0:nk], in0=negR[:, 0:nk, h], scalar=Rb[:, 4 * j, h:h + 1],
                      in1=negcin[:, 0:nk, h], op0=ALU.add, op1=ALU.add)
                pnum = ps[4 + 2 * ub]
                pden = ps[5 + 2 * ub]
                for kb in range(nk):
                    i = kb - 4 * j
                    c0 = 128 * i if i > 0 else 0
                    pS = ps[sc % 4]
                    pt_ = Pt[sc % 3]
                    sc += 1
                    P.mm(pS[:, c0:512], KT[hb][:, kb * 128:(kb + 1) * 128], QT[hb][:, j * 512 + c0:(j + 1) * 512])
                    P.act("activation", out=pt_[:, c0:512], in_=pS[:, c0:512], func=AF.Exp,
                          scale=0.125, bias=bq[:, kb:kb + 1])
                    if i >= 0:
                        P.pool("tensor_tensor", out=pt_[:, c0:c0 + 128], in0=pt_[:, c0:c0 + 128],
                               in1=triUb.v(), op=ALU.mult)
                    P.mm(pnum[0:64, c0:512], Vall[:, kb, h * 64:(h + 1) * 64], pt_[:, c0:512],
                         start=(kb == 0), stop=(kb == nk - 1))
                    P.mm(pden[0:64, c0:512], onesb.v(), pt_[:, c0:512],
                         start=(kb == 0), stop=(kb == nk - 1))
                P.dve("reciprocal", out=rden[ub].v(), in_=pden[0:64, :])
                P.dve("tensor_tensor", out=ot[ub].v(), in0=pnum[0:64, :], in1=rden[ub].v(), op=ALU.mult)
                P.pool("tensor_tensor", out=ot[ub].v(), in0=ot[ub].v(), in1=OG[hb][:, j * 512:(j + 1) * 512], op=ALU.mult)
                P.dma(XT[h * 64:(h + 1) * 64, j * 512:(j + 1) * 512], ot[ub].v(), lane="sp", final=True)


def build_FOX(nc):
    P = Prog(nc)
    hT = P.dram("hT", [D, S], F32, "ExternalInput")
    grp = dict(wq=P.dram("wq", [D, 512], F32, "ExternalInput").v(), wk=P.dram("wk", [D, 512], F32, "ExternalInput").v(),
               wv=P.dram("wv", [D, 512], F32, "ExternalInput").v(), wog=P.dram("wog", [D, 512], F32, "ExternalInput").v(),
               wf=P.dram("wf", [D, NH], F32, "ExternalInput").v(), bf=P.dram("bf", [1, NH], F32, "ExternalInput").v(),
               XT=P.dram("XT", [512, S], F32, "ExternalOutput").v())
    body_FOX(P, hT.v(), [grp])
    P.emit()
    P.close()
    return nc


D = 1024
S = 4096
NB = S // 128
NQ = S // 512
NH = 8


def body_SB(P, hT, groups):
    hTb = P.sbuf([128, 8, S], BF16, "hTb")
    wqb = P.sbuf([128, 8, 512], BF16, "wqb")
    wkb = P.sbuf([128, 8, 512], BF16, "wkb")
    wvb = P.sbuf([128, 8, 512], BF16, "wvb")
    Vall = P.sbuf([128, NB, 512], BF16, "Vall")
    QT = [P.sbuf([64, S], BF16, "QT%d" % i) for i in range(2)]
    KT = [P.sbuf([64, S], BF16, "KT%d" % i) for i in range(2)]
    tmpf = P.sbuf([128, 128], F32, "tmpf")
    strictUb = P.sbuf([128, 128], BF16, "strictUb")
    negTriLb = P.sbuf([128, 128], BF16, "negTriLb")
    negones = P.sbuf([128, 128], BF16, "negones")
    zerosb = P.sbuf([128, 64], BF16, "zerosb")
    et = [P.sbuf([128, 512], F32, "et%d" % i) for i in range(2)]
    spb = [P.sbuf([128, 512], BF16, "spb%d" % i) for i in range(3)]
    At = [P.sbuf([128, 512], BF16, "At%d" % i) for i in range(3)]
    Lsum = [P.sbuf([128, 512], BF16, "Lsum%d" % i) for i in range(2)]
    ot = [P.sbuf([64, 512], F32, "ot%d" % i) for i in range(2)]
    ps = [P.psum([128, 512], F32, "ps%d" % i) for i in range(8)]

    for k in range(8):
        for c4 in range(4):
            P.dma(hTb[:, k, c4 * 1024:(c4 + 1) * 1024], hT[k * 128:(k + 1) * 128, c4 * 1024:(c4 + 1) * 1024], lane="pool")
    P.op("pool", "memset", extra_writes=[tmpf], ap=tmpf.v().ap, constant=1.0)
    P.pool("affine_select", out=tmpf.v(), in_=tmpf.v(), pattern=[[1, 128]],
           compare_op=ALU.is_gt, fill=0.0, base=0, channel_multiplier=-1)
    P.pool("tensor_copy", out=strictUb.v(), in_=tmpf.v())
    P.op("pool", "memset", extra_writes=[tmpf], ap=tmpf.v().ap, constant=-1.0)
    P.pool("affine_select", out=tmpf.v(), in_=tmpf.v(), pattern=[[-1, 128]],
           compare_op=ALU.is_ge, fill=0.0, base=0, channel_multiplier=1)
    P.pool("tensor_copy", out=negTriLb.v(), in_=tmpf.v())
    P.op("pool", "memset", extra_writes=[negones], ap=negones.v().ap, constant=-1.0)
    P.op("pool", "memset", extra_writes=[zerosb], ap=zerosb.v().ap, constant=0.0)

    unit = 0
    sc = 0
    for grp in groups:
        wq, wk, wv, XT = (grp[n] for n in ("wq", "wk", "wv", "XT"))
        for wsrc, wdst in ((wq, wqb), (wk, wkb), (wv, wvb)):
            P.dma(wdst.v(), wsrc.rearrange("(k p) f -> p k f", p=128), lane="pool")
        for kb in range(NB):
            pv = ps[4 + kb % 4]
            for k in range(8):
                P.mm(pv.v(), hTb[:, k, kb * 128:(kb + 1) * 128], wvb[:, k, :], start=(k == 0), stop=(k == 7))
            if kb % 2 == 0:
                P.dve("tensor_copy", out=Vall[:, kb, :], in_=pv.v())
            else:
                P.act("copy", out=Vall[:, kb, :], in_=pv.v())

        for h in range(NH):
            hb = h % 2
            for j in range(NQ):
                for (wsrc, dst, scl) in ((wqb, QT[hb], 0.125), (wkb, KT[hb], 1.0)):
                    pp = ps[sc % 2]
                    sc += 1
                    for k in range(8):
                        P.mm(pp[0:64, :], wsrc[:, k, h * 64:(h + 1) * 64], hTb[:, k, j * 512:(j + 1) * 512],
                             start=(k == 0), stop=(k == 7))
                    P.dve("tensor_scalar", out=dst[:, j * 512:(j + 1) * 512], in0=pp[0:64, :],
                          scalar1=scl, scalar2=None, op0=ALU.mult)
            for j in range(NQ):
                ub = unit % 2
                unit += 1
                nk = 4 * j + 4
                pnum = ps[4 + ub]
                ls = Lsum[ub]
                P.op("pool", "memset", extra_writes=[ls], ap=ls.v().ap, constant=0.0)
                P.mm(pnum[0:64, :], zerosb.v(), hTb[:, 0, 0:512], start=True, stop=False)
                for kb in range(nk - 1, -1, -1):
                    i = kb - 4 * j
                    c0 = 128 * i if i > 0 else 0
                    pA = ps[sc % 2]
                    pB = ps[2 + sc % 2]
                    e_ = et[sc % 2]
                    sp_ = spb[sc % 3]
                    a_ = At[sc % 3]
                    sc += 1
                    kT = KT[hb][:, kb * 128:(kb + 1) * 128]
                    qT = QT[hb][:, j * 512 + c0:(j + 1) * 512]
                    P.mm(pA[:, c0:512], kT, qT)
                    P.act("activation", out=e_[:, c0:512], in_=pA[:, c0:512], func=AF.Exp)
                    P.act("activation", out=sp_[:, c0:512], in_=e_[:, c0:512], func=AF.Ln, bias=1.0)
                    if i >= 0:
                        P.pool("tensor_tensor", out=sp_[:, c0:c0 + 128], in0=sp_[:, c0:c0 + 128],
                               in1=strictUb.v(), op=ALU.mult)
                    first = (kb == nk - 1)
                    P.mm(pB[:, c0:512], kT, qT, start=True, stop=False)
                    P.mm(pB[:, c0:512], negTriLb.v(), sp_[:, c0:512], start=False, stop=first)
                    if not first:
                        P.mm(pB[:, c0:512], negones.v(), ls[:, c0:512], start=False, stop=True)
                    P.act("activation", out=a_[:, c0:512], in_=pB[:, c0:512], func=AF.Exp)
                    if i >= 0:
                        P.pool("tensor_tensor", out=a_[:, c0:c0 + 128], in0=a_[:, c0:c0 + 128],
                               in1=strictUb.v(), op=ALU.mult)
                    P.mm(pnum[0:64, c0:512], Vall[:, kb, h * 64:(h + 1) * 64], a_[:, c0:512],
                         start=False, stop=(kb == 0))
                    if kb > 0:
                        P.dve("tensor_tensor", out=ls[:, c0:512], in0=ls[:, c0:512], in1=sp_[:, c0:512], op=ALU.add)
                P.dve("tensor_copy", out=ot[ub].v(), in_=pnum[0:64, :])
                P.dma(XT[h * 64:(h + 1) * 64, j * 512:(j + 1) * 512], ot[ub].v(), lane="sp", final=True)


def build_SB(nc):
    P = Prog(nc)
    hT = P.dram("hT", [D, S], F32, "ExternalInput")
    grp = dict(wq=P.dram("wq", [D, 512], F32, "ExternalInput").v(), wk=P.dram("wk", [D, 512], F32, "ExternalInput").v(),
               wv=P.dram("wv", [D, 512], F32, "ExternalInput").v(), XT=P.dram("XT", [512, S], F32, "ExternalOutput").v())
    body_SB(P, hT.v(), [grp])
    P.emit()
    P.close()
    return nc


import math

D = 1024
S = 4096
NSB = S // 128
NH = 8
CNEG = -math.exp(-0.5)
GN_EPS = 64e-5


def body_RW(P, hT, muT, w1, a1, g1, groups, nsb=NSB):
    DEBUG = False
    dbg = None

    def dump(view, idx):
        pass

    def dump2(view, idx, rows, cols):
        pass

    def sb_(shape, dt, name):
        return P.sbuf(shape, dt, name)

    mut = sb_([128, 8, 6], F32, "mut")
    omut = sb_([128, 8, 6], F32, "omut")
    Wa = {}
    Wb = {}
    for nm, ncol in (("r", 512), ("k", 512), ("v", 512), ("w1", 64), ("a1", 64), ("g1", 128)):
        Wa[nm] = sb_([128, 8, ncol], BF16, "Wa_" + nm)
        Wb[nm] = sb_([128, 8, ncol], BF16, "Wb_" + nm)
    w2b = sb_([64, 512], BF16, "w2b")
    a2b = sb_([64, 512], BF16, "a2b")
    g2b = sb_([128, 512], BF16, "g2b")
    vt = [sb_([128, 512], F32, "vec%d" % i) for i in range(7)]
    W0, A0, KK_, KA_, GNG, GNB, RK = vt

    wstl = [sb_([128, 512], F32, "wsl%d" % i) for i in range(4)]
    P.dma(mut.v(), muT.v(), lane="sp")
    P.dve("tensor_scalar", out=omut.v(), in0=mut.v(), scalar1=-1.0, scalar2=1.0, op0=ALU.mult, op1=ALU.add)

    identf = sb_([128, 128], F32, "identf")
    P.op("pool", "memset", extra_writes=[identf], ap=identf.v().ap, constant=1.0)
    P.pool("affine_select", out=identf.v(), in_=identf.v(), pattern=[[-1, 128]],
           compare_op=ALU.is_equal, fill=0.0, base=0, channel_multiplier=1)
    BD = sb_([128, 128], F32, "BD")
    bd3 = BD.v().rearrange("p (c i) -> p c i", c=4)
    P.op("pool", "memset", extra_writes=[BD], ap=BD.v().ap, constant=1.0)
    P.pool("affine_select", out=bd3, in_=bd3, pattern=[[-32, 4], [0, 32]],
           compare_op=ALU.is_ge, fill=0.0, base=0, channel_multiplier=1)
    P.pool("affine_select", out=bd3, in_=bd3, pattern=[[32, 4], [0, 32]],
           compare_op=ALU.is_ge, fill=0.0, base=31, channel_multiplier=-1)
    mAB = sb_([128, 512], F32, "mAB")
    mC = sb_([128, 256], F32, "mC")
    triBD = sb_([128, 128], F32, "triBD")
    blkBD = sb_([128, 128], F32, "blkBD")
    P.pool("affine_select", out=mAB[:, 0:128], in_=BD.v(), pattern=[[1, 128]],
           compare_op=ALU.is_gt, fill=0.0, base=0, channel_multiplier=-1)
    P.pool("affine_select", out=mAB[:, 128:256], in_=BD.v(), pattern=[[1, 128]],
           compare_op=ALU.is_ge, fill=0.0, base=0, channel_multiplier=-1)
    P.pool("tensor_copy", out=mAB[:, 256:512], in_=mAB[:, 0:256])
    P.pool("affine_select", out=mC[:, 0:128], in_=BD.v(), pattern=[[-1, 128]],
           compare_op=ALU.is_gt, fill=0.0, base=0, channel_multiplier=1)
    P.pool("tensor_copy", out=mC[:, 128:256], in_=mC[:, 0:128])
    P.pool("tensor_scalar", out=triBD.v(), in0=mAB[:, 128:256], scalar1=CNEG, scalar2=None, op0=ALU.mult)
    P.pool("tensor_scalar", out=blkBD.v(), in0=BD.v(), scalar1=CNEG, scalar2=None, op0=ALU.mult)
    RMexp = sb_([128, 4, 64], F32, "RMexp")
    P.op("pool", "memset", extra_writes=[RMexp], ap=RMexp.v().ap, constant=1.0)
    P.pool("affine_select", out=RMexp.v(), in_=RMexp.v(), pattern=[[-32, 4], [0, 64]],
           compare_op=ALU.is_ge, fill=0.0, base=0, channel_multiplier=1)
    P.pool("affine_select", out=RMexp.v(), in_=RMexp.v(), pattern=[[32, 4], [0, 64]],
           compare_op=ALU.is_ge, fill=0.0, base=31, channel_multiplier=-1)
    CM = sb_([64, 4, 128], F32, "CM")
    cm4 = CM.v().rearrange("p c (d i) -> p c d i", d=4)
    P.op("pool", "memset", extra_writes=[CM], ap=CM.v().ap, constant=1.0)
    P.pool("affine_select", out=cm4, in_=cm4, pattern=[[1, 4], [-1, 4], [0, 32]],
           compare_op=ALU.is_equal, fill=0.0, base=0, channel_multiplier=0)
    Sel = sb_([128, 4], F32, "Sel")
    P.op("pool", "memset", extra_writes=[Sel], ap=Sel.v().ap, constant=1.0)
    P.pool("affine_select", out=Sel.v(), in_=Sel.v(), pattern=[[-32, 4]],
           compare_op=ALU.is_equal, fill=0.0, base=0, channel_multiplier=1)

    hg = [sb_([128, 8, 513], BF16, "hg%d" % i) for i in range(1)]
    th = [sb_([64, 512], BF16, "th%d" % i) for i in range(1)]
    xa = [sb_([64, 512], BF16, "xa%d" % i) for i in range(1)]
    sgT = [sb_([128, 512], BF16, "sgT%d" % i) for i in range(1)]

    def f32t(name, n=1):
        return [sb_([128, 512], F32, "%s%d" % (name, i)) for i in range(n)]
    r_t = f32t("r_t", 1)
    k_t = f32t("k_t", 1)
    V_t = f32t("V_t", 1)
    sgd = f32t("sgd", 1)
    lwc = f32t("lwc", 1)
    tmpA = f32t("tmpA", 1)
    tmpB = f32t("tmpB", 1)
    a_t = f32t("a_t", 1)
    kk_t = f32t("kk_t", 1)
    kp_t = f32t("kp_t", 1)
    ka_t = a_t
    E2 = f32t("E2", 1)
    E4 = sgd
    At = [wstl[0]]
    Bt = [wstl[1]]
    Kt = [wstl[2]]
    Rt = [wstl[3]]
    Bh = f32t("Bh", 1)
    Kh = f32t("Kh", 1)
    E5 = f32t("E5", 1)
    g_t = f32t("g_t", 1)
    bon = f32t("bon", 1)
    Vm = [sb_([128, 8, 4, 64], F32, "Vm%d" % i) for i in range(1)]
    ss8 = sb_([128, 8], F32, "ss8")
    rk8 = sb_([128, 8], F32, "rk8")
    TT = [sb_([64, 512], F32, "TT%d" % i) for i in range(2)]
    PP = [[sb_([128, 256], F32, "PP%d_%d" % (i, j)) for j in range(2)] for i in range(2)]
    XX = [[sb_([128, 192], F32, "XX%d_%d" % (i, j)) for j in range(2)] for i in range(2)]
    KrKh = [sb_([128, 192], F32, "KrKh%d" % i) for i in range(2)]
    AkaT = [sb_([128, 128], F32, "AkaT%d" % i) for i in range(2)]
    G1 = [sb_([64, 128], F32, "G1_%d" % i) for i in range(2)]
    G1pad = [[sb_([64, 4, 128], F32, "G1pad%d_%d" % (i, h)) for h in range(NH)] for i in range(1)]
    G2H2 = [sb_([128, 192], F32, "G2H2_%d" % i) for i in range(2)]
    X2m = [sb_([128, 4, 64], F32, "X2m%d" % i) for i in range(2)]
    WCT = [sb_([64, 4], F32, "WCT%d" % i) for i in range(2)]
    H1 = [[sb_([64, 4, 64], F32, "H1_%d_%d" % (i, h)) for h in range(NH)] for i in range(1)]
    SvT = [sb_([64, 4, NH, 64], F32, "SvT%d" % i) for i in range(1)]
    ST = [sb_([64, NH, 64], F32, "ST%d" % i) for i in range(2)]
    y_t = f32t("y_t", 1)
    s1 = sb_([128, 8], F32, "s1")
    s2 = sb_([128, 8], F32, "s2")
    o_t = f32t("o_t", 1)
    ysq = o_t

    psG = [P.psum([128, 512], F32, "psG%d" % i) for i in range(2)]
    psY = [P.psum([128, 512], F32, "psY%d" % i) for i in range(1)]
    psS = P.psum([128, 512], F32, "psS")
    B0, B1, B2, B3 = [P.psum([128, 512], F32, "B%d" % i) for i in range(4)]


    def bc8(tile8):
        return View(tile8, tile8.tensor[:, :].unsqueeze(2).to_broadcast([128, 8, 64]))

    def v3(view):
        return view.rearrange("p (h j) -> p h j", h=8)

    gi = 0
    for grp in groups:
        wr, wk, wv, w2, a2, g2, vecs = (grp[n] for n in ("wr", "wk", "wv", "w2", "a2", "g2", "vecs"))
        for ci, (nm, src, ncol) in enumerate((("r", wr, 512), ("k", wk, 512), ("v", wv, 512),
                                              ("w1", w1, 64), ("a1", a1, 64), ("g1", g1, 128))):
            for k in range(8):
                wk_ = wstl[(ci * 8 + k) % 4]
                P.dma(wk_[:, 0:ncol], src[k * 128:(k + 1) * 128, :], lane="sp")
                P.dve("tensor_scalar", out=Wb[nm][:, k, :], in0=wk_[:, 0:ncol], scalar1=mut[:, k, ci:ci + 1],
                      scalar2=None, op0=ALU.mult)
                P.pool("tensor_scalar", out=Wa[nm][:, k, :], in0=wk_[:, 0:ncol], scalar1=omut[:, k, ci:ci + 1],
                       scalar2=None, op0=ALU.mult)
        P.dma(w2b.v(), w2, lane="pool")
        P.dma(a2b.v(), a2, lane="pool")
        P.dma(g2b.v(), g2, lane="pool")
        for i in range(7):
            P.dma(vt[i].v(), View(vecs[i], vecs[i].tensor.broadcast_to([128, 512])), lane="sp")
        P.op("pool", "memset", extra_writes=[ST[0]], ap=ST[0].v().ap, constant=0.0)
        st_cur = 0
        for sb in range(nsb):
            q = sb % 4
            g = sb // 4
            gb = 0
            pb = 0
            yb = 0
            t0 = sb * 128
            if q == 0:
                hgt = hg[gb]
                for k in range(8):
                    if g == 0:
                        P.op("pool", "memset", extra_writes=[hgt], ap=hgt[:, k, 0:1].ap, constant=0.0)
                        P.dma(hgt[:, k, 1:513], hT[k * 128:(k + 1) * 128, 0:512], lane="pool")
                    else:
                        P.dma(hgt[:, k, 0:513], hT[k * 128:(k + 1) * 128, g * 512 - 1:(g + 1) * 512], lane="pool")
                for nm, dst, fn, rows in (("w1", th[gb], AF.Tanh, 64), ("a1", xa[gb], None, 64), ("g1", sgT[gb], AF.Sigmoid, 128)):
                    pp = psG[gi % 2]
                    gi += 1
                    for k in range(8):
                        P.mm(pp[0:rows, :], Wa[nm][:, k, :], hgt[:, k, 1:513], start=(k == 0), stop=False)
                        P.mm(pp[0:rows, :], Wb[nm][:, k, :], hgt[:, k, 0:512], start=False, stop=(k == 7))
                    if fn is None:
                        P.dve("tensor_copy", out=dst.v(), in_=pp[0:rows, :])
                    else:
                        P.act("activation", out=dst.v(), in_=pp[0:rows, :], func=fn)
            hgt = hg[gb]
            cur = lambda k: hgt[:, k, 1 + q * 128:1 + (q + 1) * 128]
            prv = lambda k: hgt[:, k, q * 128:(q + 1) * 128]

            def proj(nm):
                nonlocal gi
                pp = psG[gi % 2]
                gi += 1
                for k in range(8):
                    P.mm(pp.v(), cur(k), Wa[nm][:, k, :], start=(k == 0), stop=False)
                    P.mm(pp.v(), prv(k), Wb[nm][:, k, :], start=False, stop=(k == 7))
                return pp

            def nextps():
                nonlocal gi
                pp = psG[gi % 2]
                gi += 1
                return pp
            pp = proj("r")
            P.act("copy", out=r_t[pb].v(), in_=pp.v())
            pp = proj("k")
            P.act("copy", out=k_t[0].v(), in_=pp.v())
            pp = proj("v")
            P.act("copy", out=V_t[pb].v(), in_=pp.v())
            if sb == 0:
                dump(r_t[0].v(), 0); dump(k_t[0].v(), 1); dump(V_t[0].v(), 2)
            pp = nextps()
            P.mm(pp.v(), th[gb][:, q * 128:(q + 1) * 128], w2b.v())
            P.dve("tensor_tensor", out=tmpA[0].v(), in0=pp.v(), in1=W0.v(), op=ALU.add)
            P.act("activation", out=sgd[0].v(), in_=tmpA[0].v(), func=AF.Sigmoid)
            if sb == 0:
                dump(sgd[0].v(), 3)
            pp = nextps()
            P.mm(pp.v(), xa[gb][:, q * 128:(q + 1) * 128], a2b.v())
            P.dve("tensor_tensor", out=tmpA[0].v(), in0=pp.v(), in1=A0.v(), op=ALU.add)
            P.act("activation", out=a_t[0].v(), in_=tmpA[0].v(), func=AF.Sigmoid)
            pp = nextps()
            P.mm(pp.v(), sgT[gb][:, q * 128:(q + 1) * 128], g2b.v())
            P.act("copy", out=g_t[pb].v(), in_=pp.v())
            pl = nextps()
            P.mm(pl.v(), triBD.v(), sgd[0].v())
            P.act("copy", out=lwc[0].v(), in_=pl.v())
            if sb == 0:
                dump(lwc[0].v(), 4); dump(a_t[0].v(), 5); dump(g_t[0].v(), 6)
            pe_ = nextps()
            P.mm(pe_.v(), blkBD.v(), sgd[0].v())
            P.act("activation", out=E5[pb].v(), in_=pe_.v(), func=AF.Exp)
            P.dve("tensor_tensor", out=tmpB[0].v(), in0=pe_.v(), in1=lwc[0].v(), op=ALU.subtract)
            P.dve("scalar_tensor_tensor", out=tmpA[0].v(), in0=sgd[0].v(), scalar=-CNEG, in1=lwc[0].v(),
                  op0=ALU.mult, op1=ALU.add)
            P.act("activation", out=tmpA[0].v(), in_=tmpA[0].v(), func=AF.Exp)
            P.act("activation", out=E4[0].v(), in_=tmpB[0].v(), func=AF.Exp)
            P.act("activation", out=E2[0].v(), in_=lwc[0].v(), func=AF.Exp, scale=-1.0)
            P.act("activation", out=lwc[0].v(), in_=lwc[0].v(), func=AF.Exp)
            P.pool("tensor_tensor", out=kk_t[0].v(), in0=k_t[0].v(), in1=KK_.v(), op=ALU.mult)
            P.pool("tensor_tensor", out=tmpB[0].v(), in0=kk_t[0].v(), in1=kk_t[0].v(), op=ALU.mult)
            P.dve("tensor_reduce", out=ss8.v(), in_=v3(tmpB[0].v()), axis=AX.X, op=ALU.add)
            P.act("activation", out=ss8.v(), in_=ss8.v(), func=AF.Sqrt)
            P.dve("tensor_scalar", out=ss8.v(), in0=ss8.v(), scalar1=1e-12, scalar2=None, op0=ALU.max)
            P.dve("reciprocal", out=ss8.v(), in_=ss8.v())
            P.dve("tensor_tensor", out=v3(kk_t[0].v()), in0=v3(kk_t[0].v()), in1=bc8(ss8), op=ALU.mult)
            P.dve("scalar_tensor_tensor", out=kp_t[0].v(), in0=a_t[0].v(), scalar=-1.0, in1=KA_.v(),
                   op0=ALU.add, op1=ALU.mult)
            P.dve("scalar_tensor_tensor", out=kp_t[0].v(), in0=kp_t[0].v(), scalar=1.0, in1=k_t[0].v(),
                   op0=ALU.add, op1=ALU.mult)
            P.pool("tensor_tensor", out=ka_t[0].v(), in0=kk_t[0].v(), in1=a_t[0].v(), op=ALU.mult)
            P.dve("scalar_tensor_tensor", out=At[pb].v(), in0=kk_t[0].v(), scalar=-1.0, in1=tmpA[0].v(),
                  op0=ALU.mult, op1=ALU.mult)
            P.pool("tensor_tensor", out=Bt[pb].v(), in0=ka_t[0].v(), in1=E2[0].v(), op=ALU.mult)
            P.dve("tensor_tensor", out=Kt[pb].v(), in0=kp_t[0].v(), in1=E2[0].v(), op=ALU.mult)
            P.pool("tensor_tensor", out=Rt[pb].v(), in0=r_t[pb].v(), in1=lwc[0].v(), op=ALU.mult)
            P.dve("tensor_tensor", out=Bh[pb].v(), in0=ka_t[0].v(), in1=E4[0].v(), op=ALU.mult)
            P.pool("tensor_tensor", out=Kh[pb].v(), in0=kp_t[0].v(), in1=E4[0].v(), op=ALU.mult)
            if sb == 0:
                dump(At[0].v(), 7); dump(Bt[0].v(), 8); dump(Kt[0].v(), 9); dump(Rt[0].v(), 10)
                dump(Bh[0].v(), 11); dump(Kh[0].v(), 12); dump(E5[0].v(), 13); dump(kk_t[0].v(), 14); dump(kp_t[0].v(), 15)
            for c in range(4):
                P.pool("tensor_scalar", out=Vm[pb][:, :, c, :], in0=v3(V_t[pb].v()), scalar1=RMexp[:, c, 0:1],
                       scalar2=None, op0=ALU.mult)
            P.dve("tensor_tensor", out=tmpB[0].v(), in0=r_t[pb].v(), in1=kp_t[0].v(), op=ALU.mult)
            P.dve("tensor_tensor", out=tmpB[0].v(), in0=tmpB[0].v(), in1=RK.v(), op=ALU.mult)
            P.dve("tensor_reduce", out=rk8.v(), in_=v3(tmpB[0].v()), axis=AX.X, op=ALU.add)
            P.dve("tensor_tensor", out=v3(bon[pb].v()), in0=v3(V_t[pb].v()), in1=bc8(rk8), op=ALU.mult)

            pY = psY[yb]
            for h in range(NH):
                u = h % 2
                hs = slice(h * 64, (h + 1) * 64)
                P.pe("transpose", out=B0[0:64, 0:128], in_=At[pb][:, hs], identity=identf.v())
                P.pe("transpose", out=B0[0:64, 128:256], in_=Rt[pb][:, hs], identity=identf.v())
                P.pe("transpose", out=B0[0:64, 256:384], in_=Bt[pb][:, hs], identity=identf.v())
                P.pe("transpose", out=B0[0:64, 384:512], in_=Kt[pb][:, hs], identity=identf.v())
                P.act("copy", out=TT[u][:, 0:256], in_=B0[0:64, 0:256])
                P.dve("tensor_copy", out=TT[u][:, 256:512], in_=B0[0:64, 256:512])
                P.mm(B2[0:64, 256:260], E5[pb][:, hs], Sel.v())
                P.act("copy", out=WCT[u].v(), in_=B2[0:64, 256:260])
                P.mm(B1[:, 0:256], TT[u][:, 256:384], TT[u][:, 0:256])
                P.mm(B1[:, 256:512], TT[u][:, 384:512], TT[u][:, 0:256])
                P.mm(B2[:, 0:256], TT[u][:, 0:128], TT[u][:, 256:512])
                pk = PP[u][0]
                xx = XX[u][0]
                P.dve("tensor_tensor", out=pk[:, 0:128], in0=B1[:, 0:128], in1=mAB[:, 0:128], op=ALU.mult)
                P.dve("tensor_tensor", out=xx[:, 0:128], in0=B1[:, 128:256], in1=mAB[:, 128:256], op=ALU.mult)
                P.pool("tensor_copy", out=xx[:, 128:192], in_=Bh[pb][:, hs])
                P.dve("tensor_tensor", out=AkaT[u].v(), in0=B2[:, 128:256], in1=mC[:, 128:256], op=ALU.mult)
                P.dve("tensor_tensor", out=pk[:, 128:256], in0=B2[:, 0:128], in1=mC[:, 0:128], op=ALU.mult)
                P.dve("tensor_tensor", out=KrKh[u][:, 0:128], in0=B1[:, 384:512], in1=mAB[:, 128:256], op=ALU.mult)
                P.pool("tensor_copy", out=KrKh[u][:, 128:192], in_=Kh[pb][:, hs])
                if sb == 0 and h == 0:
                    dump2(TT[u].v(), 18, 64, 512); dump2(PP[u][0].v(), 19, 128, 256); dump2(XX[u][0].v(), 20, 128, 192)
                    dump2(KrKh[u].v(), 21, 128, 192)
                cp = 0
                for lev in range(5):
                    pk = PP[u][cp]
                    xx = XX[u][cp]
                    xn = XX[u][1 - cp]
                    P.mm(B0[:, 0:192], pk[:, 128:256], xx.v())
                    P.dve("tensor_tensor", out=xn.v(), in0=B0[:, 0:192], in1=xx.v(), op=ALU.add)
                    if lev < 4:
                        pn = PP[u][1 - cp]
                        P.mm(B1[:, 0:128], pk[:, 128:256], pk[:, 0:128])
                        P.mm(B1[:, 128:256], pk[:, 0:128], pk[:, 128:256])
                        P.act("copy", out=pn.v(), in_=B1[:, 0:256])
                    cp = 1 - cp
                xf = XX[u][cp]
                P.mm(B2[0:64, 0:128], At[pb][:, hs], xf[:, 0:128])
                P.dve("tensor_tensor", out=G1[u].v(), in0=B2[0:64, 0:128], in1=TT[u][:, 128:256], op=ALU.add)
                P.pool("tensor_tensor", out=G1pad[pb][h].v(),
                       in0=View(G1[u], G1[u].tensor[:, :].unsqueeze(1).to_broadcast([64, 4, 128])),
                       in1=CM.v(), op=ALU.mult)
                P.mm(B3[:, 0:192], AkaT[u].v(), xf.v())
                P.dve("tensor_tensor", out=G2H2[u].v(), in0=B3[:, 0:192], in1=KrKh[u].v(), op=ALU.add)
                if sb == 0 and h == 0:
                    dump2(xf.v(), 22, 128, 192); dump2(G2H2[u].v(), 23, 128, 192)
                P.pool("tensor_tensor", out=X2m[u].v(),
                       in0=View(xf, xf.tensor[:, 128:192].unsqueeze(1).to_broadcast([128, 4, 64])),
                       in1=RMexp.v(), op=ALU.mult)
                P.mm(B2[0:64, 256:512], At[pb][:, hs], X2m[u].v().rearrange("p c j -> p (c j)"))
                for c in range(4):
                    P.dve("scalar_tensor_tensor", out=H1[pb][h][:, c, :], in0=identf[0:64, 0:64],
                          scalar=WCT[u][:, c:c + 1], in1=B2[0:64, 256 + c * 64:256 + (c + 1) * 64],
                          op0=ALU.mult, op1=ALU.add)
                P.mm(pY[:, hs], G2H2[u][:, 0:128], V_t[pb][:, hs], start=(h == 0), stop=False)
                P.mm(B1[0:64, 0:256], G2H2[u][:, 128:192], Vm[pb][:, h, :, :].rearrange("p c i -> p (c i)"))
                P.act("copy", out=SvT[pb][:, :, h, :], in_=B1[0:64, 0:256].rearrange("p (c i) -> p c i", c=4))

            for c in range(4):
                stc = ST[st_cur]
                stn = ST[1 - st_cur]
                for h in range(NH):
                    hs = slice(h * 64, (h + 1) * 64)
                    P.mm(pY[:, hs], G1pad[pb][h][:, c, :], stc[:, h, :], start=False, stop=(c == 3))
                    P.mm(psS[0:64, hs], H1[pb][h][:, c, :], stc[:, h, :])
                P.dve("tensor_tensor", out=stn.v().rearrange("p h i -> p (h i)"), in0=psS[0:64, :],
                      in1=SvT[pb][:, c, :, :].rearrange("p h i -> p (h i)"), op=ALU.add)
                st_cur = 1 - st_cur

            P.act("copy", out=y_t[0].v(), in_=pY.v())
            if sb == 0:
                dump(y_t[0].v(), 16); dump(bon[0].v(), 17)
                dump(ST[st_cur].v().rearrange("p h i -> p (h i)"), 18) if False else None
            P.dve("tensor_reduce", out=s1.v(), in_=v3(y_t[0].v()), axis=AX.X, op=ALU.add)
            P.pool("tensor_tensor", out=ysq[0].v(), in0=y_t[0].v(), in1=y_t[0].v(), op=ALU.mult)
            P.dve("tensor_reduce", out=s2.v(), in_=v3(ysq[0].v()), axis=AX.X, op=ALU.add)
            P.dve("tensor_scalar", out=s1.v(), in0=s1.v(), scalar1=1.0 / 64, scalar2=None, op0=ALU.mult)
            P.dve("tensor_scalar", out=s2.v(), in0=s2.v(), scalar1=1.0 / 64, scalar2=GN_EPS, op0=ALU.mult, op1=ALU.add)
            P.dve("tensor_tensor", out=rk8.v(), in0=s1.v(), in1=s1.v(), op=ALU.mult)
            P.dve("tensor_tensor", out=s2.v(), in0=s2.v(), in1=rk8.v(), op=ALU.subtract)
            P.act("activation", out=s2.v(), in_=s2.v(), func=AF.Sqrt)
            P.dve("reciprocal", out=s2.v(), in_=s2.v())
            P.dve("tensor_tensor", out=v3(y_t[0].v()), in0=v3(y_t[0].v()), in1=bc8(s1), op=ALU.subtract)
            P.dve("tensor_tensor", out=v3(y_t[0].v()), in0=v3(y_t[0].v()), in1=bc8(s2), op=ALU.mult)
            P.pool("tensor_tensor", out=y_t[0].v(), in0=y_t[0].v(), in1=GNG.v(), op=ALU.mult)
            P.pool("tensor_tensor", out=y_t[0].v(), in0=y_t[0].v(), in1=GNB.v(), op=ALU.add)
            P.pool("tensor_tensor", out=y_t[0].v(), in0=y_t[0].v(), in1=bon[pb].v(), op=ALU.add)
            P.pool("tensor_tensor", out=o_t[pb].v(), in0=y_t[0].v(), in1=g_t[pb].v(), op=ALU.mult)
            if "X" in grp:
                P.dma(grp["X"][t0:t0 + 128, :], o_t[pb].v(), lane="sp", final=True)
            else:
                pto = psG[gi % 2]
                gi += 1
                for k4 in range(4):
                    P.pe("transpose", out=pto[:, k4 * 128:(k4 + 1) * 128], in_=o_t[pb][:, k4 * 128:(k4 + 1) * 128],
                         identity=identf.v())
                P.act("copy", out=y_t[0].v(), in_=pto.v())
                P.dma(grp["XT"][:, t0:t0 + 128].rearrange("(k p) t -> p k t", p=128),
                      y_t[0].v().rearrange("p (k t) -> p k t", k=4), lane="sp", final=True)


def build_RW(nc, nsb=NSB):
    P = Prog(nc)
    hT = P.dram("hT", [D, S], F32, "ExternalInput")
    muT = P.dram("muT", [128, 8, 6], F32, "ExternalInput")
    wr = P.dram("wr", [D, 512], F32, "ExternalInput")
    wk = P.dram("wk", [D, 512], F32, "ExternalInput")
    wv = P.dram("wv", [D, 512], F32, "ExternalInput")
    w1 = P.dram("w1", [D, 64], F32, "ExternalInput")
    a1 = P.dram("a1", [D, 64], F32, "ExternalInput")
    g1 = P.dram("g1", [D, 128], F32, "ExternalInput")
    w2 = P.dram("w2", [64, 512], F32, "ExternalInput")
    a2 = P.dram("a2", [64, 512], F32, "ExternalInput")
    g2 = P.dram("g2", [128, 512], F32, "ExternalInput")
    vecs = P.dram("vecs", [7, 512], F32, "ExternalInput")
    X = P.dram("X", [S, 512], F32, "ExternalOutput")
    grp = dict(wr=wr.v(), wk=wk.v(), wv=wv.v(), w2=w2.v(), a2=a2.v(), g2=g2.v(),
               vecs=[vecs[i:i + 1, :] for i in range(7)], X=X.v())
    body_RW(P, hT.v(), muT.v(), w1.v(), a1.v(), g1.v(), [grp], nsb)
    P.emit()
    P.close()
    return nc


_S, _D = 4096, 1024

_IN_SHAPES = dict(
    x_tok=[_S, _D], x_T=[_D, _S],
    ln1_g=[4, _D], ln1_b=[4, _D], ln2_g=[4, _D], ln2_b=[4, _D],
    fox_w_in=[_D, 4112], fox_b_f=[1, 16], fox_w_out=[_D, _D],
    gm_w_in=[_D, 2048], gm_b_in=[1, 2048], gm_ln_g=[1, _D], gm_ln_b=[1, _D],
    gm_wsT=[128, 8, 128], gm_bsT=[128, 8], gm_w_out=[_D, _D],
    sb_w_in=[_D, 3072], sb_w_out=[_D, _D],
    rw_muT=[128, 8, 6], rw_w_rkv=[3, _D, _D], rw_w0=[1, _D], rw_w1=[_D, 64], rw_w2=[64, _D],
    rw_a0=[1, _D], rw_a1=[_D, 64], rw_a2=[64, _D], rw_g1=[_D, 128], rw_g2=[128, _D],
    rw_k_k=[1, _D], rw_k_a=[1, _D], rw_r_k=[1, _D], rw_gn_g=[1, _D], rw_gn_b=[1, _D], rw_w_out=[_D, _D],
    router_w=[_D, 16], router_b=[1, 16],
    moe_w_gate=[4, 16, _D, 512], moe_w_up=[4, 16, _D, 512], moe_w_down=[4, 16, 512, _D],
)


def build_fused(nc, layers=(0, 1, 2, 3)):
    P = Prog(nc)
    I = {k: P.dram(k, shp, F32, "ExternalInput").v() for k, shp in _IN_SHAPES.items()}
    out = P.dram("out", [_S, _D], F32, "ExternalOutput").v()
    xT_d = P.dram("xT_d", [_D, _S], F32, "Internal").v()
    h_d = P.dram("h_d", [_S, _D], F32, "Internal").v()
    hT_d = P.dram("hT_d", [_D, _S], F32, "Internal").v()

    def t_stage(L, wout, last, first):
        for p in range(2):
            sl = slice(p * 2048, (p + 1) * 2048)
            io = dict(hin=(I["x_tok"] if first else h_d)[sl, :], xT=xT_d[:, sl], wout=wout,
                      ln=[I["ln1_g"][L:L + 1, :], I["ln1_b"][L:L + 1, :], I["ln2_g"][L:L + 1, :], I["ln2_b"][L:L + 1, :]],
                      rw=I["router_w"], rb=I["router_b"], wg=I["moe_w_gate"][L], wu=I["moe_w_up"][L],
                      wd=I["moe_w_down"][L], hout=(out if last else h_d)[sl, :])
            if not last:
                io["houtT"] = hT_d[:, sl]
            P.begin_stage()
            body_T(P, io, 2048)
            P.end_stage(last=(last and p == 1))

    nl = len(layers)
    for li, L in enumerate(layers):
        last = (li == nl - 1)
        hT = I["x_T"] if li == 0 else hT_d
        P.begin_stage()
        if L == 0:
            w = I["fox_w_in"]
            groups = [dict(wq=w[:, hg * 512:(hg + 1) * 512], wk=w[:, 1024 + hg * 512:1024 + (hg + 1) * 512],
                           wv=w[:, 2048 + hg * 512:2048 + (hg + 1) * 512], wog=w[:, 3088 + hg * 512:3088 + (hg + 1) * 512],
                           wf=w[:, 3072 + hg * 8:3072 + (hg + 1) * 8], bf=I["fox_b_f"][:, hg * 8:(hg + 1) * 8],
                           XT=xT_d[hg * 512:(hg + 1) * 512, :]) for hg in range(2)]
            body_FOX(P, hT, groups)
            wout = I["fox_w_out"]
        elif L == 1:
            body_GM(P, dict(hT=hT, w_in=I["gm_w_in"], b_in=I["gm_b_in"], ln=[I["gm_ln_g"], I["gm_ln_b"]],
                            wsT=I["gm_wsT"], bsT=I["gm_bsT"], XT=xT_d), _S)
            wout = I["gm_w_out"]
        elif L == 2:
            w = I["sb_w_in"]
            groups = [dict(wq=w[:, hg * 512:(hg + 1) * 512], wk=w[:, 1024 + hg * 512:1024 + (hg + 1) * 512],
                           wv=w[:, 2048 + hg * 512:2048 + (hg + 1) * 512], XT=xT_d[hg * 512:(hg + 1) * 512, :])
                      for hg in range(2)]
            body_SB(P, hT, groups)
            wout = I["sb_w_out"]
        else:
            groups = []
            for hg in range(2):
                cs = slice(hg * 512, (hg + 1) * 512)
                groups.append(dict(wr=I["rw_w_rkv"][0][:, cs], wk=I["rw_w_rkv"][1][:, cs], wv=I["rw_w_rkv"][2][:, cs],
                                   w2=I["rw_w2"][:, cs], a2=I["rw_a2"][:, cs], g2=I["rw_g2"][:, cs],
                                   vecs=[I[n][:, cs] for n in ("rw_w0", "rw_a0", "rw_k_k", "rw_k_a", "rw_gn_g", "rw_gn_b", "rw_r_k")],
                                   XT=xT_d[cs, :]))
            body_RW(P, hT, I["rw_muT"], I["rw_w1"], I["rw_a1"], I["rw_g1"], groups)
            wout = I["rw_w_out"]
        P.end_stage()
        t_stage(L, wout, last, li == 0)
    P.close()
    return nc


def fused_inputs(inp, b):
    c = lambda a: np.ascontiguousarray(a, dtype=np.float32)
    m = dict(x_tok=c(inp["x"][b]), x_T=c(inp["x"][b].T))
    for k in ("ln1_g", "ln1_b", "ln2_g", "ln2_b", "router_w", "moe_w_gate", "moe_w_up", "moe_w_down"):
        m[k] = c(inp[k])
    m["router_b"] = c(inp["router_b"]).reshape(1, 16)
    for k in ("fox_w_in", "fox_w_out", "gm_w_in", "gm_w_out", "sb_w_in", "sb_w_out", "rw_w_rkv", "rw_w1", "rw_w2",
              "rw_a1", "rw_a2", "rw_g1", "rw_g2", "rw_w_out"):
        m[k] = c(inp[k][0])
    m["fox_b_f"] = c(inp["fox_b_f"][0]).reshape(1, 16)
    m["gm_b_in"] = c(inp["gm_b_in"][0]).reshape(1, -1)
    for k in ("gm_ln_g", "gm_ln_b", "rw_w0", "rw_a0", "rw_k_k", "rw_k_a", "rw_r_k", "rw_gn_g", "rw_gn_b"):
        m[k] = c(inp[k][0]).reshape(1, -1)
    m["gm_wsT"] = c(inp["gm_w_s"][0].transpose(2, 0, 1))
    m["gm_bsT"] = c(inp["gm_b_s"][0].T)
    m["rw_muT"] = c(inp["rw_mu"][0].T.reshape(8, 128, 6).transpose(1, 0, 2))
    return m


from concourse.bass_utils import run_bass_kernel_spmd


def kernel(**inp):
    inp = {k: np.asarray(v) for k, v in inp.items()}
    nc = bass.Bass("TRN2", target_bir_lowering=False)
    build_fused(nc)
    per_batch = [fused_inputs(inp, b) for b in range(4)]
    maps = [per_batch[c // 2] for c in range(8)]
    res = run_bass_kernel_spmd(nc, maps, core_ids=list(range(8))).results
    out = np.stack([res[2 * b]["out"] for b in range(4)], 0)
    return out.astype(np.float32)
```

```python
import numpy as np
import concourse.bass as bass
import concourse.mybir as mybir
from contextlib import ExitStack

F32 = mybir.dt.float32
BF16 = mybir.dt.bfloat16
AF = mybir.ActivationFunctionType
ALU = mybir.AluOpType
AX = mybir.AxisListType

SAME_SYNC_DEFAULT = True
ENGS = ["pe", "dve", "act", "pool", "sp"]
_WRITE_KEYS = ("out", "accum_out", "out_max", "out_indices")


class Tile:
    def __init__(self, tensor, name):
        self.tensor = tensor
        self.name = name
        self.is_psum = False
        self.last_w = None
        self.readers = {}

    def __getitem__(self, idx):
        return View(self, self.tensor[idx])

    def v(self):
        return View(self, self.tensor[:])


class View:
    def __init__(self, tile, ap):
        if isinstance(tile, View):
            tile = tile.tile
        self.tile = tile
        self.ap = ap

    @property
    def tensor(self):
        return self.ap

    def v(self):
        return self

    def __getitem__(self, idx):
        return View(self.tile, self.ap[idx])

    def rearrange(self, s, **kw):
        return View(self.tile, self.ap.rearrange(s, **kw))

    def bitcast(self, dt):
        return View(self.tile, self.ap.bitcast(dt))

    def to_broadcast(self, shape):
        return View(self.tile, self.ap.to_broadcast(shape))

    def broadcast_to(self, shape):
        return View(self.tile, self.ap.broadcast_to(shape))

    def unsqueeze(self, a):
        return View(self.tile, self.ap.unsqueeze(a))

    def partition_broadcast(self, n):
        return View(self.tile, self.ap.partition_broadcast(n))


class RowBlocks:
    def __init__(self, views, bs):
        self.views, self.bs = views, bs

    def rows(self, r0, n):
        blk = self.views[r0 // self.bs]
        o = r0 % self.bs
        assert o + n <= self.bs
        return blk[o:o + n, :]


def rows_view(X, r0, n):
    return X.rows(r0, n) if isinstance(X, RowBlocks) else X[r0:r0 + n, :]


class Prog:
    def __init__(self, nc, same_engine_sync=SAME_SYNC_DEFAULT, n_dma_sems=12):
        self.nc = nc
        self.es = ExitStack()
        self.ops = {e: [] for e in ENGS}
        self.count = {e: 0 for e in ENGS}
        self.waited = {e: {} for e in ENGS}
        self.same_engine_sync = same_engine_sync
        self.sems = {}
        for e in ENGS:
            self.sems[("eng", e)] = self.es.enter_context(nc.semaphore("s_" + e))
        self.n_dma_sems = n_dma_sems
        self.dma_k = {}
        for lane in ("sp", "pool", "act"):
            self.dma_k[lane] = 0
            for i in range(n_dma_sems):
                self.sems[("dma", lane, i)] = self.es.enter_context(
                    nc.semaphore("d_%s_%d" % (lane, i)))
        self.sems[("cc",)] = self.es.enter_context(nc.semaphore("s_cc"))
        self.cc_count = 0
        self.n_tiles = 0
        self.final_tokens = []
        self.ses = None
        self.stage_id = 0

    def begin_stage(self):
        self.ses = ExitStack()
        self.stage_id += 1

    def end_stage(self, last=False):
        self.barrier()
        self.emit(last=last)
        self.ses.close()
        self.ses = None

    def sbuf(self, shape, dt, name=None):
        self.n_tiles += 1
        name = name or ("t%d" % self.n_tiles)
        if self.ses is not None:
            name = "s%d_%s" % (self.stage_id, name)
        t = (self.ses or self.es).enter_context(self.nc.sbuf_tensor(name, list(shape), dt))
        return Tile(t, name)

    def psum(self, shape, dt, name=None):
        self.n_tiles += 1
        name = name or ("p%d" % self.n_tiles)
        if self.ses is not None:
            name = "s%d_%s" % (self.stage_id, name)
        t = (self.ses or self.es).enter_context(self.nc.psum_tensor(name, list(shape), dt))
        tl = Tile(t, name)
        tl.is_psum = True
        return tl

    def dram(self, name, shape, dt, kind):
        t = self.nc.dram_tensor(name, list(shape), dt, kind=kind)
        return Tile(t.ap(), name)

    def subtile(self, tile, idx, name=None):
        return Tile(tile.tensor[idx], name or (tile.name + "_sub"))

    def _deps(self, eng, reads, writes, skip_same=False):
        deps = {}

        def add(tok):
            if tok is None:
                return
            k, v = tok
            if deps.get(k, 0) < v:
                deps[k] = v

        for t in reads:
            add(t.last_w)
        for t in writes:
            add(t.last_w)
            for k, v in t.readers.items():
                add((k, v))
        out = []
        for k, v in deps.items():
            if k == ("eng", eng):
                if skip_same:
                    continue
            if self.waited[eng].get(k, 0) >= v:
                continue
            self.waited[eng][k] = v
            out.append((k, v))
        return out

    def _commit(self, tok, reads, writes):
        k, v = tok
        for t in writes:
            t.last_w = tok
            t.readers = {}
        for t in reads:
            if t in writes:
                continue
            if t.readers.get(k, 0) < v:
                t.readers[k] = v

    def _split(self, kwargs):
        reads, writes, real = [], [], {}
        for k, a in kwargs.items():
            if isinstance(a, View):
                (writes if k in _WRITE_KEYS else reads).append(a.tile)
                real[k] = a.ap
            else:
                real[k] = a
        return reads, writes, real

    def op(self, eng, method, extra_reads=(), extra_writes=(), **kwargs):
        reads, writes, real = self._split(kwargs)
        reads += [v.tile if isinstance(v, View) else v for v in extra_reads]
        writes += [v.tile if isinstance(v, View) else v for v in extra_writes]
        if eng != "pe":
            writes += [t for t in reads if t.is_psum and t not in writes]
        waits = self._deps(eng, reads, writes,
                           skip_same=(eng == "pe" or not self.same_engine_sync))
        self.count[eng] += 1
        tok = (("eng", eng), self.count[eng])
        self.ops[eng].append((waits, method, real, tok, 1))
        self._commit(tok, reads, writes)
        return tok

    def dve(self, method, **kw):
        return self.op("dve", method, **kw)

    def act(self, method, **kw):
        return self.op("act", method, **kw)

    def pool(self, method, **kw):
        return self.op("pool", method, **kw)

    def pe(self, method, **kw):
        return self.op("pe", method, **kw)

    def mm(self, out, lhsT, rhs, start=True, stop=True, **kw):
        return self.op("pe", "matmul", out=out, lhsT=lhsT, rhs=rhs, start=start, stop=stop, **kw)

    def dma(self, out, in_, lane="sp", final=False, **kw):
        reads, writes = [in_.tile], [out.tile]
        k = self.dma_k[lane]
        self.dma_k[lane] += 1
        si = k % self.n_dma_sems
        semkey = ("dma", lane, si)
        waits = self._deps(lane, reads, writes)
        prev = 16 * (k // self.n_dma_sems)
        if prev > 0 and self.waited[lane].get(semkey, 0) < prev:
            self.waited[lane][semkey] = prev
            waits.append((semkey, prev))
        tok = (semkey, prev + 16)
        real = dict(out=out.ap, in_=in_.ap, **kw)
        self.ops[lane].append((waits, "dma_start", real, tok, 16))
        self._commit(tok, reads, writes)
        if final:
            self.final_tokens.append(tok)
        return tok

    def all_gather(self, out, in_, groups, inc=1):
        lane = "pool"
        reads, writes = [in_.tile], [out.tile]
        waits = self._deps(lane, reads, writes)
        self.cc_count += inc
        tok = (("cc",), self.cc_count)
        real = dict(kind="AllGather", op=ALU.bypass, replica_groups=groups, ins=[in_.ap], outs=[out.ap])
        self.ops[lane].append((waits, "collective_compute", real, tok, inc))
        self._commit(tok, reads, writes)
        return tok

    def barrier(self):
        allk = {}
        for e in ENGS:
            if self.count[e] > 0:
                allk[("eng", e)] = self.count[e]
        for lane in ("sp", "pool", "act"):
            k = self.dma_k[lane]
            for i in range(self.n_dma_sems):
                n = (k - i + self.n_dma_sems - 1) // self.n_dma_sems if k > i else 0
                if n > 0:
                    allk[("dma", lane, i)] = 16 * n
        if self.cc_count > 0:
            allk[("cc",)] = self.cc_count
        for e in ENGS:
            waits = []
            for k, v in allk.items():
                if self.waited[e].get(k, 0) >= v:
                    continue
                self.waited[e][k] = v
                waits.append((k, v))
            if waits:
                self.ops[e].append((waits, None, None, None, 0))

    def emit(self, last=True):
        nc = self.nc
        fin = []
        seen = {}
        for k, v in self.final_tokens:
            if seen.get(k, 0) < v:
                seen[k] = v
        fin = list(seen.items())
        engobj = {"pe": "tensor", "dve": "vector", "act": "scalar", "pool": "gpsimd", "sp": "sync"}
        with nc.Block() as block:
            for e in ENGS:
                ops = self.ops[e]
                extra = fin if (e == "sp" and last) else []

                def body(eng, ops=ops, extra=extra):
                    for waits, method, real, tok, inc in ops:
                        for k, v in waits:
                            eng.wait_ge(self.sems[k], v)
                        if method is None:
                            continue
                        ins = getattr(eng, method)(**real)
                        ins.then_inc(self.sems[tok[0]], inc)
                    for k, v in extra:
                        eng.wait_ge(self.sems[k], v)

                if not ops and not extra:
                    continue
                getattr(block, engobj[e])(body)
        self.ops = {e: [] for e in ENGS}

    def close(self):
        self.es.close()


D = 1024
NE = 16
DE = 512
ALPHA = 8 ** 0.25
LN_EPS = 1e-5


def layer_norm_tile(P, x, out, g_t, b_t, stats, mv, rstd, tmp, eps=LN_EPS):
    for c in range(2):
        P.dve("bn_stats", out=stats[:, c, :], in_=x[:, c * 512:(c + 1) * 512])
    P.dve("bn_aggr", out=mv.v(), in_=stats.v())
    P.dve("tensor_scalar", out=rstd.v(), in0=mv[:, 1:2], scalar1=eps, scalar2=None, op0=ALU.add)
    P.act("activation", out=rstd.v(), in_=rstd.v(), func=AF.Sqrt)
    P.dve("reciprocal", out=rstd.v(), in_=rstd.v())
    P.dve("tensor_scalar", out=out, in0=x, scalar1=mv[:, 0:1], scalar2=rstd[:, 0:1],
          op0=ALU.subtract, op1=ALU.mult)
    P.pool("tensor_tensor", out=out, in0=out, in1=g_t, op=ALU.mult)
    P.pool("tensor_tensor", out=out, in0=out, in1=b_t, op=ALU.add)


def body_T(P, io, TOK=2048, stop=99, sub=99):
    nc = P.nc
    NT = TOK // 128
    NC4 = TOK // 512
    hin, xT, wout, rw, rb = io["hin"], io.get("xT"), io["wout"], io["rw"], io["rb"]
    wg, wu, wd, hout = io["wg"], io["wu"], io["wd"], io["hout"]
    lnrows = io["ln"]
    houtT = io.get("houtT")

    acc = P.sbuf([128, NT, D], F32, "acc")
    acc_t = [P.subtile(acc, (slice(None), t, slice(None)), "acc%d" % t) for t in range(NT)]
    h1T = P.sbuf([128, 8, TOK], BF16, "h1T")
    arena = P.sbuf([128, 24576], BF16, "arena")
    lnt = [P.sbuf([128, D], F32, "lnt%d" % i) for i in range(4)]
    rwt = P.sbuf([128, 8, NE], F32, "rwt")
    rbt = P.sbuf([128, NE], F32, "rbt")
    ident = P.sbuf([128, 128], F32, "ident")
    hin_t = [P.sbuf([128, D], F32, "hin0")] * 2
    h1_t = [P.sbuf([128, D], F32, "h1_%d" % i) for i in range(2)]
    h1Tf = [P.sbuf([128, 8, 128], F32, "h1Tf0")] * 2
    tmp_t = [None, None]
    stats = [P.sbuf([128, 2, 6], F32, "st%d" % i) for i in range(2)]
    mv = [P.sbuf([128, 2], F32, "mv%d" % i) for i in range(2)]
    rstd = [P.sbuf([128, 1], F32, "rstd%d" % i) for i in range(2)]
    scores = P.sbuf([128, NT, NE], F32, "scores")
    comb = P.sbuf([128, NT, NE], F32, "comb")
    r_sel = P.sbuf([128, NT, NE], F32, "r_sel")
    r_cnt = P.sbuf([128, NT, NE], F32, "r_cnt")
    r_tmp = P.sbuf([128, NT, NE], F32, "r_tmp")
    r_gs = P.sbuf([128, NT, 4], F32, "r_gs")
    r_gm = P.sbuf([128, NT], F32, "r_gm")
    r_gmask = P.sbuf([128, NT, 4], F32, "r_gmask")
    r_den = P.sbuf([128, NT], F32, "r_den")
    heT = [[P.sbuf([128, 512], BF16, "heT%d_%d" % (b, f)) for f in range(4)] for b in range(2)]
    sg = [P.sbuf([128, 512], F32, "sg%d" % i) for i in range(2)]
    ps = [P.psum([128, 512], F32, "ps%d" % i) for i in range(8)]

    ar = arena.tensor
    woutb = Tile(ar[:, 0:8192].rearrange("p (k f) -> p k f", k=8), "woutb")
    xTb = Tile(ar[:, 8192:8192 + 8 * TOK].rearrange("p (k t) -> p k t", k=8), "xTb")
    wbuf = []
    for b in range(2):
        o = b * 12288
        wbuf.append(dict(
            g=Tile(ar[:, o:o + 4096].rearrange("p (k f) -> p k f", k=8), "wg%d" % b),
            u=Tile(ar[:, o + 4096:o + 8192].rearrange("p (k f) -> p k f", k=8), "wu%d" % b),
            d=Tile(ar[:, o + 8192:o + 12288].rearrange("p (k f) -> p k f", k=4), "wd%d" % b)))

    for i in range(4):
        P.dma(lnt[i].v(), View(lnrows[i], lnrows[i].tensor.broadcast_to([128, D])), lane="sp")
    P.dma(rwt.v(), rw.v().rearrange("(k p) e -> p k e", p=128), lane="sp")
    P.dma(rbt.v(), View(rb, rb.tensor[0:1, :].broadcast_to([128, NE])), lane="sp")
    P.op("pool", "memset", extra_writes=[ident], ap=ident.v().ap, constant=1.0)
    P.pool("affine_select", out=ident.v(), in_=ident.v(), pattern=[[-1, 128]],
           compare_op=ALU.is_equal, fill=0.0, base=0, channel_multiplier=1)
    P.dma(woutb.v(), wout.v().rearrange("(k p) f -> p k f", p=128), lane="pool")
    if "xT_blend" in io:
        xfull, sel = io["xT_blend"]
        selt = P.sbuf([128, 2], F32, "selt")
        stg = [P.sbuf([128, TOK], BF16, "stg%d" % i) for i in range(2)]
        P.dma(selt.v(), sel, lane="sp")
        for k in range(8):
            P.dma(stg[0].v(), xfull[k][:, 0:TOK], lane="pool")
            P.dma(stg[1].v(), xfull[k][:, TOK:2 * TOK], lane="pool")
            P.dve("tensor_scalar", out=stg[0].v(), in0=stg[0].v(), scalar1=selt[:, 0:1], scalar2=None, op0=ALU.mult)
            P.dve("scalar_tensor_tensor", out=xTb[:, k, :], in0=stg[1].v(), scalar=selt[:, 1:2], in1=stg[0].v(),
                  op0=ALU.mult, op1=ALU.add)
    else:
        for k in range(8):
            P.dma(xTb[:, k, :], xT[k * 128:(k + 1) * 128, :], lane="pool")

    for tt in range(NT):
        b = tt % 2
        if sub < 99 and tt > 0:
            break
        P.dma(hin_t[b].v(), hin[tt * 128:(tt + 1) * 128, :], lane="sp")
        pm = [ps[(2 * tt) % 4], ps[(2 * tt) % 4 + 1]]
        for half in range(2):
            for k in range(8):
                P.mm(pm[half].v(), xTb[:, k, tt * 128:(tt + 1) * 128],
                     woutb[:, k, half * 512:(half + 1) * 512], start=(k == 0), stop=(k == 7))
        if sub == 1:
            break
        for half in range(2):
            P.dve("scalar_tensor_tensor", out=hin_t[b][:, half * 512:(half + 1) * 512],
                  in0=hin_t[b][:, half * 512:(half + 1) * 512], scalar=ALPHA, in1=pm[half].v(),
                  op0=ALU.mult, op1=ALU.add)
        if sub == 2:
            break
        layer_norm_tile(P, hin_t[b].v(), h1_t[b].v(), lnt[0].v(), lnt[1].v(),
                        stats[b], mv[b], rstd[b], tmp_t[b])
        if sub == 3:
            break
        P.act("mul", out=acc_t[tt].v(), in_=h1_t[b].v(), mul=ALPHA)
        pt = [ps[4 + (2 * tt) % 4], ps[4 + (2 * tt) % 4 + 1]]
        for k in range(8):
            P.pe("transpose", out=pt[k // 4][:, (k % 4) * 128:(k % 4 + 1) * 128],
                 in_=h1_t[b][:, k * 128:(k + 1) * 128], identity=ident.v())
        if sub == 4:
            break
        for hf in range(2):
            P.act("copy", out=h1T[:, hf * 4:(hf + 1) * 4, tt * 128:(tt + 1) * 128],
                  in_=pt[hf].v().rearrange("p (k t) -> p k t", k=4))
            P.dve("tensor_copy", out=h1Tf[b][:, hf * 4:(hf + 1) * 4, :],
                  in_=pt[hf].v().rearrange("p (k t) -> p k t", k=4))
        if sub == 5:
            break
        pr = pm[0]
        for k in range(8):
            P.mm(pr[:, 0:NE], h1Tf[b][:, k, :], rwt[:, k, :], start=(k == 0), stop=(k == 7))
        P.act("activation", out=scores[:, tt, :], in_=pr[:, 0:NE], func=AF.Sigmoid)

    def v4(t):
        return t.v().rearrange("p t (g i) -> p t g i", g=4)
    P.dve("tensor_tensor", out=r_sel.v(), in0=scores.v(),
          in1=View(rbt, rbt.tensor[:, :].unsqueeze(1).to_broadcast([128, NT, NE])), op=ALU.add)
    P.op("dve", "memset", extra_writes=[r_cnt], ap=r_cnt.v().ap, constant=0.0)
    for j in range(4):
        selj = View(r_sel, v4(r_sel).ap[:, :, :, j:j + 1].to_broadcast([128, NT, 4, 4]))
        P.dve("tensor_tensor", out=v4(r_tmp), in0=selj, in1=v4(r_sel), op=ALU.is_gt)
        P.dve("tensor_tensor", out=r_cnt.v(), in0=r_cnt.v(), in1=r_tmp.v(), op=ALU.add)
    P.dve("tensor_single_scalar", out=r_cnt.v(), in_=r_cnt.v(), scalar=1.5, op=ALU.is_lt)
    P.dve("tensor_tensor", out=r_tmp.v(), in0=r_sel.v(), in1=r_cnt.v(), op=ALU.mult)
    P.dve("tensor_reduce", out=r_gs.v(), in_=v4(r_tmp), axis=AX.X, op=ALU.add)
    P.dve("tensor_reduce", out=r_gm.v(), in_=r_gs.v(), axis=AX.X, op=ALU.max)
    P.dve("tensor_tensor", out=r_gmask.v(), in0=r_gs.v(),
          in1=View(r_gm, r_gm.tensor[:, :].unsqueeze(2).to_broadcast([128, NT, 4])), op=ALU.is_ge)
    P.dve("tensor_tensor", out=v4(r_cnt), in0=v4(r_cnt),
          in1=View(r_gmask, r_gmask.tensor[:, :, :].unsqueeze(3).to_broadcast([128, NT, 4, 4])),
          op=ALU.mult)
    P.dve("tensor_tensor", out=r_tmp.v(), in0=scores.v(), in1=r_cnt.v(), op=ALU.mult)
    P.dve("tensor_reduce", out=r_den.v(), in_=r_tmp.v(), axis=AX.X, op=ALU.add)
    P.dve("reciprocal", out=r_den.v(), in_=r_den.v())
    P.dve("tensor_tensor", out=comb.v(), in0=r_tmp.v(),
          in1=View(r_den, r_den.tensor[:, :].unsqueeze(2).to_broadcast([128, NT, NE])), op=ALU.mult)

    P.barrier()

    it = 0
    for e in range(NE):
        wb = wbuf[e % 2]
        P.dma(wb["g"].v(), wg[e].rearrange("(k p) f -> p k f", p=128), lane="pool")
        P.dma(wb["u"].v(), wu[e].rearrange("(k p) f -> p k f", p=128), lane="pool")
        P.dma(wb["d"].v(), wd[e].rearrange("(k p) f -> p k f", p=128), lane="pool")
        for tc in range(NC4):
            hb = heT[it % 2]
            for ft in range(4):
                pg = ps[(2 * (it * 4 + ft)) % 4]
                pu = ps[(2 * (it * 4 + ft)) % 4 + 1]
                for k in range(8):
                    P.mm(pg.v(), wb["g"][:, k, ft * 128:(ft + 1) * 128],
                         h1T[:, k, tc * 512:(tc + 1) * 512], start=(k == 0), stop=(k == 7))
                for k in range(8):
                    P.mm(pu.v(), wb["u"][:, k, ft * 128:(ft + 1) * 128],
                         h1T[:, k, tc * 512:(tc + 1) * 512], start=(k == 0), stop=(k == 7))
                s_ = sg[(it * 4 + ft) % 2]
                P.act("activation", out=s_.v(), in_=pg.v(), func=AF.Silu)
                P.dve("tensor_tensor", out=hb[ft].v(), in0=s_.v(), in1=pu.v(), op=ALU.mult)
            for t4 in range(4):
                tt = tc * 4 + t4
                for half in range(2):
                    py = ps[4 + (2 * (it * 4 + t4)) % 4 + half]
                    for ft in range(4):
                        P.mm(py.v(), hb[ft][:, t4 * 128:(t4 + 1) * 128],
                             wb["d"][:, ft, half * 512:(half + 1) * 512], start=(ft == 0), stop=(ft == 3))
                    P.dve("scalar_tensor_tensor", out=acc_t[tt][:, half * 512:(half + 1) * 512],
                          in0=py.v(), scalar=comb[:, tt, e:e + 1],
                          in1=acc_t[tt][:, half * 512:(half + 1) * 512], op0=ALU.mult, op1=ALU.add)
            it += 1

    for tt in range(NT):
        b = tt % 2
        layer_norm_tile(P, acc_t[tt].v(), h1_t[b].v(), lnt[2].v(), lnt[3].v(),
                        stats[b], mv[b], rstd[b], tmp_t[b])
        P.dma(hout[tt * 128:(tt + 1) * 128, :], h1_t[b].v(), lane="sp", final=True)
        if houtT is not None:
            ptt = [ps[(2 * tt) % 4], ps[(2 * tt) % 4 + 1]]
            for k in range(8):
                P.pe("transpose", out=ptt[k // 4][:, (k % 4) * 128:(k % 4 + 1) * 128],
                     in_=h1_t[b][:, k * 128:(k + 1) * 128], identity=ident.v())
            for hf in range(2):
                P.act("copy", out=h1Tf[b][:, hf * 4:(hf + 1) * 4, :],
                      in_=ptt[hf].v().rearrange("p (k t) -> p k t", k=4))
            if isinstance(houtT, RowBlocks):
                assert houtT.bs == 256
                for j2 in range(4):
                    P.dma(houtT.views[j2][:, tt * 128:(tt + 1) * 128].rearrange("(k p) t -> p k t", p=128),
                          h1Tf[b][:, 2 * j2:2 * j2 + 2, :], lane="sp", final=True)
            else:
                P.dma(houtT[:, tt * 128:(tt + 1) * 128].rearrange("(k p) t -> p k t", p=128), h1Tf[b].v(),
                      lane="sp", final=True)


def build_T(nc, TOK=2048):
    P = Prog(nc)
    io = dict(hin=P.dram("hin", [TOK, D], F32, "ExternalInput"), xT=P.dram("xT", [D, TOK], F32, "ExternalInput"),
              wout=P.dram("wout", [D, D], F32, "ExternalInput"), rw=P.dram("rw", [D, NE], F32, "ExternalInput"),
              rb=P.dram("rb", [1, NE], F32, "ExternalInput"), wg=P.dram("wg", [NE, D, DE], F32, "ExternalInput"),
              wu=P.dram("wu", [NE, D, DE], F32, "ExternalInput"), wd=P.dram("wd", [NE, DE, D], F32, "ExternalInput"),
              hout=P.dram("hout", [TOK, D], F32, "ExternalOutput"))
    lnp = P.dram("lnp", [4, D], F32, "ExternalInput")
    io["ln"] = [lnp[i:i + 1, :] for i in range(4)]
    body_T(P, io, TOK)
    P.emit()
    P.close()
    return nc


D = 1024


def body_GM(P, io, TOK=2048):
    NT = TOK // 128
    hT, w_in, b_in, wsT, bsT = io["hT"], io["w_in"], io["b_in"], io["wsT"], io["bsT"]
    lnrows = io["ln"]

    hTb = P.sbuf([128, 8, TOK], BF16, "hTb")
    winb = P.sbuf([128, 8, 2 * D], BF16, "winb")
    bint = P.sbuf([128, 2 * D], F32, "bint")
    lnt = [P.sbuf([128, D], F32, "lnt%d" % i) for i in range(2)]
    wst = P.sbuf([128, 8, 128], F32, "wst")
    wsb = P.sbuf([128, 8, 128], BF16, "wsb")
    bst = P.sbuf([128, 8], F32, "bst")
    zt = [P.sbuf([128, 2 * D], F32, "z%d" % i) for i in range(2)]
    vn = [P.sbuf([128, D], F32, "vn%d" % i) for i in range(2)]
    vnb = [P.sbuf([128, D], BF16, "vnb%d" % i) for i in range(2)]
    yt = [P.sbuf([128, D], F32, "y%d" % i) for i in range(2)]
    stats = [P.sbuf([128, 2, 6], F32, "st%d" % i) for i in range(2)]
    mv = [P.sbuf([128, 2], F32, "mv%d" % i) for i in range(2)]
    rstd = [P.sbuf([128, 1], F32, "rstd%d" % i) for i in range(2)]
    ps = [P.psum([128, 512], F32, "ps%d" % i) for i in range(8)]
    identf = P.sbuf([128, 128], F32, "identf")
    yT = [P.sbuf([128, 8, 128], F32, "yT%d" % i) for i in range(2)]
    P.op("pool", "memset", extra_writes=[identf], ap=identf.v().ap, constant=1.0)
    P.pool("affine_select", out=identf.v(), in_=identf.v(), pattern=[[-1, 128]],
           compare_op=ALU.is_equal, fill=0.0, base=0, channel_multiplier=1)

    for k in range(8):
        for c2 in range(max(1, TOK // 2048)):
            w_ = min(TOK, 2048)
            P.dma(hTb[:, k, c2 * w_:(c2 + 1) * w_], rows_view(hT, k * 128, 128)[:, c2 * w_:(c2 + 1) * w_], lane="pool")
        P.dma(winb[:, k, 0:1024], w_in[k * 128:(k + 1) * 128, 0:1024], lane="pool")
        P.dma(winb[:, k, 1024:2048], w_in[k * 128:(k + 1) * 128, 1024:2048], lane="pool")
    P.dma(bint.v(), View(b_in, b_in.tensor[0:1, :].broadcast_to([128, 2 * D])), lane="sp")
    for i in range(2):
        P.dma(lnt[i].v(), View(lnrows[i], lnrows[i].tensor.broadcast_to([128, D])), lane="sp")
    P.dma(wst.v(), wsT.v(), lane="sp")
    P.dma(bst.v(), bsT.v(), lane="sp")
    P.pool("affine_select", out=wst.v(), in_=wst.v(), pattern=[[0, 8], [1, 128]],
           compare_op=ALU.is_ge, fill=0.0, base=0, channel_multiplier=-1)
    P.pool("tensor_copy", out=wsb.v(), in_=wst.v())

    for tt in range(NT):
        b = tt % 2
        for cb in range(4):
            pz = ps[cb]
            for k in range(8):
                P.mm(pz.v(), hTb[:, k, tt * 128:(tt + 1) * 128], winb[:, k, cb * 512:(cb + 1) * 512],
                     start=(k == 0), stop=(k == 7))
            P.dve("tensor_tensor", out=zt[b][:, cb * 512:(cb + 1) * 512], in0=pz.v(),
                  in1=bint[:, cb * 512:(cb + 1) * 512], op=ALU.add)
        P.act("activation", out=zt[b].v(), in_=zt[b].v(), func=AF.Gelu)
        layer_norm_tile(P, zt[b][:, D:2 * D], vn[b].v(), lnt[0].v(), lnt[1].v(), stats[b], mv[b], rstd[b], None)
        P.act("copy", out=vnb[b].v(), in_=vn[b].v())
        for g in range(8):
            psv = ps[4 + g // 4]
            P.mm(psv[:, (g % 4) * 128:(g % 4 + 1) * 128], wsb[:, g, :], vnb[b][:, g * 128:(g + 1) * 128])
        for g in range(8):
            psv = ps[4 + g // 4]
            P.dve("scalar_tensor_tensor", out=yt[b][:, g * 128:(g + 1) * 128],
                  in0=psv[:, (g % 4) * 128:(g % 4 + 1) * 128], scalar=bst[:, g:g + 1],
                  in1=zt[b][:, g * 128:(g + 1) * 128], op0=ALU.add, op1=ALU.mult)
        if "X" in io:
            P.dma(io["X"][tt * 128:(tt + 1) * 128, :], yt[b].v(), lane="sp", final=True)
        else:
            for k in range(8):
                P.pe("transpose", out=ps[6 + k // 4][:, (k % 4) * 128:(k % 4 + 1) * 128],
                     in_=yt[b][:, k * 128:(k + 1) * 128], identity=identf.v())
            for hf in range(2):
                P.act("copy", out=yT[b][:, hf * 4:(hf + 1) * 4, :],
                      in_=ps[6 + hf].v().rearrange("p (k t) -> p k t", k=4))
            P.dma(io["XT"][:, tt * 128:(tt + 1) * 128].rearrange("(k p) t -> p k t", p=128), yT[b].v(),
                  lane="sp", final=True)


def build_GM(nc, TOK=2048):
    P = Prog(nc)
    lnp = P.dram("lnp", [2, D], F32, "ExternalInput")
    io = dict(hT=P.dram("hT", [D, TOK], F32, "ExternalInput").v(), w_in=P.dram("w_in", [D, 2 * D], F32, "ExternalInput").v(),
              b_in=P.dram("b_in", [1, 2 * D], F32, "ExternalInput").v(), ln=[lnp[i:i + 1, :] for i in range(2)],
              wsT=P.dram("wsT", [128, 8, 128], F32, "ExternalInput").v(), bsT=P.dram("bsT", [128, 8], F32, "ExternalInput").v(),
              X=P.dram("X", [TOK, D], F32, "ExternalOutput").v())
    body_GM(P, io, TOK)
    P.emit()
    P.close()
    return nc


D = 1024
S = 4096
NB = S // 128
NQ = S // 512
NH = 8


def body_FOX(P, hT, groups):
    hTb = P.sbuf([128, 8, S], BF16, "hTb")
    wqb = P.sbuf([128, 8, 512], BF16, "wqb")
    wkb = P.sbuf([128, 8, 512], BF16, "wkb")
    wvb = P.sbuf([128, 8, 512], BF16, "wvb")
    wogb = P.sbuf([128, 8, 512], BF16, "wogb")
    wfb = P.sbuf([128, 8, NH], BF16, "wfb")
    bft = P.sbuf([128, NH], F32, "bft")
    Vall = P.sbuf([128, NB, 512], BF16, "Vall")
    QT = [P.sbuf([64, S], BF16, "QT%d" % i) for i in range(2)]
    KT = [P.sbuf([64, S], BF16, "KT%d" % i) for i in range(2)]
    OG = [P.sbuf([64, S], BF16, "OG%d" % i) for i in range(2)]
    triU = P.sbuf([128, 128], F32, "triU")
    triUb = P.sbuf([128, 128], BF16, "triUb")
    onesf = P.sbuf([128, 128], F32, "onesf")
    onesb = P.sbuf([128, 64], BF16, "onesb")
    logf = P.sbuf([128, NB, NH], F32, "logf")
    negcin = P.sbuf([128, NB, NH], F32, "negcin")
    Rb = P.sbuf([128, NB + 1, NH], F32, "Rb")
    negR = P.sbuf([128, NB + 1, NH], F32, "negR")
    biasq = [P.sbuf([128, NB], F32, "biasq%d" % i) for i in range(2)]
    Pt = [P.sbuf([128, 512], BF16, "P%d" % i) for i in range(3)]
    rden = [P.sbuf([64, 512], F32, "rden%d" % i) for i in range(2)]
    ot = [P.sbuf([64, 512], F32, "ot%d" % i) for i in range(2)]
    ps = [P.psum([128, 512], F32, "ps%d" % i) for i in range(8)]

    for k in range(8):
        for c4 in range(4):
            P.dma(hTb[:, k, c4 * 1024:(c4 + 1) * 1024], hT[k * 128:(k + 1) * 128, c4 * 1024:(c4 + 1) * 1024], lane="pool")
    P.op("pool", "memset", extra_writes=[triU], ap=triU.v().ap, constant=1.0)
    P.pool("affine_select", out=triU.v(), in_=triU.v(), pattern=[[1, 128]],
           compare_op=ALU.is_ge, fill=0.0, base=0, channel_multiplier=-1)
    P.pool("tensor_copy", out=triUb.v(), in_=triU.v())
    P.op("pool", "memset", extra_writes=[onesf], ap=onesf.v().ap, constant=1.0)
    P.op("pool", "memset", extra_writes=[onesb], ap=onesb.v().ap, constant=1.0)

    unit = 0
    sc = 0
    for grp in groups:
        wq, wk, wv, wog, wf, bf, XT = (grp[n] for n in ("wq", "wk", "wv", "wog", "wf", "bf", "XT"))
        for wsrc, wdst in ((wq, wqb), (wk, wkb), (wv, wvb), (wog, wogb)):
            P.dma(wdst.v(), wsrc.rearrange("(k p) f -> p k f", p=128), lane="pool")
        P.dma(wfb.v(), wf.rearrange("(k p) f -> p k f", p=128), lane="pool")
        P.dma(bft.v(), View(bf, bf.tensor.broadcast_to([128, NH])), lane="sp")
        pf = ps[0]
        for kb in range(NB):
            for k in range(8):
                P.mm(pf[:, kb * NH:(kb + 1) * NH], hTb[:, k, kb * 128:(kb + 1) * 128], wfb[:, k, :],
                     start=(k == 0), stop=(k == 7))
        lf2 = logf.v().rearrange("p b h -> p (b h)")
        P.dve("tensor_tensor", out=logf.v(), in0=pf[:, 0:NB * NH].rearrange("p (b h) -> p b h", h=NH),
              in1=View(bft, bft.tensor[:, :].unsqueeze(1).to_broadcast([128, NB, NH])), op=ALU.add)
        P.act("activation", out=lf2, in_=lf2, func=AF.Exp, scale=-1.0)
        P.act("activation", out=lf2, in_=lf2, func=AF.Ln, bias=1.0)
        P.dve("tensor_scalar", out=lf2, in0=lf2, scalar1=-1.0, scalar2=None, op0=ALU.mult)
        pc = ps[1]
        P.mm(pc[:, 0:NB * NH], triU.v(), lf2)
        P.dve("tensor_scalar", out=negcin.v().rearrange("p b h -> p (b h)"), in0=pc[:, 0:NB * NH],
              scalar1=-1.0, scalar2=None, op0=ALU.mult)
        pT = ps[2]
        P.mm(pT[:, 0:NB * NH], onesf.v(), lf2)
        P.op("dve", "memset", extra_writes=[Rb], ap=Rb[:, 0, :].ap, constant=0.0)
        for m in range(NB):
            P.dve("tensor_tensor", out=Rb[:, m + 1, :], in0=Rb[:, m, :], in1=pT[:, m * NH:(m + 1) * NH], op=ALU.add)
        P.dve("tensor_scalar", out=negR.v(), in0=Rb.v(), scalar1=-1.0, scalar2=None, op0=ALU.mult)

        for kb in range(NB):
            pv = ps[4 + kb % 4]
            for k in range(8):
                P.mm(pv.v(), hTb[:, k, kb * 128:(kb + 1) * 128], wvb[:, k, :], start=(k == 0), stop=(k == 7))
            if kb % 2 == 0:
                P.dve("tensor_copy", out=Vall[:, kb, :], in_=pv.v())
            else:
                P.act("copy", out=Vall[:, kb, :], in_=pv.v())

        for h in range(NH):
            hb = h % 2
            for j in range(NQ):
                for (wsrc, dst, kind) in ((wqb, QT[hb], 0), (wkb, KT[hb], 0), (wogb, OG[hb], 1)):
                    pp = ps[sc % 4]
                    sc += 1
                    for k in range(8):
                        P.mm(pp[0:64, :], wsrc[:, k, h * 64:(h + 1) * 64], hTb[:, k, j * 512:(j + 1) * 512],
                             start=(k == 0), stop=(k == 7))
                    if kind == 0:
                        P.dve("tensor_copy", out=dst[:, j * 512:(j + 1) * 512], in_=pp[0:64, :])
                    else:
                        P.act("activation", out=dst[:, j * 512:(j + 1) * 512], in_=pp[0:64, :], func=AF.Sigmoid)
            for j in range(NQ):
                ub = unit % 2
                unit += 1
                nk = 4 * j + 4
                bq = biasq[ub]
                P.dve("scalar_tensor_tensor", out=bq[:, 0:nk], in0=negR[:, 0:nk, h], scalar=Rb[:, 4 * j, h:h + 1],
                      in1=negcin[:, 0:nk, h], op0=ALU.add, op1=ALU.add)
                pnum = ps[4 + 2 * ub]
                pden = ps[5 + 2 * ub]
                pend = []
                for kb in range(nk):
                    i = kb - 4 * j
                    c0 = 128 * i if i > 0 else 0
                    pS = ps[sc % 4]
                    pt_ = Pt[sc % 3]
                    sc += 1
                    P.mm(pS[:, c0:512], KT[hb][:, kb * 128:(kb + 1) * 128], QT[hb][:, j * 512 + c0:(j + 1) * 512])
                    P.act("activation", out=pt_[:, c0:512], in_=pS[:, c0:512], func=AF.Exp,
                          scale=0.125, bias=bq[:, kb:kb + 1])
                    if i >= 0:
                        P.pool("tensor_tensor", out=pt_[:, c0:c0 + 128], in0=pt_[:, c0:c0 + 128],
                               in1=triUb.v(), op=ALU.mult)
                    def pv(kb=kb, c0=c0, pt_=pt_):
                        P.mm(pnum[0:64, c0:512], Vall[:, kb, h * 64:(h + 1) * 64], pt_[:, c0:512],
                             start=(kb == 0), stop=(kb == nk - 1))
                        P.mm(pden[0:64, c0:512], onesb.v(), pt_[:, c0:512],
                             start=(kb == 0), stop=(kb == nk - 1))
                    pend.append(pv)
                    if len(pend) > 2:
                        pend.pop(0)()
                while pend:
                    pend.pop(0)()
                P.dve("reciprocal", out=rden[ub].v(), in_=pden[0:64, :])
                P.dve("tensor_tensor", out=ot[ub].v(), in0=pnum[0:64, :], in1=rden[ub].v(), op=ALU.mult)
                P.pool("tensor_tensor", out=ot[ub].v(), in0=ot[ub].v(), in1=OG[hb][:, j * 512:(j + 1) * 512], op=ALU.mult)
                P.dma(rows_view(XT, h * 64, 64)[:, j * 512:(j + 1) * 512], ot[ub].v(), lane="sp", final=True)


def build_FOX(nc):
    P = Prog(nc)
    hT = P.dram("hT", [D, S], F32, "ExternalInput")
    grp = dict(wq=P.dram("wq", [D, 512], F32, "ExternalInput").v(), wk=P.dram("wk", [D, 512], F32, "ExternalInput").v(),
               wv=P.dram("wv", [D, 512], F32, "ExternalInput").v(), wog=P.dram("wog", [D, 512], F32, "ExternalInput").v(),
               wf=P.dram("wf", [D, NH], F32, "ExternalInput").v(), bf=P.dram("bf", [1, NH], F32, "ExternalInput").v(),
               XT=P.dram("XT", [512, S], F32, "ExternalOutput").v())
    body_FOX(P, hT.v(), [grp])
    P.emit()
    P.close()
    return nc


D = 1024
S = 4096
NB = S // 128
NQ = S // 512
NH = 8


def body_SB(P, hT, groups):
    hTb = P.sbuf([128, 8, S], BF16, "hTb")
    wqb = P.sbuf([128, 8, 512], BF16, "wqb")
    wkb = P.sbuf([128, 8, 512], BF16, "wkb")
    wvb = P.sbuf([128, 8, 512], BF16, "wvb")
    Vall = P.sbuf([128, NB, 512], BF16, "Vall")
    QT = [P.sbuf([64, S], BF16, "QT%d" % i) for i in range(2)]
    KT = [P.sbuf([64, S], BF16, "KT%d" % i) for i in range(2)]
    tmpf = P.sbuf([128, 128], F32, "tmpf")
    strictUb = P.sbuf([128, 128], BF16, "strictUb")
    negTriLb = P.sbuf([128, 128], BF16, "negTriLb")
    negones = P.sbuf([128, 128], BF16, "negones")
    zerosb = P.sbuf([128, 64], BF16, "zerosb")
    et = [P.sbuf([128, 512], F32, "et%d" % i) for i in range(2)]
    spb = [P.sbuf([128, 512], BF16, "spb%d" % i) for i in range(3)]
    At = [P.sbuf([128, 512], BF16, "At%d" % i) for i in range(3)]
    Lsum = [P.sbuf([128, 512], BF16, "Lsum%d" % i) for i in range(2)]
    ot = [P.sbuf([64, 512], F32, "ot%d" % i) for i in range(2)]
    ps = [P.psum([128, 512], F32, "ps%d" % i) for i in range(8)]

    for k in range(8):
        for c4 in range(4):
            P.dma(hTb[:, k, c4 * 1024:(c4 + 1) * 1024], hT[k * 128:(k + 1) * 128, c4 * 1024:(c4 + 1) * 1024], lane="pool")
    P.op("pool", "memset", extra_writes=[tmpf], ap=tmpf.v().ap, constant=1.0)
    P.pool("affine_select", out=tmpf.v(), in_=tmpf.v(), pattern=[[1, 128]],
           compare_op=ALU.is_gt, fill=0.0, base=0, channel_multiplier=-1)
    P.pool("tensor_copy", out=strictUb.v(), in_=tmpf.v())
    P.op("pool", "memset", extra_writes=[tmpf], ap=tmpf.v().ap, constant=-1.0)
    P.pool("affine_select", out=tmpf.v(), in_=tmpf.v(), pattern=[[-1, 128]],
           compare_op=ALU.is_ge, fill=0.0, base=0, channel_multiplier=1)
    P.pool("tensor_copy", out=negTriLb.v(), in_=tmpf.v())
    P.op("pool", "memset", extra_writes=[negones], ap=negones.v().ap, constant=-1.0)
    P.op("pool", "memset", extra_writes=[zerosb], ap=zerosb.v().ap, constant=0.0)

    unit = 0
    sc = 0
    for grp in groups:
        wq, wk, wv, XT = (grp[n] for n in ("wq", "wk", "wv", "XT"))
        for wsrc, wdst in ((wq, wqb), (wk, wkb), (wv, wvb)):
            P.dma(wdst.v(), wsrc.rearrange("(k p) f -> p k f", p=128), lane="pool")
        for kb in range(NB):
            pv = ps[4 + kb % 4]
            for k in range(8):
                P.mm(pv.v(), hTb[:, k, kb * 128:(kb + 1) * 128], wvb[:, k, :], start=(k == 0), stop=(k == 7))
            if kb % 2 == 0:
                P.dve("tensor_copy", out=Vall[:, kb, :], in_=pv.v())
            else:
                P.act("copy", out=Vall[:, kb, :], in_=pv.v())

        for h in range(NH):
            hb = h % 2
            for j in range(NQ):
                for (wsrc, dst, scl) in ((wqb, QT[hb], 0.125), (wkb, KT[hb], 1.0)):
                    pp = ps[sc % 2]
                    sc += 1
                    for k in range(8):
                        P.mm(pp[0:64, :], wsrc[:, k, h * 64:(h + 1) * 64], hTb[:, k, j * 512:(j + 1) * 512],
                             start=(k == 0), stop=(k == 7))
                    P.dve("tensor_scalar", out=dst[:, j * 512:(j + 1) * 512], in0=pp[0:64, :],
                          scalar1=scl, scalar2=None, op0=ALU.mult)
            for j in range(NQ):
                ub = unit % 2
                unit += 1
                nk = 4 * j + 4
                pnum = ps[4 + ub]
                ls = Lsum[ub]
                P.op("pool", "memset", extra_writes=[ls], ap=ls.v().ap, constant=0.0)
                P.mm(pnum[0:64, :], zerosb.v(), hTb[:, 0, 0:512], start=True, stop=False)
                steps = []
                for kb in range(nk - 1, -1, -1):
                    i = kb - 4 * j
                    c0 = 128 * i if i > 0 else 0
                    steps.append(dict(kb=kb, i=i, c0=c0, pA=ps[sc % 2], pB=ps[2 + sc % 2], e_=et[sc % 2],
                                      sp_=spb[sc % 3], a_=At[sc % 3],
                                      kT=KT[hb][:, kb * 128:(kb + 1) * 128],
                                      qT=QT[hb][:, j * 512 + c0:(j + 1) * 512]))
                    sc += 1

                def s1(st):
                    c0 = st["c0"]
                    P.mm(st["pA"][:, c0:512], st["kT"], st["qT"])
                    P.act("activation", out=st["e_"][:, c0:512], in_=st["pA"][:, c0:512], func=AF.Exp)
                    P.act("activation", out=st["sp_"][:, c0:512], in_=st["e_"][:, c0:512], func=AF.Ln, bias=1.0)
                    if st["i"] >= 0:
                        P.pool("tensor_tensor", out=st["sp_"][:, c0:c0 + 128], in0=st["sp_"][:, c0:c0 + 128],
                               in1=strictUb.v(), op=ALU.mult)

                def s2(st):
                    c0, kb = st["c0"], st["kb"]
                    first = (kb == nk - 1)
                    P.mm(st["pB"][:, c0:512], st["kT"], st["qT"], start=True, stop=False)
                    P.mm(st["pB"][:, c0:512], negTriLb.v(), st["sp_"][:, c0:512], start=False, stop=first)
                    if not first:
                        P.mm(st["pB"][:, c0:512], negones.v(), ls[:, c0:512], start=False, stop=True)
                    P.act("activation", out=st["a_"][:, c0:512], in_=st["pB"][:, c0:512], func=AF.Exp)
                    if st["i"] >= 0:
                        P.pool("tensor_tensor", out=st["a_"][:, c0:c0 + 128], in0=st["a_"][:, c0:c0 + 128],
                               in1=strictUb.v(), op=ALU.mult)
                    if kb > 0:
                        P.dve("tensor_tensor", out=ls[:, c0:512], in0=ls[:, c0:512], in1=st["sp_"][:, c0:512], op=ALU.add)

                def s3(st):
                    c0, kb = st["c0"], st["kb"]
                    P.mm(pnum[0:64, c0:512], Vall[:, kb, h * 64:(h + 1) * 64], st["a_"][:, c0:512],
                         start=False, stop=(kb == 0))

                ns = len(steps)
                s1(steps[0])
                for n in range(ns):
                    if n + 1 < ns:
                        s1(steps[n + 1])
                    s2(steps[n])
                    if n >= 1:
                        s3(steps[n - 1])
                s3(steps[ns - 1])
                P.dve("tensor_copy", out=ot[ub].v(), in_=pnum[0:64, :])
                P.dma(rows_view(XT, h * 64, 64)[:, j * 512:(j + 1) * 512], ot[ub].v(), lane="sp", final=True)


def build_SB(nc):
    P = Prog(nc)
    hT = P.dram("hT", [D, S], F32, "ExternalInput")
    grp = dict(wq=P.dram("wq", [D, 512], F32, "ExternalInput").v(), wk=P.dram("wk", [D, 512], F32, "ExternalInput").v(),
               wv=P.dram("wv", [D, 512], F32, "ExternalInput").v(), XT=P.dram("XT", [512, S], F32, "ExternalOutput").v())
    body_SB(P, hT.v(), [grp])
    P.emit()
    P.close()
    return nc


import math

D = 1024
S = 4096
NSB = S // 128
NH = 8
CNEG = -math.exp(-0.5)
GN_EPS = 64e-5


def body_RW(P, hT, muT, w1, a1, g1, groups, nsb=NSB):
    DEBUG = False
    dbg = None

    def dump(view, idx):
        pass

    def dump2(view, idx, rows, cols):
        pass

    def sb_(shape, dt, name):
        return P.sbuf(shape, dt, name)

    mut = sb_([128, 8, 6], F32, "mut")
    omut = sb_([128, 8, 6], F32, "omut")
    Wa = {}
    Wb = {}
    for nm, ncol in (("r", 512), ("k", 512), ("v", 512), ("w1", 64), ("a1", 64), ("g1", 128)):
        Wa[nm] = sb_([128, 8, ncol], BF16, "Wa_" + nm)
        Wb[nm] = sb_([128, 8, ncol], BF16, "Wb_" + nm)
    w2b = sb_([64, 512], BF16, "w2b")
    a2b = sb_([64, 512], BF16, "a2b")
    g2b = sb_([128, 512], BF16, "g2b")
    vt = [sb_([128, 512], F32, "vec%d" % i) for i in range(7)]
    W0, A0, KK_, KA_, GNG, GNB, RK = vt

    wstl = [sb_([128, 512], F32, "wsl%d" % i) for i in range(4)]
    P.dma(mut.v(), muT.v(), lane="sp")
    P.dve("tensor_scalar", out=omut.v(), in0=mut.v(), scalar1=-1.0, scalar2=1.0, op0=ALU.mult, op1=ALU.add)

    identf = sb_([128, 128], F32, "identf")
    P.op("pool", "memset", extra_writes=[identf], ap=identf.v().ap, constant=1.0)
    P.pool("affine_select", out=identf.v(), in_=identf.v(), pattern=[[-1, 128]],
           compare_op=ALU.is_equal, fill=0.0, base=0, channel_multiplier=1)
    BD = sb_([128, 128], F32, "BD")
    bd3 = BD.v().rearrange("p (c i) -> p c i", c=4)
    P.op("pool", "memset", extra_writes=[BD], ap=BD.v().ap, constant=1.0)
    P.pool("affine_select", out=bd3, in_=bd3, pattern=[[-32, 4], [0, 32]],
           compare_op=ALU.is_ge, fill=0.0, base=0, channel_multiplier=1)
    P.pool("affine_select", out=bd3, in_=bd3, pattern=[[32, 4], [0, 32]],
           compare_op=ALU.is_ge, fill=0.0, base=31, channel_multiplier=-1)
    mAB = sb_([128, 512], F32, "mAB")
    mC = sb_([128, 256], F32, "mC")
    triBD = sb_([128, 128], F32, "triBD")
    blkBD = sb_([128, 128], F32, "blkBD")
    P.pool("affine_select", out=mAB[:, 0:128], in_=BD.v(), pattern=[[1, 128]],
           compare_op=ALU.is_gt, fill=0.0, base=0, channel_multiplier=-1)
    P.pool("affine_select", out=mAB[:, 128:256], in_=BD.v(), pattern=[[1, 128]],
           compare_op=ALU.is_ge, fill=0.0, base=0, channel_multiplier=-1)
    P.pool("tensor_copy", out=mAB[:, 256:512], in_=mAB[:, 0:256])
    P.pool("affine_select", out=mC[:, 0:128], in_=BD.v(), pattern=[[-1, 128]],
           compare_op=ALU.is_gt, fill=0.0, base=0, channel_multiplier=1)
    P.pool("tensor_copy", out=mC[:, 128:256], in_=mC[:, 0:128])
    P.pool("tensor_scalar", out=triBD.v(), in0=mAB[:, 128:256], scalar1=CNEG, scalar2=None, op0=ALU.mult)
    P.pool("tensor_scalar", out=blkBD.v(), in0=BD.v(), scalar1=CNEG, scalar2=None, op0=ALU.mult)
    RMexp = sb_([128, 4, 64], F32, "RMexp")
    P.op("pool", "memset", extra_writes=[RMexp], ap=RMexp.v().ap, constant=1.0)
    P.pool("affine_select", out=RMexp.v(), in_=RMexp.v(), pattern=[[-32, 4], [0, 64]],
           compare_op=ALU.is_ge, fill=0.0, base=0, channel_multiplier=1)
    P.pool("affine_select", out=RMexp.v(), in_=RMexp.v(), pattern=[[32, 4], [0, 64]],
           compare_op=ALU.is_ge, fill=0.0, base=31, channel_multiplier=-1)
    CM = sb_([64, 4, 128], F32, "CM")
    cm4 = CM.v().rearrange("p c (d i) -> p c d i", d=4)
    P.op("pool", "memset", extra_writes=[CM], ap=CM.v().ap, constant=1.0)
    P.pool("affine_select", out=cm4, in_=cm4, pattern=[[1, 4], [-1, 4], [0, 32]],
           compare_op=ALU.is_equal, fill=0.0, base=0, channel_multiplier=0)
    Sel = sb_([128, 4], F32, "Sel")
    P.op("pool", "memset", extra_writes=[Sel], ap=Sel.v().ap, constant=1.0)
    P.pool("affine_select", out=Sel.v(), in_=Sel.v(), pattern=[[-32, 4]],
           compare_op=ALU.is_equal, fill=0.0, base=0, channel_multiplier=1)

    hg = [sb_([128, 8, 513], BF16, "hg%d" % i) for i in range(1)]
    th = [sb_([64, 512], BF16, "th%d" % i) for i in range(1)]
    xa = [sb_([64, 512], BF16, "xa%d" % i) for i in range(1)]
    sgT = [sb_([128, 512], BF16, "sgT%d" % i) for i in range(1)]

    def f32t(name, n=1):
        return [sb_([128, 512], F32, "%s%d" % (name, i)) for i in range(n)]
    r_t = f32t("r_t", 1)
    k_t = f32t("k_t", 1)
    V_t = f32t("V_t", 1)
    sgd = f32t("sgd", 1)
    lwc = f32t("lwc", 1)
    tmpA = f32t("tmpA", 1)
    tmpB = f32t("tmpB", 1)
    a_t = f32t("a_t", 1)
    kk_t = f32t("kk_t", 1)
    kp_t = f32t("kp_t", 1)
    ka_t = a_t
    E2 = f32t("E2", 1)
    E4 = sgd
    At = [wstl[0]]
    Bt = [wstl[1]]
    Kt = [wstl[2]]
    Rt = [wstl[3]]
    Bh = f32t("Bh", 1)
    Kh = f32t("Kh", 1)
    E5 = f32t("E5", 1)
    g_t = f32t("g_t", 1)
    bon = f32t("bon", 1)
    Vm = [sb_([128, 8, 4, 64], F32, "Vm%d" % i) for i in range(1)]
    ss8 = sb_([128, 8], F32, "ss8")
    rk8 = sb_([128, 8], F32, "rk8")
    TT = [sb_([64, 512], F32, "TT%d" % i) for i in range(2)]
    PP = [[sb_([128, 256], F32, "PP%d_%d" % (i, j)) for j in range(2)] for i in range(2)]
    XX = [[sb_([128, 192], F32, "XX%d_%d" % (i, j)) for j in range(2)] for i in range(2)]
    KrKh = [sb_([128, 192], F32, "KrKh%d" % i) for i in range(2)]
    AkaT = [sb_([128, 128], F32, "AkaT%d" % i) for i in range(2)]
    G1 = [sb_([64, 128], F32, "G1_%d" % i) for i in range(2)]
    G1pad = [[sb_([64, 4, 128], F32, "G1pad%d_%d" % (i, h)) for h in range(NH)] for i in range(1)]
    G2H2 = [sb_([128, 192], F32, "G2H2_%d" % i) for i in range(2)]
    X2m = [sb_([128, 4, 64], F32, "X2m%d" % i) for i in range(2)]
    WCT = [sb_([64, 4], F32, "WCT%d" % i) for i in range(2)]
    H1 = [[sb_([64, 4, 64], F32, "H1_%d_%d" % (i, h)) for h in range(NH)] for i in range(1)]
    SvT = [sb_([64, 4, NH, 64], F32, "SvT%d" % i) for i in range(1)]
    ST = [sb_([64, NH, 64], F32, "ST%d" % i) for i in range(2)]
    y_t = f32t("y_t", 1)
    s1 = sb_([128, 8], F32, "s1")
    s2 = sb_([128, 8], F32, "s2")
    o_t = f32t("o_t", 1)
    ysq = o_t

    psG = [P.psum([128, 512], F32, "psG%d" % i) for i in range(2)]
    psY = [P.psum([128, 512], F32, "psY%d" % i) for i in range(1)]
    psS = P.psum([128, 512], F32, "psS")
    B0, B1, B2, B3 = [P.psum([128, 512], F32, "B%d" % i) for i in range(4)]


    def bc8(tile8):
        return View(tile8, tile8.tensor[:, :].unsqueeze(2).to_broadcast([128, 8, 64]))

    def v3(view):
        return view.rearrange("p (h j) -> p h j", h=8)

    gi = 0
    for grp in groups:
        wr, wk, wv, w2, a2, g2, vecs = (grp[n] for n in ("wr", "wk", "wv", "w2", "a2", "g2", "vecs"))
        for ci, (nm, src, ncol) in enumerate((("r", wr, 512), ("k", wk, 512), ("v", wv, 512),
                                              ("w1", w1, 64), ("a1", a1, 64), ("g1", g1, 128))):
            for k in range(8):
                wk_ = wstl[(ci * 8 + k) % 4]
                P.dma(wk_[:, 0:ncol], src[k * 128:(k + 1) * 128, :], lane="sp")
                P.dve("tensor_scalar", out=Wb[nm][:, k, :], in0=wk_[:, 0:ncol], scalar1=mut[:, k, ci:ci + 1],
                      scalar2=None, op0=ALU.mult)
                P.pool("tensor_scalar", out=Wa[nm][:, k, :], in0=wk_[:, 0:ncol], scalar1=omut[:, k, ci:ci + 1],
                       scalar2=None, op0=ALU.mult)
        P.dma(w2b.v(), w2, lane="pool")
        P.dma(a2b.v(), a2, lane="pool")
        P.dma(g2b.v(), g2, lane="pool")
        for i in range(7):
            P.dma(vt[i].v(), View(vecs[i], vecs[i].tensor.broadcast_to([128, 512])), lane="sp")
        P.op("pool", "memset", extra_writes=[ST[0]], ap=ST[0].v().ap, constant=0.0)
        st_cur = 0
        for sb in range(nsb):
            q = sb % 4
            g = sb // 4
            gb = 0
            pb = 0
            yb = 0
            t0 = sb * 128
            if q == 0:
                hgt = hg[gb]
                for k in range(8):
                    if g == 0:
                        P.op("pool", "memset", extra_writes=[hgt], ap=hgt[:, k, 0:1].ap, constant=0.0)
                        P.dma(hgt[:, k, 1:513], hT[k * 128:(k + 1) * 128, 0:512], lane="pool")
                    else:
                        P.dma(hgt[:, k, 0:513], hT[k * 128:(k + 1) * 128, g * 512 - 1:(g + 1) * 512], lane="pool")
                for nm, dst, fn, rows in (("w1", th[gb], AF.Tanh, 64), ("a1", xa[gb], None, 64), ("g1", sgT[gb], AF.Sigmoid, 128)):
                    pp = psG[gi % 2]
                    gi += 1
                    for k in range(8):
                        P.mm(pp[0:rows, :], Wa[nm][:, k, :], hgt[:, k, 1:513], start=(k == 0), stop=False)
                        P.mm(pp[0:rows, :], Wb[nm][:, k, :], hgt[:, k, 0:512], start=False, stop=(k == 7))
                    if fn is None:
                        P.dve("tensor_copy", out=dst.v(), in_=pp[0:rows, :])
                    else:
                        P.act("activation", out=dst.v(), in_=pp[0:rows, :], func=fn)
            hgt = hg[gb]
            cur = lambda k: hgt[:, k, 1 + q * 128:1 + (q + 1) * 128]
            prv = lambda k: hgt[:, k, q * 128:(q + 1) * 128]

            def proj(nm):
                nonlocal gi
                pp = psG[gi % 2]
                gi += 1
                for k in range(8):
                    P.mm(pp.v(), cur(k), Wa[nm][:, k, :], start=(k == 0), stop=False)
                    P.mm(pp.v(), prv(k), Wb[nm][:, k, :], start=False, stop=(k == 7))
                return pp

            def nextps():
                nonlocal gi
                pp = psG[gi % 2]
                gi += 1
                return pp
            pp = proj("r")
            P.act("copy", out=r_t[pb].v(), in_=pp.v())
            pp = proj("k")
            P.act("copy", out=k_t[0].v(), in_=pp.v())
            pp = proj("v")
            P.act("copy", out=V_t[pb].v(), in_=pp.v())
            if sb == 0:
                dump(r_t[0].v(), 0); dump(k_t[0].v(), 1); dump(V_t[0].v(), 2)
            pp = nextps()
            P.mm(pp.v(), th[gb][:, q * 128:(q + 1) * 128], w2b.v())
            P.dve("tensor_tensor", out=tmpA[0].v(), in0=pp.v(), in1=W0.v(), op=ALU.add)
            P.act("activation", out=sgd[0].v(), in_=tmpA[0].v(), func=AF.Sigmoid)
            if sb == 0:
                dump(sgd[0].v(), 3)
            pp = nextps()
            P.mm(pp.v(), xa[gb][:, q * 128:(q + 1) * 128], a2b.v())
            P.dve("tensor_tensor", out=tmpA[0].v(), in0=pp.v(), in1=A0.v(), op=ALU.add)
            P.act("activation", out=a_t[0].v(), in_=tmpA[0].v(), func=AF.Sigmoid)
            pp = nextps()
            P.mm(pp.v(), sgT[gb][:, q * 128:(q + 1) * 128], g2b.v())
            P.act("copy", out=g_t[pb].v(), in_=pp.v())
            pl = nextps()
            P.mm(pl.v(), triBD.v(), sgd[0].v())
            P.act("copy", out=lwc[0].v(), in_=pl.v())
            if sb == 0:
                dump(lwc[0].v(), 4); dump(a_t[0].v(), 5); dump(g_t[0].v(), 6)
            pe_ = nextps()
            P.mm(pe_.v(), blkBD.v(), sgd[0].v())
            P.act("activation", out=E5[pb].v(), in_=pe_.v(), func=AF.Exp)
            P.dve("tensor_tensor", out=tmpB[0].v(), in0=pe_.v(), in1=lwc[0].v(), op=ALU.subtract)
            P.dve("scalar_tensor_tensor", out=tmpA[0].v(), in0=sgd[0].v(), scalar=-CNEG, in1=lwc[0].v(),
                  op0=ALU.mult, op1=ALU.add)
            P.act("activation", out=tmpA[0].v(), in_=tmpA[0].v(), func=AF.Exp)
            P.act("activation", out=E4[0].v(), in_=tmpB[0].v(), func=AF.Exp)
            P.act("activation", out=E2[0].v(), in_=lwc[0].v(), func=AF.Exp, scale=-1.0)
            P.act("activation", out=lwc[0].v(), in_=lwc[0].v(), func=AF.Exp)
            P.pool("tensor_tensor", out=kk_t[0].v(), in0=k_t[0].v(), in1=KK_.v(), op=ALU.mult)
            P.pool("tensor_tensor", out=tmpB[0].v(), in0=kk_t[0].v(), in1=kk_t[0].v(), op=ALU.mult)
            P.dve("tensor_reduce", out=ss8.v(), in_=v3(tmpB[0].v()), axis=AX.X, op=ALU.add)
            P.act("activation", out=ss8.v(), in_=ss8.v(), func=AF.Sqrt)
            P.dve("tensor_scalar", out=ss8.v(), in0=ss8.v(), scalar1=1e-12, scalar2=None, op0=ALU.max)
            P.dve("reciprocal", out=ss8.v(), in_=ss8.v())
            P.dve("tensor_tensor", out=v3(kk_t[0].v()), in0=v3(kk_t[0].v()), in1=bc8(ss8), op=ALU.mult)
            P.dve("scalar_tensor_tensor", out=kp_t[0].v(), in0=a_t[0].v(), scalar=-1.0, in1=KA_.v(),
                   op0=ALU.add, op1=ALU.mult)
            P.dve("scalar_tensor_tensor", out=kp_t[0].v(), in0=kp_t[0].v(), scalar=1.0, in1=k_t[0].v(),
                   op0=ALU.add, op1=ALU.mult)
            P.pool("tensor_tensor", out=ka_t[0].v(), in0=kk_t[0].v(), in1=a_t[0].v(), op=ALU.mult)
            P.dve("scalar_tensor_tensor", out=At[pb].v(), in0=kk_t[0].v(), scalar=-1.0, in1=tmpA[0].v(),
                  op0=ALU.mult, op1=ALU.mult)
            P.pool("tensor_tensor", out=Bt[pb].v(), in0=ka_t[0].v(), in1=E2[0].v(), op=ALU.mult)
            P.dve("tensor_tensor", out=Kt[pb].v(), in0=kp_t[0].v(), in1=E2[0].v(), op=ALU.mult)
            P.pool("tensor_tensor", out=Rt[pb].v(), in0=r_t[pb].v(), in1=lwc[0].v(), op=ALU.mult)
            P.dve("tensor_tensor", out=Bh[pb].v(), in0=ka_t[0].v(), in1=E4[0].v(), op=ALU.mult)
            P.pool("tensor_tensor", out=Kh[pb].v(), in0=kp_t[0].v(), in1=E4[0].v(), op=ALU.mult)
            if sb == 0:
                dump(At[0].v(), 7); dump(Bt[0].v(), 8); dump(Kt[0].v(), 9); dump(Rt[0].v(), 10)
                dump(Bh[0].v(), 11); dump(Kh[0].v(), 12); dump(E5[0].v(), 13); dump(kk_t[0].v(), 14); dump(kp_t[0].v(), 15)
            for c in range(4):
                P.pool("tensor_scalar", out=Vm[pb][:, :, c, :], in0=v3(V_t[pb].v()), scalar1=RMexp[:, c, 0:1],
                       scalar2=None, op0=ALU.mult)
            P.dve("tensor_tensor", out=tmpB[0].v(), in0=r_t[pb].v(), in1=kp_t[0].v(), op=ALU.mult)
            P.dve("tensor_tensor", out=tmpB[0].v(), in0=tmpB[0].v(), in1=RK.v(), op=ALU.mult)
            P.dve("tensor_reduce", out=rk8.v(), in_=v3(tmpB[0].v()), axis=AX.X, op=ALU.add)
            P.dve("tensor_tensor", out=v3(bon[pb].v()), in0=v3(V_t[pb].v()), in1=bc8(rk8), op=ALU.mult)

            pY = psY[yb]
            def head_gen(h, BX, BY):
                u = h % 2
                hs = slice(h * 64, (h + 1) * 64)
                P.pe("transpose", out=BX[0:64, 0:128], in_=At[pb][:, hs], identity=identf.v())
                P.pe("transpose", out=BX[0:64, 128:256], in_=Rt[pb][:, hs], identity=identf.v())
                P.pe("transpose", out=BX[0:64, 256:384], in_=Bt[pb][:, hs], identity=identf.v())
                P.pe("transpose", out=BX[0:64, 384:512], in_=Kt[pb][:, hs], identity=identf.v())
                P.act("copy", out=TT[u][:, 0:256], in_=BX[0:64, 0:256])
                P.dve("tensor_copy", out=TT[u][:, 256:512], in_=BX[0:64, 256:512])
                yield
                P.mm(BY[:, 0:256], TT[u][:, 256:384], TT[u][:, 0:256])
                P.mm(BY[:, 256:512], TT[u][:, 384:512], TT[u][:, 0:256])
                P.mm(BX[:, 0:256], TT[u][:, 0:128], TT[u][:, 256:512])
                P.mm(BX[0:64, 256:260], E5[pb][:, hs], Sel.v())
                pk = PP[u][0]
                xx = XX[u][0]
                P.dve("tensor_tensor", out=pk[:, 0:128], in0=BY[:, 0:128], in1=mAB[:, 0:128], op=ALU.mult)
                P.dve("tensor_tensor", out=xx[:, 0:128], in0=BY[:, 128:256], in1=mAB[:, 128:256], op=ALU.mult)
                P.dve("tensor_tensor", out=KrKh[u][:, 0:128], in0=BY[:, 384:512], in1=mAB[:, 128:256], op=ALU.mult)
                P.act("copy", out=WCT[u].v(), in_=BX[0:64, 256:260])
                P.dve("tensor_tensor", out=pk[:, 128:256], in0=BX[:, 0:128], in1=mC[:, 0:128], op=ALU.mult)
                P.dve("tensor_tensor", out=AkaT[u].v(), in0=BX[:, 128:256], in1=mC[:, 128:256], op=ALU.mult)
                P.pool("tensor_copy", out=xx[:, 128:192], in_=Bh[pb][:, hs])
                P.pool("tensor_copy", out=KrKh[u][:, 128:192], in_=Kh[pb][:, hs])
                yield
                cp = 0
                for lev in range(5):
                    pk = PP[u][cp]
                    xx = XX[u][cp]
                    xn = XX[u][1 - cp]
                    P.mm(BX[:, 0:192], pk[:, 128:256], xx.v())
                    if lev < 4:
                        pn = PP[u][1 - cp]
                        P.mm(BY[:, 0:128], pk[:, 128:256], pk[:, 0:128])
                        P.mm(BY[:, 128:256], pk[:, 0:128], pk[:, 128:256])
                    P.dve("tensor_tensor", out=xn.v(), in0=BX[:, 0:192], in1=xx.v(), op=ALU.add)
                    if lev < 4:
                        P.act("copy", out=pn.v(), in_=BY[:, 0:256])
                    cp = 1 - cp
                    yield
                xf = XX[u][cp]
                P.mm(BX[0:64, 256:384], At[pb][:, hs], xf[:, 0:128])
                P.mm(BY[:, 0:192], AkaT[u].v(), xf.v())
                P.pool("tensor_tensor", out=X2m[u].v(),
                       in0=View(xf, xf.tensor[:, 128:192].unsqueeze(1).to_broadcast([128, 4, 64])),
                       in1=RMexp.v(), op=ALU.mult)
                P.dve("tensor_tensor", out=G1[u].v(), in0=BX[0:64, 256:384], in1=TT[u][:, 128:256], op=ALU.add)
                P.dve("tensor_tensor", out=G2H2[u].v(), in0=BY[:, 0:192], in1=KrKh[u].v(), op=ALU.add)
                P.pool("tensor_tensor", out=G1pad[pb][h].v(),
                       in0=View(G1[u], G1[u].tensor[:, :].unsqueeze(1).to_broadcast([64, 4, 128])),
                       in1=CM.v(), op=ALU.mult)
                yield
                P.mm(BX[0:64, 0:256], At[pb][:, hs], X2m[u].v().rearrange("p c j -> p (c j)"))
                P.mm(pY[:, hs], G2H2[u][:, 0:128], V_t[pb][:, hs], start=(h == 0), stop=False)
                P.mm(BY[0:64, 256:512], G2H2[u][:, 128:192], Vm[pb][:, h, :, :].rearrange("p c i -> p (c i)"))
                for c in range(4):
                    P.dve("scalar_tensor_tensor", out=H1[pb][h][:, c, :], in0=identf[0:64, 0:64],
                          scalar=WCT[u][:, c:c + 1], in1=BX[0:64, c * 64:(c + 1) * 64],
                          op0=ALU.mult, op1=ALU.add)
                P.act("copy", out=SvT[pb][:, :, h, :], in_=BY[0:64, 256:512].rearrange("p (c i) -> p c i", c=4))

            banks = [(B0, B1), (B2, B3)]
            active = [head_gen(0, *banks[0]), head_gen(1, *banks[1])]
            next_h = 2
            while any(g is not None for g in active):
                for slot in range(2):
                    g = active[slot]
                    if g is None:
                        continue
                    try:
                        next(g)
                    except StopIteration:
                        if next_h < NH:
                            assert next_h % 2 == slot
                            active[slot] = head_gen(next_h, *banks[slot])
                            next_h += 1
                            next(active[slot])
                        else:
                            active[slot] = None

            for c in range(4):
                stc = ST[st_cur]
                stn = ST[1 - st_cur]
                for h in range(NH):
                    hs = slice(h * 64, (h + 1) * 64)
                    P.mm(pY[:, hs], G1pad[pb][h][:, c, :], stc[:, h, :], start=False, stop=(c == 3))
                    P.mm(psS[0:64, hs], H1[pb][h][:, c, :], stc[:, h, :])
                P.dve("tensor_tensor", out=stn.v().rearrange("p h i -> p (h i)"), in0=psS[0:64, :],
                      in1=SvT[pb][:, c, :, :].rearrange("p h i -> p (h i)"), op=ALU.add)
                st_cur = 1 - st_cur

            P.act("copy", out=y_t[0].v(), in_=pY.v())
            if sb == 0:
                dump(y_t[0].v(), 16); dump(bon[0].v(), 17)
                dump(ST[st_cur].v().rearrange("p h i -> p (h i)"), 18) if False else None
            P.dve("tensor_reduce", out=s1.v(), in_=v3(y_t[0].v()), axis=AX.X, op=ALU.add)
            P.pool("tensor_tensor", out=ysq[0].v(), in0=y_t[0].v(), in1=y_t[0].v(), op=ALU.mult)
            P.dve("tensor_reduce", out=s2.v(), in_=v3(ysq[0].v()), axis=AX.X, op=ALU.add)
            P.dve("tensor_scalar", out=s1.v(), in0=s1.v(), scalar1=1.0 / 64, scalar2=None, op0=ALU.mult)
            P.dve("tensor_scalar", out=s2.v(), in0=s2.v(), scalar1=1.0 / 64, scalar2=GN_EPS, op0=ALU.mult, op1=ALU.add)
            P.dve("tensor_tensor", out=rk8.v(), in0=s1.v(), in1=s1.v(), op=ALU.mult)
            P.dve("tensor_tensor", out=s2.v(), in0=s2.v(), in1=rk8.v(), op=ALU.subtract)
            P.act("activation", out=s2.v(), in_=s2.v(), func=AF.Sqrt)
            P.dve("reciprocal", out=s2.v(), in_=s2.v())
            P.dve("tensor_tensor", out=v3(y_t[0].v()), in0=v3(y_t[0].v()), in1=bc8(s1), op=ALU.subtract)
            P.dve("tensor_tensor", out=v3(y_t[0].v()), in0=v3(y_t[0].v()), in1=bc8(s2), op=ALU.mult)
            P.pool("tensor_tensor", out=y_t[0].v(), in0=y_t[0].v(), in1=GNG.v(), op=ALU.mult)
            P.pool("tensor_tensor", out=y_t[0].v(), in0=y_t[0].v(), in1=GNB.v(), op=ALU.add)
            P.pool("tensor_tensor", out=y_t[0].v(), in0=y_t[0].v(), in1=bon[pb].v(), op=ALU.add)
            P.pool("tensor_tensor", out=o_t[pb].v(), in0=y_t[0].v(), in1=g_t[pb].v(), op=ALU.mult)
            if "X" in grp:
                P.dma(grp["X"][t0:t0 + 128, :], o_t[pb].v(), lane="sp", final=True)
            else:
                pto = psG[gi % 2]
                gi += 1
                for k4 in range(4):
                    P.pe("transpose", out=pto[:, k4 * 128:(k4 + 1) * 128], in_=o_t[pb][:, k4 * 128:(k4 + 1) * 128],
                         identity=identf.v())
                P.act("copy", out=y_t[0].v(), in_=pto.v())
                for k4 in range(4):
                    P.dma(rows_view(grp["XT"], k4 * 128, 128)[:, t0:t0 + 128], y_t[0][:, k4 * 128:(k4 + 1) * 128],
                          lane="sp", final=True)


def build_RW(nc, nsb=NSB):
    P = Prog(nc)
    hT = P.dram("hT", [D, S], F32, "ExternalInput")
    muT = P.dram("muT", [128, 8, 6], F32, "ExternalInput")
    wr = P.dram("wr", [D, 512], F32, "ExternalInput")
    wk = P.dram("wk", [D, 512], F32, "ExternalInput")
    wv = P.dram("wv", [D, 512], F32, "ExternalInput")
    w1 = P.dram("w1", [D, 64], F32, "ExternalInput")
    a1 = P.dram("a1", [D, 64], F32, "ExternalInput")
    g1 = P.dram("g1", [D, 128], F32, "ExternalInput")
    w2 = P.dram("w2", [64, 512], F32, "ExternalInput")
    a2 = P.dram("a2", [64, 512], F32, "ExternalInput")
    g2 = P.dram("g2", [128, 512], F32, "ExternalInput")
    vecs = P.dram("vecs", [7, 512], F32, "ExternalInput")
    X = P.dram("X", [S, 512], F32, "ExternalOutput")
    grp = dict(wr=wr.v(), wk=wk.v(), wv=wv.v(), w2=w2.v(), a2=a2.v(), g2=g2.v(),
               vecs=[vecs[i:i + 1, :] for i in range(7)], X=X.v())
    body_RW(P, hT.v(), muT.v(), w1.v(), a1.v(), g1.v(), [grp], nsb)
    P.emit()
    P.close()
    return nc


_S, _D, _H = 4096, 1024, 2048
_PAIRS = [[0, 1], [2, 3], [4, 5], [6, 7]]

_IN2 = dict(
    x_half=[_H, _D], x_T=[_D, _S], sel=[128, 2],
    ln1_g=[4, _D], ln1_b=[4, _D], ln2_g=[4, _D], ln2_b=[4, _D],
    fox_wq=[_D, 512], fox_wk=[_D, 512], fox_wv=[_D, 512], fox_wog=[_D, 512], fox_wf=[_D, 8], fox_bf=[1, 8],
    fox_w_out=[_D, _D],
    gm_w_in=[_D, 2048], gm_b_in=[1, 2048], gm_ln_g=[1, _D], gm_ln_b=[1, _D],
    gm_wsT=[128, 8, 128], gm_bsT=[128, 8], gm_w_out=[_D, _D],
    sb_wq=[_D, 512], sb_wk=[_D, 512], sb_wv=[_D, 512], sb_w_out=[_D, _D],
    rw_muT=[128, 8, 6], rw_wr=[_D, 512], rw_wk=[_D, 512], rw_wv=[_D, 512], rw_w1=[_D, 64], rw_a1=[_D, 64],
    rw_g1=[_D, 128], rw_w2=[64, 512], rw_a2=[64, 512], rw_g2=[128, 512], rw_vecs=[7, 512], rw_w_out=[_D, _D],
    router_w=[_D, 16], router_b=[1, 16],
    moe_w_gate=[4, 16, _D, 512], moe_w_up=[4, 16, _D, 512], moe_w_down=[4, 16, 512, _D],
)


def build_fused2(nc, layers=(0, 1, 2, 3)):
    P = Prog(nc)
    I = {k: P.dram(k, shp, F32, "ExternalInput").v() for k, shp in _IN2.items()}
    out = P.dram("out", [_H, _D], F32, "ExternalOutput").v()
    xgi = [P.dram("xgi%d" % j, [128, _S], F32, "Internal").v() for j in range(4)]
    xgo = [P.dram("xgo%d" % j, [256, _S], F32, "Internal").v() for j in range(4)]
    xg_in = RowBlocks(xgi, 128)
    xblocks = [xgo[k % 4][(k // 4) * 128:(k // 4 + 1) * 128, :] for k in range(8)]
    h_d = P.dram("h_d", [_H, _D], F32, "Internal").v()
    hTo = [P.dram("hTo%d" % j, [256, _H], F32, "Internal").v() for j in range(4)]
    hTg = [P.dram("hTg%d" % j, [512, _H], F32, "Internal").v() for j in range(4)]
    hT_own = RowBlocks(hTo, 256)
    xT_own = P.dram("xT_own", [_D, _H], F32, "Internal").v()
    hT_full = P.dram("hT_full", [_D, _S], F32, "Internal").v()

    def gather_x():
        P.barrier()
        for j in range(4):
            P.all_gather(xgo[j], xgi[j], _PAIRS)

    def gather_h():
        P.barrier()
        for j in range(4):
            P.all_gather(hTg[j], hTo[j], _PAIRS)
        for j in range(4):
            for r in range(2):
                P.dma(hT_full[j * 256:(j + 1) * 256, r * _H:(r + 1) * _H], hTg[j][r * 256:(r + 1) * 256, :], lane="sp")

    def t_stage(L, wout, first, last, blend):
        io = dict(hin=(I["x_half"] if first else h_d), wout=wout,
                  ln=[I["ln1_g"][L:L + 1, :], I["ln1_b"][L:L + 1, :], I["ln2_g"][L:L + 1, :], I["ln2_b"][L:L + 1, :]],
                  rw=I["router_w"], rb=I["router_b"], wg=I["moe_w_gate"][L], wu=I["moe_w_up"][L],
                  wd=I["moe_w_down"][L], hout=(out if last else h_d))
        if blend:
            io["xT_blend"] = (xblocks, I["sel"])
        else:
            io["xT"] = xT_own
        if not last:
            io["houtT"] = hT_own
        P.begin_stage()
        body_T(P, io, _H)
        if (not last) and L != 0:
            gather_h()
        P.end_stage(last=last)

    nl = len(layers)
    for li, L in enumerate(layers):
        first, last = (li == 0), (li == nl - 1)
        hT = I["x_T"] if first else hT_full
        P.begin_stage()
        if L == 0:
            grp = dict(wq=I["fox_wq"], wk=I["fox_wk"], wv=I["fox_wv"], wog=I["fox_wog"], wf=I["fox_wf"],
                       bf=I["fox_bf"], XT=xg_in)
            body_FOX(P, hT, [grp])
            gather_x()
            wout = I["fox_w_out"]
        elif L == 1:
            body_GM(P, dict(hT=(hT_own if not first else None), w_in=I["gm_w_in"], b_in=I["gm_b_in"],
                            ln=[I["gm_ln_g"], I["gm_ln_b"]], wsT=I["gm_wsT"], bsT=I["gm_bsT"], XT=xT_own), _H)
            wout = I["gm_w_out"]
        elif L == 2:
            body_SB(P, hT, [dict(wq=I["sb_wq"], wk=I["sb_wk"], wv=I["sb_wv"], XT=xg_in)])
            gather_x()
            wout = I["sb_w_out"]
        else:
            grp = dict(wr=I["rw_wr"], wk=I["rw_wk"], wv=I["rw_wv"], w2=I["rw_w2"], a2=I["rw_a2"], g2=I["rw_g2"],
                       vecs=[I["rw_vecs"][i:i + 1, :] for i in range(7)], XT=xg_in)
            body_RW(P, hT, I["rw_muT"], I["rw_w1"], I["rw_a1"], I["rw_g1"], [grp])
            gather_x()
            wout = I["rw_w_out"]
        P.end_stage()
        t_stage(L, wout, first, last, blend=(L != 1))
    P.close()
    return nc


def fused2_inputs(inp, c):
    b, r = c // 2, c % 2
    f = lambda a: np.ascontiguousarray(a, dtype=np.float32)
    cs = slice(r * 512, (r + 1) * 512)
    m = dict(x_half=f(inp["x"][b][r * _H:(r + 1) * _H]), x_T=f(inp["x"][b].T))
    sel = np.zeros((128, 2), np.float32)
    sel[:, r] = 1.0
    m["sel"] = sel
    for k in ("ln1_g", "ln1_b", "ln2_g", "ln2_b", "router_w", "moe_w_gate", "moe_w_up", "moe_w_down"):
        m[k] = f(inp[k])
    m["router_b"] = f(inp["router_b"]).reshape(1, 16)
    W = inp["fox_w_in"][0]
    m["fox_wq"] = f(W[:, 0:1024][:, cs]); m["fox_wk"] = f(W[:, 1024:2048][:, cs]); m["fox_wv"] = f(W[:, 2048:3072][:, cs])
    m["fox_wog"] = f(W[:, 3088:4112][:, cs]); m["fox_wf"] = f(W[:, 3072 + r * 8:3072 + (r + 1) * 8])
    m["fox_bf"] = f(inp["fox_b_f"][0][r * 8:(r + 1) * 8]).reshape(1, 8)
    for k in ("fox_w_out", "gm_w_in", "gm_w_out", "sb_w_out", "rw_w1", "rw_a1", "rw_g1", "rw_w_out"):
        m[k] = f(inp[k][0])
    m["gm_b_in"] = f(inp["gm_b_in"][0]).reshape(1, -1)
    m["gm_ln_g"] = f(inp["gm_ln_g"][0]).reshape(1, -1); m["gm_ln_b"] = f(inp["gm_ln_b"][0]).reshape(1, -1)
    m["gm_wsT"] = f(inp["gm_w_s"][0].transpose(2, 0, 1)); m["gm_bsT"] = f(inp["gm_b_s"][0].T)
    W = inp["sb_w_in"][0]
    m["sb_wq"] = f(W[:, 0:1024][:, cs]); m["sb_wk"] = f(W[:, 1024:2048][:, cs]); m["sb_wv"] = f(W[:, 2048:3072][:, cs])
    m["rw_muT"] = f(inp["rw_mu"][0].T.reshape(8, 128, 6).transpose(1, 0, 2))
    W = inp["rw_w_rkv"][0]
    m["rw_wr"] = f(W[0][:, cs]); m["rw_wk"] = f(W[1][:, cs]); m["rw_wv"] = f(W[2][:, cs])
    m["rw_w2"] = f(inp["rw_w2"][0][:, cs]); m["rw_a2"] = f(inp["rw_a2"][0][:, cs]); m["rw_g2"] = f(inp["rw_g2"][0][:, cs])
    m["rw_vecs"] = f(np.stack([inp["rw_w0"][0][cs], inp["rw_a0"][0][cs], inp["rw_k_k"][0][cs], inp["rw_k_a"][0][cs],
                               inp["rw_gn_g"][0][cs], inp["rw_gn_b"][0][cs], inp["rw_r_k"][0].reshape(-1)[cs]]))
    return m


from concourse.bass_utils import run_bass_kernel_spmd


def kernel(**inp):
    inp = {k: np.asarray(v) for k, v in inp.items()}
    nc = bass.Bass("TRN2", target_bir_lowering=False, num_devices=8)
    build_fused2(nc)
    maps = [fused2_inputs(inp, c) for c in range(8)]
    res = run_bass_kernel_spmd(nc, maps, core_ids=list(range(8))).results
    out = np.stack([np.concatenate([res[2 * b]["out"], res[2 * b + 1]["out"]], 0) for b in range(4)], 0)
    return out.astype(np.float32)
```

```python
import numpy as np
import concourse.bass as bass
import concourse.mybir as mybir
from contextlib import ExitStack

F32 = mybir.dt.float32
BF16 = mybir.dt.bfloat16
AF = mybir.ActivationFunctionType
ALU = mybir.AluOpType
AX = mybir.AxisListType

SAME_SYNC_DEFAULT = True
ENGS = ["pe", "dve", "act", "pool", "sp"]
_WRITE_KEYS = ("out", "accum_out", "out_max", "out_indices")


class Tile:
    def __init__(self, tensor, name):
        self.tensor = tensor
        self.name = name
        self.is_psum = False
        self.last_w = None
        self.readers = {}

    def __getitem__(self, idx):
        return View(self, self.tensor[idx])

    def v(self):
        return View(self, self.tensor[:])


class View:
    def __init__(self, tile, ap):
        if isinstance(tile, View):
            tile = tile.tile
        self.tile = tile
        self.ap = ap

    @property
    def tensor(self):
        return self.ap

    def v(self):
        return self

    def __getitem__(self, idx):
        return View(self.tile, self.ap[idx])

    def rearrange(self, s, **kw):
        return View(self.tile, self.ap.rearrange(s, **kw))

    def bitcast(self, dt):
        return View(self.tile, self.ap.bitcast(dt))

    def to_broadcast(self, shape):
        return View(self.tile, self.ap.to_broadcast(shape))

    def broadcast_to(self, shape):
        return View(self.tile, self.ap.broadcast_to(shape))

    def unsqueeze(self, a):
        return View(self.tile, self.ap.unsqueeze(a))

    def partition_broadcast(self, n):
        return View(self.tile, self.ap.partition_broadcast(n))


class RowBlocks:
    def __init__(self, views, bs):
        self.views, self.bs = views, bs

    def rows(self, r0, n):
        blk = self.views[r0 // self.bs]
        o = r0 % self.bs
        assert o + n <= self.bs
        return blk[o:o + n, :]


def rows_view(X, r0, n):
    return X.rows(r0, n) if isinstance(X, RowBlocks) else X[r0:r0 + n, :]


class Prog:
    def __init__(self, nc, same_engine_sync=SAME_SYNC_DEFAULT, n_dma_sems=12):
        self.nc = nc
        self.es = ExitStack()
        self.ops = {e: [] for e in ENGS}
        self.count = {e: 0 for e in ENGS}
        self.waited = {e: {} for e in ENGS}
        self.same_engine_sync = same_engine_sync
        self.sems = {}
        for e in ENGS:
            self.sems[("eng", e)] = self.es.enter_context(nc.semaphore("s_" + e))
        self.n_dma_sems = n_dma_sems
        self.dma_k = {}
        for lane in ("sp", "pool", "act"):
            self.dma_k[lane] = 0
            for i in range(n_dma_sems):
                self.sems[("dma", lane, i)] = self.es.enter_context(
                    nc.semaphore("d_%s_%d" % (lane, i)))
        self.sems[("cc",)] = self.es.enter_context(nc.semaphore("s_cc"))
        self.cc_count = 0
        self.n_tiles = 0
        self.final_tokens = []
        self.ses = None
        self.stage_id = 0

    def begin_stage(self):
        self.ses = ExitStack()
        self.stage_id += 1

    def end_stage(self, last=False):
        self.barrier()
        self.emit(last=last)
        self.ses.close()
        self.ses = None

    def sbuf(self, shape, dt, name=None):
        self.n_tiles += 1
        name = name or ("t%d" % self.n_tiles)
        if self.ses is not None:
            name = "s%d_%s" % (self.stage_id, name)
        t = (self.ses or self.es).enter_context(self.nc.sbuf_tensor(name, list(shape), dt))
        return Tile(t, name)

    def psum(self, shape, dt, name=None):
        self.n_tiles += 1
        name = name or ("p%d" % self.n_tiles)
        if self.ses is not None:
            name = "s%d_%s" % (self.stage_id, name)
        t = (self.ses or self.es).enter_context(self.nc.psum_tensor(name, list(shape), dt))
        tl = Tile(t, name)
        tl.is_psum = True
        return tl

    def dram(self, name, shape, dt, kind):
        t = self.nc.dram_tensor(name, list(shape), dt, kind=kind)
        return Tile(t.ap(), name)

    def subtile(self, tile, idx, name=None):
        return Tile(tile.tensor[idx], name or (tile.name + "_sub"))

    def _deps(self, eng, reads, writes, skip_same=False):
        deps = {}

        def add(tok):
            if tok is None:
                return
            k, v = tok
            if deps.get(k, 0) < v:
                deps[k] = v

        for t in reads:
            add(t.last_w)
        for t in writes:
            add(t.last_w)
            for k, v in t.readers.items():
                add((k, v))
        out = []
        for k, v in deps.items():
            if k == ("eng", eng):
                if skip_same:
                    continue
            if self.waited[eng].get(k, 0) >= v:
                continue
            self.waited[eng][k] = v
            out.append((k, v))
        return out

    def _commit(self, tok, reads, writes):
        k, v = tok
        for t in writes:
            t.last_w = tok
            t.readers = {}
        for t in reads:
            if t in writes:
                continue
            if t.readers.get(k, 0) < v:
                t.readers[k] = v

    def _split(self, kwargs):
        reads, writes, real = [], [], {}
        for k, a in kwargs.items():
            if isinstance(a, View):
                (writes if k in _WRITE_KEYS else reads).append(a.tile)
                real[k] = a.ap
            else:
                real[k] = a
        return reads, writes, real

    def op(self, eng, method, extra_reads=(), extra_writes=(), **kwargs):
        reads, writes, real = self._split(kwargs)
        reads += [v.tile if isinstance(v, View) else v for v in extra_reads]
        writes += [v.tile if isinstance(v, View) else v for v in extra_writes]
        if eng != "pe":
            writes += [t for t in reads if t.is_psum and t not in writes]
        waits = self._deps(eng, reads, writes,
                           skip_same=(eng == "pe" or not self.same_engine_sync))
        self.count[eng] += 1
        tok = (("eng", eng), self.count[eng])
        self.ops[eng].append((waits, method, real, tok, 1))
        self._commit(tok, reads, writes)
        return tok

    def dve(self, method, **kw):
        return self.op("dve", method, **kw)

    def act(self, method, **kw):
        return self.op("act", method, **kw)

    def pool(self, method, **kw):
        return self.op("pool", method, **kw)

    def pe(self, method, **kw):
        return self.op("pe", method, **kw)

    def mm(self, out, lhsT, rhs, start=True, stop=True, **kw):
        return self.op("pe", "matmul", out=out, lhsT=lhsT, rhs=rhs, start=start, stop=stop, **kw)

    def dma(self, out, in_, lane="sp", final=False, **kw):
        reads, writes = [in_.tile], [out.tile]
        k = self.dma_k[lane]
        self.dma_k[lane] += 1
        si = k % self.n_dma_sems
        semkey = ("dma", lane, si)
        waits = self._deps(lane, reads, writes)
        prev = 16 * (k // self.n_dma_sems)
        if prev > 0 and self.waited[lane].get(semkey, 0) < prev:
            self.waited[lane][semkey] = prev
            waits.append((semkey, prev))
        tok = (semkey, prev + 16)
        real = dict(out=out.ap, in_=in_.ap, **kw)
        self.ops[lane].append((waits, "dma_start", real, tok, 16))
        self._commit(tok, reads, writes)
        if final:
            self.final_tokens.append(tok)
        return tok

    def all_gather(self, out, in_, groups, inc=1):
        lane = "pool"
        reads, writes = [in_.tile], [out.tile]
        waits = self._deps(lane, reads, writes)
        self.cc_count += inc
        tok = (("cc",), self.cc_count)
        real = dict(kind="AllGather", op=ALU.bypass, replica_groups=groups, ins=[in_.ap], outs=[out.ap])
        self.ops[lane].append((waits, "collective_compute", real, tok, inc))
        self._commit(tok, reads, writes)
        return tok

    def barrier(self):
        allk = {}
        for e in ENGS:
            if self.count[e] > 0:
                allk[("eng", e)] = self.count[e]
        for lane in ("sp", "pool", "act"):
            k = self.dma_k[lane]
            for i in range(self.n_dma_sems):
                n = (k - i + self.n_dma_sems - 1) // self.n_dma_sems if k > i else 0
                if n > 0:
                    allk[("dma", lane, i)] = 16 * n
        if self.cc_count > 0:
            allk[("cc",)] = self.cc_count
        for e in ENGS:
            waits = []
            for k, v in allk.items():
                if self.waited[e].get(k, 0) >= v:
                    continue
                self.waited[e][k] = v
                waits.append((k, v))
            if waits:
                self.ops[e].append((waits, None, None, None, 0))

    def emit(self, last=True):
        nc = self.nc
        fin = []
        seen = {}
        for k, v in self.final_tokens:
            if seen.get(k, 0) < v:
                seen[k] = v
        fin = list(seen.items())
        engobj = {"pe": "tensor", "dve": "vector", "act": "scalar", "pool": "gpsimd", "sp": "sync"}
        with nc.Block() as block:
            for e in ENGS:
                ops = self.ops[e]
                extra = fin if (e == "sp" and last) else []

                def body(eng, ops=ops, extra=extra):
                    for waits, method, real, tok, inc in ops:
                        for k, v in waits:
                            eng.wait_ge(self.sems[k], v)
                        if method is None:
                            continue
                        ins = getattr(eng, method)(**real)
                        ins.then_inc(self.sems[tok[0]], inc)
                    for k, v in extra:
                        eng.wait_ge(self.sems[k], v)

                if not ops and not extra:
                    continue
                getattr(block, engobj[e])(body)
        self.ops = {e: [] for e in ENGS}

    def close(self):
        self.es.close()


D = 1024
NE = 16
DE = 512
ALPHA = 8 ** 0.25
LN_EPS = 1e-5


def layer_norm_tile(P, x, out, g_t, b_t, stats, mv, rstd, tmp, eps=LN_EPS):
    for c in range(2):
        P.dve("bn_stats", out=stats[:, c, :], in_=x[:, c * 512:(c + 1) * 512])
    P.dve("bn_aggr", out=mv.v(), in_=stats.v())
    P.dve("tensor_scalar", out=rstd.v(), in0=mv[:, 1:2], scalar1=eps, scalar2=None, op0=ALU.add)
    P.act("activation", out=rstd.v(), in_=rstd.v(), func=AF.Sqrt)
    P.dve("reciprocal", out=rstd.v(), in_=rstd.v())
    P.dve("tensor_scalar", out=out, in0=x, scalar1=mv[:, 0:1], scalar2=rstd[:, 0:1],
          op0=ALU.subtract, op1=ALU.mult)
    P.dve("tensor_tensor", out=out, in0=out, in1=g_t, op=ALU.mult)
    P.dve("tensor_tensor", out=out, in0=out, in1=b_t, op=ALU.add)


def body_T(P, io, TOK=2048, stop=99, sub=99):
    nc = P.nc
    NT = TOK // 128
    NC4 = TOK // 512
    hin, xT, wout, rw, rb = io["hin"], io.get("xT"), io["wout"], io["rw"], io["rb"]
    wg, wu, wd, hout = io["wg"], io["wu"], io["wd"], io["hout"]
    lnrows = io["ln"]
    houtT = io.get("houtT")

    acc = P.sbuf([128, NT, D], F32, "acc")
    acc_t = [P.subtile(acc, (slice(None), t, slice(None)), "acc%d" % t) for t in range(NT)]
    h1T = P.sbuf([128, 8, TOK], BF16, "h1T")
    arena = P.sbuf([128, 24576], BF16, "arena")
    lnt = [P.sbuf([128, D], F32, "lnt%d" % i) for i in range(4)]
    rwt = P.sbuf([128, 8, NE], F32, "rwt")
    rbt = P.sbuf([128, NE], F32, "rbt")
    ident = P.sbuf([128, 128], F32, "ident")
    hin_t = [P.sbuf([128, D], F32, "hin0")] * 2
    h1_t = [P.sbuf([128, D], F32, "h1_%d" % i) for i in range(2)]
    h1Tf = [P.sbuf([128, 8, 128], F32, "h1Tf0")] * 2
    tmp_t = [None, None]
    stats = [P.sbuf([128, 2, 6], F32, "st%d" % i) for i in range(2)]
    mv = [P.sbuf([128, 2], F32, "mv%d" % i) for i in range(2)]
    rstd = [P.sbuf([128, 1], F32, "rstd%d" % i) for i in range(2)]
    scores = P.sbuf([128, NT, NE], F32, "scores")
    comb = P.sbuf([128, NT, NE], F32, "comb")
    r_sel = P.sbuf([128, NT, NE], F32, "r_sel")
    r_cnt = P.sbuf([128, NT, NE], F32, "r_cnt")
    r_tmp = P.sbuf([128, NT, NE], F32, "r_tmp")
    r_gs = P.sbuf([128, NT, 4], F32, "r_gs")
    r_gm = P.sbuf([128, NT], F32, "r_gm")
    r_gmask = P.sbuf([128, NT, 4], F32, "r_gmask")
    r_den = P.sbuf([128, NT], F32, "r_den")
    heT = [[P.sbuf([128, 512], BF16, "heT%d_%d" % (b, f)) for f in range(4)] for b in range(2)]
    sg = [P.sbuf([128, 512], F32, "sg%d" % i) for i in range(2)]
    ps = [P.psum([128, 512], F32, "ps%d" % i) for i in range(8)]

    ar = arena.tensor
    woutb = Tile(ar[:, 0:8192].rearrange("p (k f) -> p k f", k=8), "woutb")
    xTb = Tile(ar[:, 8192:8192 + 8 * TOK].rearrange("p (k t) -> p k t", k=8), "xTb")
    wbuf = []
    for b in range(2):
        o = b * 12288
        wbuf.append(dict(
            g=Tile(ar[:, o:o + 4096].rearrange("p (k f) -> p k f", k=8), "wg%d" % b),
            u=Tile(ar[:, o + 4096:o + 8192].rearrange("p (k f) -> p k f", k=8), "wu%d" % b),
            d=Tile(ar[:, o + 8192:o + 12288].rearrange("p (k f) -> p k f", k=4), "wd%d" % b)))

    for i in range(4):
        P.dma(lnt[i].v(), View(lnrows[i], lnrows[i].tensor.broadcast_to([128, D])), lane="sp")
    P.dma(rwt.v(), rw.v().rearrange("(k p) e -> p k e", p=128), lane="sp")
    P.dma(rbt.v(), View(rb, rb.tensor[0:1, :].broadcast_to([128, NE])), lane="sp")
    P.op("pool", "memset", extra_writes=[ident], ap=ident.v().ap, constant=1.0)
    P.pool("affine_select", out=ident.v(), in_=ident.v(), pattern=[[-1, 128]],
           compare_op=ALU.is_equal, fill=0.0, base=0, channel_multiplier=1)
    P.dma(woutb.v(), wout.v().rearrange("(k p) f -> p k f", p=128), lane="pool")
    if "xT_blend" in io:
        xfull, sel = io["xT_blend"]
        selt = P.sbuf([128, 2], F32, "selt")
        stg = [P.sbuf([128, TOK], BF16, "stg%d" % i) for i in range(2)]
        P.dma(selt.v(), sel, lane="sp")
        for k in range(8):
            P.dma(stg[0].v(), xfull[k][:, 0:TOK], lane="pool")
            P.dma(stg[1].v(), xfull[k][:, TOK:2 * TOK], lane="pool")
            P.dve("tensor_scalar", out=stg[0].v(), in0=stg[0].v(), scalar1=selt[:, 0:1], scalar2=None, op0=ALU.mult)
            P.dve("scalar_tensor_tensor", out=xTb[:, k, :], in0=stg[1].v(), scalar=selt[:, 1:2], in1=stg[0].v(),
                  op0=ALU.mult, op1=ALU.add)
    else:
        for k in range(8):
            P.dma(xTb[:, k, :], xT[k * 128:(k + 1) * 128, :], lane="pool")

    for tt in range(NT):
        b = tt % 2
        if sub < 99 and tt > 0:
            break
        P.dma(hin_t[b].v(), hin[tt * 128:(tt + 1) * 128, :], lane="sp")
        pm = [ps[(2 * tt) % 4], ps[(2 * tt) % 4 + 1]]
        for half in range(2):
            for k in range(8):
                P.mm(pm[half].v(), xTb[:, k, tt * 128:(tt + 1) * 128],
                     woutb[:, k, half * 512:(half + 1) * 512], start=(k == 0), stop=(k == 7))
        if sub == 1:
            break
        for half in range(2):
            P.dve("scalar_tensor_tensor", out=hin_t[b][:, half * 512:(half + 1) * 512],
                  in0=hin_t[b][:, half * 512:(half + 1) * 512], scalar=ALPHA, in1=pm[half].v(),
                  op0=ALU.mult, op1=ALU.add)
        if sub == 2:
            break
        layer_norm_tile(P, hin_t[b].v(), h1_t[b].v(), lnt[0].v(), lnt[1].v(),
                        stats[b], mv[b], rstd[b], tmp_t[b])
        if sub == 3:
            break
        P.act("mul", out=acc_t[tt].v(), in_=h1_t[b].v(), mul=ALPHA)
        pt = [ps[4 + (2 * tt) % 4], ps[4 + (2 * tt) % 4 + 1]]
        for k in range(8):
            P.pe("transpose", out=pt[k // 4][:, (k % 4) * 128:(k % 4 + 1) * 128],
                 in_=h1_t[b][:, k * 128:(k + 1) * 128], identity=ident.v())
        if sub == 4:
            break
        for hf in range(2):
            P.act("copy", out=h1T[:, hf * 4:(hf + 1) * 4, tt * 128:(tt + 1) * 128],
                  in_=pt[hf].v().rearrange("p (k t) -> p k t", k=4))
            P.dve("tensor_copy", out=h1Tf[b][:, hf * 4:(hf + 1) * 4, :],
                  in_=pt[hf].v().rearrange("p (k t) -> p k t", k=4))
        if sub == 5:
            break
        pr = pm[0]
        for k in range(8):
            P.mm(pr[:, 0:NE], h1Tf[b][:, k, :], rwt[:, k, :], start=(k == 0), stop=(k == 7))
        P.act("activation", out=scores[:, tt, :], in_=pr[:, 0:NE], func=AF.Sigmoid)

    def v4(t):
        return t.v().rearrange("p t (g i) -> p t g i", g=4)
    P.dve("tensor_tensor", out=r_sel.v(), in0=scores.v(),
          in1=View(rbt, rbt.tensor[:, :].unsqueeze(1).to_broadcast([128, NT, NE])), op=ALU.add)
    P.op("dve", "memset", extra_writes=[r_cnt], ap=r_cnt.v().ap, constant=0.0)
    for j in range(4):
        selj = View(r_sel, v4(r_sel).ap[:, :, :, j:j + 1].to_broadcast([128, NT, 4, 4]))
        P.dve("tensor_tensor", out=v4(r_tmp), in0=selj, in1=v4(r_sel), op=ALU.is_gt)
        P.dve("tensor_tensor", out=r_cnt.v(), in0=r_cnt.v(), in1=r_tmp.v(), op=ALU.add)
    P.dve("tensor_single_scalar", out=r_cnt.v(), in_=r_cnt.v(), scalar=1.5, op=ALU.is_lt)
    P.dve("tensor_tensor", out=r_tmp.v(), in0=r_sel.v(), in1=r_cnt.v(), op=ALU.mult)
    P.dve("tensor_reduce", out=r_gs.v(), in_=v4(r_tmp), axis=AX.X, op=ALU.add)
    P.dve("tensor_reduce", out=r_gm.v(), in_=r_gs.v(), axis=AX.X, op=ALU.max)
    P.dve("tensor_tensor", out=r_gmask.v(), in0=r_gs.v(),
          in1=View(r_gm, r_gm.tensor[:, :].unsqueeze(2).to_broadcast([128, NT, 4])), op=ALU.is_ge)
    P.dve("tensor_tensor", out=v4(r_cnt), in0=v4(r_cnt),
          in1=View(r_gmask, r_gmask.tensor[:, :, :].unsqueeze(3).to_broadcast([128, NT, 4, 4])),
          op=ALU.mult)
    P.dve("tensor_tensor", out=r_tmp.v(), in0=scores.v(), in1=r_cnt.v(), op=ALU.mult)
    P.dve("tensor_reduce", out=r_den.v(), in_=r_tmp.v(), axis=AX.X, op=ALU.add)
    P.dve("reciprocal", out=r_den.v(), in_=r_den.v())
    P.dve("tensor_tensor", out=comb.v(), in0=r_tmp.v(),
          in1=View(r_den, r_den.tensor[:, :].unsqueeze(2).to_broadcast([128, NT, NE])), op=ALU.mult)

    P.barrier()

    it = 0
    for e in range(NE):
        wb = wbuf[e % 2]
        P.dma(wb["g"].v(), wg[e].rearrange("(k p) f -> p k f", p=128), lane="pool")
        P.dma(wb["u"].v(), wu[e].rearrange("(k p) f -> p k f", p=128), lane="pool")
        P.dma(wb["d"].v(), wd[e].rearrange("(k p) f -> p k f", p=128), lane="pool")
        for tc in range(NC4):
            hb = heT[it % 2]
            for ft in range(4):
                pg = ps[(2 * (it * 4 + ft)) % 4]
                pu = ps[(2 * (it * 4 + ft)) % 4 + 1]
                for k in range(8):
                    P.mm(pg.v(), wb["g"][:, k, ft * 128:(ft + 1) * 128],
                         h1T[:, k, tc * 512:(tc + 1) * 512], start=(k == 0), stop=(k == 7))
                for k in range(8):
                    P.mm(pu.v(), wb["u"][:, k, ft * 128:(ft + 1) * 128],
                         h1T[:, k, tc * 512:(tc + 1) * 512], start=(k == 0), stop=(k == 7))
                s_ = sg[(it * 4 + ft) % 2]
                P.act("activation", out=s_.v(), in_=pg.v(), func=AF.Silu)
                P.dve("tensor_tensor", out=hb[ft].v(), in0=s_.v(), in1=pu.v(), op=ALU.mult)
            for t4 in range(4):
                tt = tc * 4 + t4
                for half in range(2):
                    py = ps[4 + (2 * (it * 4 + t4)) % 4 + half]
                    for ft in range(4):
                        P.mm(py.v(), hb[ft][:, t4 * 128:(t4 + 1) * 128],
                             wb["d"][:, ft, half * 512:(half + 1) * 512], start=(ft == 0), stop=(ft == 3))
                    P.dve("scalar_tensor_tensor", out=acc_t[tt][:, half * 512:(half + 1) * 512],
                          in0=py.v(), scalar=comb[:, tt, e:e + 1],
                          in1=acc_t[tt][:, half * 512:(half + 1) * 512], op0=ALU.mult, op1=ALU.add)
            it += 1

    for tt in range(NT):
        b = tt % 2
        layer_norm_tile(P, acc_t[tt].v(), h1_t[b].v(), lnt[2].v(), lnt[3].v(),
                        stats[b], mv[b], rstd[b], tmp_t[b])
        P.dma(hout[tt * 128:(tt + 1) * 128, :], h1_t[b].v(), lane="sp", final=True)
        if houtT is not None:
            ptt = [ps[(2 * tt) % 4], ps[(2 * tt) % 4 + 1]]
            for k in range(8):
                P.pe("transpose", out=ptt[k // 4][:, (k % 4) * 128:(k % 4 + 1) * 128],
                     in_=h1_t[b][:, k * 128:(k + 1) * 128], identity=ident.v())
            for hf in range(2):
                P.act("copy", out=h1Tf[b][:, hf * 4:(hf + 1) * 4, :],
                      in_=ptt[hf].v().rearrange("p (k t) -> p k t", k=4))
            if isinstance(houtT, RowBlocks):
                assert houtT.bs == 256
                for j2 in range(4):
                    P.dma(houtT.views[j2][:, tt * 128:(tt + 1) * 128].rearrange("(k p) t -> p k t", p=128),
                          h1Tf[b][:, 2 * j2:2 * j2 + 2, :], lane="sp", final=True)
            else:
                P.dma(houtT[:, tt * 128:(tt + 1) * 128].rearrange("(k p) t -> p k t", p=128), h1Tf[b].v(),
                      lane="sp", final=True)


def build_T(nc, TOK=2048):
    P = Prog(nc)
    io = dict(hin=P.dram("hin", [TOK, D], F32, "ExternalInput"), xT=P.dram("xT", [D, TOK], F32, "ExternalInput"),
              wout=P.dram("wout", [D, D], F32, "ExternalInput"), rw=P.dram("rw", [D, NE], F32, "ExternalInput"),
              rb=P.dram("rb", [1, NE], F32, "ExternalInput"), wg=P.dram("wg", [NE, D, DE], F32, "ExternalInput"),
              wu=P.dram("wu", [NE, D, DE], F32, "ExternalInput"), wd=P.dram("wd", [NE, DE, D], F32, "ExternalInput"),
              hout=P.dram("hout", [TOK, D], F32, "ExternalOutput"))
    lnp = P.dram("lnp", [4, D], F32, "ExternalInput")
    io["ln"] = [lnp[i:i + 1, :] for i in range(4)]
    body_T(P, io, TOK)
    P.emit()
    P.close()
    return nc


D = 1024


def body_GM(P, io, TOK=2048):
    NT = TOK // 128
    hT, w_in, b_in, wsT, bsT = io["hT"], io["w_in"], io["b_in"], io["wsT"], io["bsT"]
    lnrows = io["ln"]

    hTb = P.sbuf([128, 8, TOK], BF16, "hTb")
    winb = P.sbuf([128, 8, 2 * D], BF16, "winb")
    bint = P.sbuf([128, 2 * D], F32, "bint")
    lnt = [P.sbuf([128, D], F32, "lnt%d" % i) for i in range(2)]
    wst = P.sbuf([128, 8, 128], F32, "wst")
    wsb = P.sbuf([128, 8, 128], BF16, "wsb")
    bst = P.sbuf([128, 8], F32, "bst")
    zt = [P.sbuf([128, 2 * D], F32, "z%d" % i) for i in range(2)]
    vn = [P.sbuf([128, D], F32, "vn%d" % i) for i in range(2)]
    vnb = [P.sbuf([128, D], BF16, "vnb%d" % i) for i in range(2)]
    yt = [P.sbuf([128, D], F32, "y%d" % i) for i in range(2)]
    stats = [P.sbuf([128, 2, 6], F32, "st%d" % i) for i in range(2)]
    mv = [P.sbuf([128, 2], F32, "mv%d" % i) for i in range(2)]
    rstd = [P.sbuf([128, 1], F32, "rstd%d" % i) for i in range(2)]
    ps = [P.psum([128, 512], F32, "ps%d" % i) for i in range(8)]
    identf = P.sbuf([128, 128], F32, "identf")
    yT = [P.sbuf([128, 8, 128], F32, "yT%d" % i) for i in range(2)]
    P.op("pool", "memset", extra_writes=[identf], ap=identf.v().ap, constant=1.0)
    P.pool("affine_select", out=identf.v(), in_=identf.v(), pattern=[[-1, 128]],
           compare_op=ALU.is_equal, fill=0.0, base=0, channel_multiplier=1)

    for k in range(8):
        for c2 in range(max(1, TOK // 2048)):
            w_ = min(TOK, 2048)
            P.dma(hTb[:, k, c2 * w_:(c2 + 1) * w_], rows_view(hT, k * 128, 128)[:, c2 * w_:(c2 + 1) * w_], lane="pool")
        P.dma(winb[:, k, 0:1024], w_in[k * 128:(k + 1) * 128, 0:1024], lane="pool")
        P.dma(winb[:, k, 1024:2048], w_in[k * 128:(k + 1) * 128, 1024:2048], lane="pool")
    P.dma(bint.v(), View(b_in, b_in.tensor[0:1, :].broadcast_to([128, 2 * D])), lane="sp")
    for i in range(2):
        P.dma(lnt[i].v(), View(lnrows[i], lnrows[i].tensor.broadcast_to([128, D])), lane="sp")
    P.dma(wst.v(), wsT.v(), lane="sp")
    P.dma(bst.v(), bsT.v(), lane="sp")
    P.pool("affine_select", out=wst.v(), in_=wst.v(), pattern=[[0, 8], [1, 128]],
           compare_op=ALU.is_ge, fill=0.0, base=0, channel_multiplier=-1)
    P.pool("tensor_copy", out=wsb.v(), in_=wst.v())

    for tt in range(NT):
        b = tt % 2
        for cb in range(4):
            pz = ps[cb]
            for k in range(8):
                P.mm(pz.v(), hTb[:, k, tt * 128:(tt + 1) * 128], winb[:, k, cb * 512:(cb + 1) * 512],
                     start=(k == 0), stop=(k == 7))
            P.dve("tensor_tensor", out=zt[b][:, cb * 512:(cb + 1) * 512], in0=pz.v(),
                  in1=bint[:, cb * 512:(cb + 1) * 512], op=ALU.add)
        P.act("activation", out=zt[b].v(), in_=zt[b].v(), func=AF.Gelu)
        layer_norm_tile(P, zt[b][:, D:2 * D], vn[b].v(), lnt[0].v(), lnt[1].v(), stats[b], mv[b], rstd[b], None)
        P.act("copy", out=vnb[b].v(), in_=vn[b].v())
        for g in range(8):
            psv = ps[4 + g // 4]
            P.mm(psv[:, (g % 4) * 128:(g % 4 + 1) * 128], wsb[:, g, :], vnb[b][:, g * 128:(g + 1) * 128])
        for g in range(8):
            psv = ps[4 + g // 4]
            P.dve("scalar_tensor_tensor", out=yt[b][:, g * 128:(g + 1) * 128],
                  in0=psv[:, (g % 4) * 128:(g % 4 + 1) * 128], scalar=bst[:, g:g + 1],
                  in1=zt[b][:, g * 128:(g + 1) * 128], op0=ALU.add, op1=ALU.mult)
        if "X" in io:
            P.dma(io["X"][tt * 128:(tt + 1) * 128, :], yt[b].v(), lane="sp", final=True)
        else:
            for k in range(8):
                P.pe("transpose", out=ps[6 + k // 4][:, (k % 4) * 128:(k % 4 + 1) * 128],
                     in_=yt[b][:, k * 128:(k + 1) * 128], identity=identf.v())
            for hf in range(2):
                P.act("copy", out=yT[b][:, hf * 4:(hf + 1) * 4, :],
                      in_=ps[6 + hf].v().rearrange("p (k t) -> p k t", k=4))
            P.dma(io["XT"][:, tt * 128:(tt + 1) * 128].rearrange("(k p) t -> p k t", p=128), yT[b].v(),
                  lane="sp", final=True)


def build_GM(nc, TOK=2048):
    P = Prog(nc)
    lnp = P.dram("lnp", [2, D], F32, "ExternalInput")
    io = dict(hT=P.dram("hT", [D, TOK], F32, "ExternalInput").v(), w_in=P.dram("w_in", [D, 2 * D], F32, "ExternalInput").v(),
              b_in=P.dram("b_in", [1, 2 * D], F32, "ExternalInput").v(), ln=[lnp[i:i + 1, :] for i in range(2)],
              wsT=P.dram("wsT", [128, 8, 128], F32, "ExternalInput").v(), bsT=P.dram("bsT", [128, 8], F32, "ExternalInput").v(),
              X=P.dram("X", [TOK, D], F32, "ExternalOutput").v())
    body_GM(P, io, TOK)
    P.emit()
    P.close()
    return nc


D = 1024
S = 4096
NB = S // 128
NQ = S // 512
NH = 8


def body_FOX(P, hT, groups):
    hTb = P.sbuf([128, 8, S], BF16, "hTb")
    wqb = P.sbuf([128, 8, 512], BF16, "wqb")
    wkb = P.sbuf([128, 8, 512], BF16, "wkb")
    wvb = P.sbuf([128, 8, 512], BF16, "wvb")
    wogb = P.sbuf([128, 8, 512], BF16, "wogb")
    wfb = P.sbuf([128, 8, NH], BF16, "wfb")
    bft = P.sbuf([128, NH], F32, "bft")
    Vall = P.sbuf([128, NB, 512], BF16, "Vall")
    QT = [P.sbuf([64, S], BF16, "QT%d" % i) for i in range(2)]
    KT = [P.sbuf([64, S], BF16, "KT%d" % i) for i in range(2)]
    OG = [P.sbuf([64, S], BF16, "OG%d" % i) for i in range(2)]
    triU = P.sbuf([128, 128], F32, "triU")
    triUb = P.sbuf([128, 128], BF16, "triUb")
    onesf = P.sbuf([128, 128], F32, "onesf")
    Vaug = [P.sbuf([128, NB, 128], BF16, "Vaug0")] * 2
    Sh = P.sbuf([128, 64], F32, "Sh")
    rd = P.sbuf([128, 512], F32, "rd")
    logf = P.sbuf([128, NB, NH], F32, "logf")
    negcin = P.sbuf([128, NB, NH], F32, "negcin")
    Rb = P.sbuf([128, NB + 1, NH], F32, "Rb")
    negR = P.sbuf([128, NB + 1, NH], F32, "negR")
    biasq = [P.sbuf([128, NB], F32, "biasq%d" % i) for i in range(2)]
    Pt = [P.sbuf([128, 512], BF16, "P%d" % i) for i in range(4)]
    rden = [P.sbuf([64, 512], F32, "rden%d" % i) for i in range(2)]
    ot = [P.sbuf([64, 512], F32, "ot%d" % i) for i in range(2)]
    ps = [P.psum([128, 512], F32, "ps%d" % i) for i in range(8)]

    for k in range(8):
        for c4 in range(4):
            P.dma(hTb[:, k, c4 * 1024:(c4 + 1) * 1024], hT[k * 128:(k + 1) * 128, c4 * 1024:(c4 + 1) * 1024], lane="pool")
    P.op("pool", "memset", extra_writes=[triU], ap=triU.v().ap, constant=1.0)
    P.pool("affine_select", out=triU.v(), in_=triU.v(), pattern=[[1, 128]],
           compare_op=ALU.is_ge, fill=0.0, base=0, channel_multiplier=-1)
    P.pool("tensor_copy", out=triUb.v(), in_=triU.v())
    P.op("pool", "memset", extra_writes=[onesf], ap=onesf.v().ap, constant=1.0)
    P.op("pool", "memset", extra_writes=[Vaug[0]], ap=Vaug[0].v().ap, constant=1.0)
    P.op("pool", "memset", extra_writes=[rd], ap=rd.v().ap, constant=0.0)
    P.op("pool", "memset", extra_writes=[Sh], ap=Sh.v().ap, constant=1.0)
    P.pool("affine_select", out=Sh.v(), in_=Sh.v(), pattern=[[-1, 64]],
           compare_op=ALU.is_equal, fill=0.0, base=-64, channel_multiplier=1)

    unit = 0
    sc = 0
    for grp in groups:
        wq, wk, wv, wog, wf, bf, XT = (grp[n] for n in ("wq", "wk", "wv", "wog", "wf", "bf", "XT"))
        for wsrc, wdst in ((wq, wqb), (wk, wkb), (wv, wvb), (wog, wogb)):
            P.dma(wdst.v(), wsrc.rearrange("(k p) f -> p k f", p=128), lane="pool")
        P.dma(wfb.v(), wf.rearrange("(k p) f -> p k f", p=128), lane="pool")
        P.dma(bft.v(), View(bf, bf.tensor.broadcast_to([128, NH])), lane="sp")
        pf = ps[0]
        for kb in range(NB):
            for k in range(8):
                P.mm(pf[:, kb * NH:(kb + 1) * NH], hTb[:, k, kb * 128:(kb + 1) * 128], wfb[:, k, :],
                     start=(k == 0), stop=(k == 7))
        lf2 = logf.v().rearrange("p b h -> p (b h)")
        P.dve("tensor_tensor", out=logf.v(), in0=pf[:, 0:NB * NH].rearrange("p (b h) -> p b h", h=NH),
              in1=View(bft, bft.tensor[:, :].unsqueeze(1).to_broadcast([128, NB, NH])), op=ALU.add)
        P.act("activation", out=lf2, in_=lf2, func=AF.Exp, scale=-1.0)
        P.act("activation", out=lf2, in_=lf2, func=AF.Ln, bias=1.0)
        P.dve("tensor_scalar", out=lf2, in0=lf2, scalar1=-1.0, scalar2=None, op0=ALU.mult)
        pc = ps[1]
        P.mm(pc[:, 0:NB * NH], triU.v(), lf2)
        P.dve("tensor_scalar", out=negcin.v().rearrange("p b h -> p (b h)"), in0=pc[:, 0:NB * NH],
              scalar1=-1.0, scalar2=None, op0=ALU.mult)
        pT = ps[2]
        P.mm(pT[:, 0:NB * NH], onesf.v(), lf2)
        P.op("dve", "memset", extra_writes=[Rb], ap=Rb[:, 0, :].ap, constant=0.0)
        for m in range(NB):
            P.dve("tensor_tensor", out=Rb[:, m + 1, :], in0=Rb[:, m, :], in1=pT[:, m * NH:(m + 1) * NH], op=ALU.add)
        P.dve("tensor_scalar", out=negR.v(), in0=Rb.v(), scalar1=-1.0, scalar2=None, op0=ALU.mult)

        for kb in range(NB):
            pv = ps[4 + kb % 4]
            for k in range(8):
                P.mm(pv.v(), hTb[:, k, kb * 128:(kb + 1) * 128], wvb[:, k, :], start=(k == 0), stop=(k == 7))
            if kb % 2 == 0:
                P.dve("tensor_copy", out=Vall[:, kb, :], in_=pv.v())
            else:
                P.act("copy", out=Vall[:, kb, :], in_=pv.v())

        for h in range(NH):
            hb = h % 2
            for j in range(NQ):
                for (wsrc, dst, kind) in ((wqb, QT[hb], 0), (wkb, KT[hb], 0), (wogb, OG[hb], 1)):
                    pp = ps[sc % 4]
                    sc += 1
                    for k in range(8):
                        P.mm(pp[0:64, :], wsrc[:, k, h * 64:(h + 1) * 64], hTb[:, k, j * 512:(j + 1) * 512],
                             start=(k == 0), stop=(k == 7))
                    if kind == 0:
                        P.dve("tensor_copy", out=dst[:, j * 512:(j + 1) * 512], in_=pp[0:64, :])
                    else:
                        P.act("activation", out=dst[:, j * 512:(j + 1) * 512], in_=pp[0:64, :], func=AF.Sigmoid)
            P.dve("tensor_copy", out=Vaug[hb][:, :, 0:64], in_=Vall[:, :, h * 64:(h + 1) * 64])
            for j in range(NQ):
                ub = unit % 2
                unit += 1
                nk = 4 * j + 4
                bq = biasq[ub]
                P.dve("scalar_tensor_tensor", out=bq[:, 0:nk], in0=negR[:, 0:nk, h], scalar=Rb[:, 4 * j, h:h + 1],
                      in1=negcin[:, 0:nk, h], op0=ALU.add, op1=ALU.add)
                pnum = ps[4 + 2 * ub]
                pden = ps[5 + 2 * ub]
                pend = []
                for kb in range(nk):
                    i = kb - 4 * j
                    c0 = 128 * i if i > 0 else 0
                    pS = ps[sc % 4]
                    pt_ = Pt[sc % 4]
                    sc += 1
                    P.mm(pS[:, c0:512], KT[hb][:, kb * 128:(kb + 1) * 128], QT[hb][:, j * 512 + c0:(j + 1) * 512])
                    P.act("activation", out=pt_[:, c0:512], in_=pS[:, c0:512], func=AF.Exp,
                          scale=0.125, bias=bq[:, kb:kb + 1])
                    if i >= 0:
                        P.dve("tensor_tensor", out=pt_[:, c0:c0 + 128], in0=pt_[:, c0:c0 + 128],
                              in1=triUb.v(), op=ALU.mult)
                    def pv(kb=kb, c0=c0, pt_=pt_):
                        P.mm(pnum[:, c0:512], Vaug[hb][:, kb, :], pt_[:, c0:512],
                             start=(kb == 0), stop=(kb == nk - 1))
                    pend.append(pv)
                    if len(pend) > 3:
                        pend.pop(0)()
                while pend:
                    pend.pop(0)()
                P.dve("reciprocal", out=rd[64:128, :], in_=pnum[64:128, :])
                P.mm(pden[0:64, :], Sh.v(), rd.v())
                P.act("copy", out=rden[ub].v(), in_=pden[0:64, :])
                P.dve("tensor_tensor", out=ot[ub].v(), in0=pnum[0:64, :], in1=rden[ub].v(), op=ALU.mult)
                P.dve("tensor_tensor", out=ot[ub].v(), in0=ot[ub].v(), in1=OG[hb][:, j * 512:(j + 1) * 512], op=ALU.mult)
                P.dma(rows_view(XT, h * 64, 64)[:, j * 512:(j + 1) * 512], ot[ub].v(), lane="sp", final=True)


def build_FOX(nc):
    P = Prog(nc)
    hT = P.dram("hT", [D, S], F32, "ExternalInput")
    grp = dict(wq=P.dram("wq", [D, 512], F32, "ExternalInput").v(), wk=P.dram("wk", [D, 512], F32, "ExternalInput").v(),
               wv=P.dram("wv", [D, 512], F32, "ExternalInput").v(), wog=P.dram("wog", [D, 512], F32, "ExternalInput").v(),
               wf=P.dram("wf", [D, NH], F32, "ExternalInput").v(), bf=P.dram("bf", [1, NH], F32, "ExternalInput").v(),
               XT=P.dram("XT", [512, S], F32, "ExternalOutput").v())
    body_FOX(P, hT.v(), [grp])
    P.emit()
    P.close()
    return nc


D = 1024
S = 4096
NB = S // 128
NQ = S // 512
NH = 8


def body_SB(P, hT, groups):
    hTb = P.sbuf([128, 8, S], BF16, "hTb")
    wqb = P.sbuf([128, 8, 512], BF16, "wqb")
    wkb = P.sbuf([128, 8, 512], BF16, "wkb")
    wvb = P.sbuf([128, 8, 512], BF16, "wvb")
    Vall = P.sbuf([128, NB, 512], BF16, "Vall")
    QT = [P.sbuf([64, S], BF16, "QT%d" % i) for i in range(2)]
    KT = [P.sbuf([64, S], BF16, "KT%d" % i) for i in range(2)]
    tmpf = P.sbuf([128, 128], F32, "tmpf")
    strictUb = P.sbuf([128, 128], BF16, "strictUb")
    negTriLb = P.sbuf([128, 128], BF16, "negTriLb")
    negones = P.sbuf([128, 128], BF16, "negones")
    zerosb = P.sbuf([128, 64], BF16, "zerosb")
    et = [P.sbuf([128, 512], F32, "et%d" % i) for i in range(2)]
    spb = [P.sbuf([128, 512], BF16, "spb%d" % i) for i in range(3)]
    At = [P.sbuf([128, 512], BF16, "At%d" % i) for i in range(3)]
    Lsum = [P.sbuf([128, 512], BF16, "Lsum%d" % i) for i in range(2)]
    ot = [P.sbuf([64, 512], F32, "ot%d" % i) for i in range(2)]
    ps = [P.psum([128, 512], F32, "ps%d" % i) for i in range(8)]

    for k in range(8):
        for c4 in range(4):
            P.dma(hTb[:, k, c4 * 1024:(c4 + 1) * 1024], hT[k * 128:(k + 1) * 128, c4 * 1024:(c4 + 1) * 1024], lane="pool")
    P.op("pool", "memset", extra_writes=[tmpf], ap=tmpf.v().ap, constant=1.0)
    P.pool("affine_select", out=tmpf.v(), in_=tmpf.v(), pattern=[[1, 128]],
           compare_op=ALU.is_gt, fill=0.0, base=0, channel_multiplier=-1)
    P.pool("tensor_copy", out=strictUb.v(), in_=tmpf.v())
    P.op("pool", "memset", extra_writes=[tmpf], ap=tmpf.v().ap, constant=-1.0)
    P.pool("affine_select", out=tmpf.v(), in_=tmpf.v(), pattern=[[-1, 128]],
           compare_op=ALU.is_ge, fill=0.0, base=0, channel_multiplier=1)
    P.pool("tensor_copy", out=negTriLb.v(), in_=tmpf.v())
    P.op("pool", "memset", extra_writes=[negones], ap=negones.v().ap, constant=-1.0)
    P.op("pool", "memset", extra_writes=[zerosb], ap=zerosb.v().ap, constant=0.0)

    unit = 0
    sc = 0
    for grp in groups:
        wq, wk, wv, XT = (grp[n] for n in ("wq", "wk", "wv", "XT"))
        for wsrc, wdst in ((wq, wqb), (wk, wkb), (wv, wvb)):
            P.dma(wdst.v(), wsrc.rearrange("(k p) f -> p k f", p=128), lane="pool")
        for kb in range(NB):
            pv = ps[4 + kb % 4]
            for k in range(8):
                P.mm(pv.v(), hTb[:, k, kb * 128:(kb + 1) * 128], wvb[:, k, :], start=(k == 0), stop=(k == 7))
            if kb % 2 == 0:
                P.dve("tensor_copy", out=Vall[:, kb, :], in_=pv.v())
            else:
                P.act("copy", out=Vall[:, kb, :], in_=pv.v())

        for h in range(NH):
            hb = h % 2
            for j in range(NQ):
                for (wsrc, dst, scl) in ((wqb, QT[hb], 0.125), (wkb, KT[hb], 1.0)):
                    pp = ps[sc % 2]
                    sc += 1
                    for k in range(8):
                        P.mm(pp[0:64, :], wsrc[:, k, h * 64:(h + 1) * 64], hTb[:, k, j * 512:(j + 1) * 512],
                             start=(k == 0), stop=(k == 7))
                    P.dve("tensor_scalar", out=dst[:, j * 512:(j + 1) * 512], in0=pp[0:64, :],
                          scalar1=scl, scalar2=None, op0=ALU.mult)
            for j in range(NQ):
                ub = unit % 2
                unit += 1
                nk = 4 * j + 4
                pnum = ps[4 + ub]
                ls = Lsum[ub]
                P.op("pool", "memset", extra_writes=[ls], ap=ls.v().ap, constant=0.0)
                P.mm(pnum[0:64, :], zerosb.v(), hTb[:, 0, 0:512], start=True, stop=False)
                steps = []
                for kb in range(nk - 1, -1, -1):
                    i = kb - 4 * j
                    c0 = 128 * i if i > 0 else 0
                    steps.append(dict(kb=kb, i=i, c0=c0, pA=ps[sc % 4], pB=ps[sc % 4], e_=et[sc % 2],
                                      sp_=spb[sc % 3], a_=At[sc % 3],
                                      kT=KT[hb][:, kb * 128:(kb + 1) * 128],
                                      qT=QT[hb][:, j * 512 + c0:(j + 1) * 512]))
                    sc += 1

                def s1(st):
                    c0 = st["c0"]
                    P.mm(st["pA"][:, c0:512], st["kT"], st["qT"], start=True, stop=False)
                    P.act("activation", out=st["e_"][:, c0:512], in_=st["pA"][:, c0:512], func=AF.Exp)
                    P.act("activation", out=st["sp_"][:, c0:512], in_=st["e_"][:, c0:512], func=AF.Ln, bias=1.0)
                    if st["i"] >= 0:
                        P.pool("tensor_tensor", out=st["sp_"][:, c0:c0 + 128], in0=st["sp_"][:, c0:c0 + 128],
                               in1=strictUb.v(), op=ALU.mult)

                def s2(st):
                    c0, kb = st["c0"], st["kb"]
                    first = (kb == nk - 1)
                    P.mm(st["pB"][:, c0:512], negTriLb.v(), st["sp_"][:, c0:512], start=False, stop=first)
                    if not first:
                        P.mm(st["pB"][:, c0:512], negones.v(), ls[:, c0:512], start=False, stop=True)
                    P.act("activation", out=st["a_"][:, c0:512], in_=st["pB"][:, c0:512], func=AF.Exp)
                    if st["i"] >= 0:
                        P.pool("tensor_tensor", out=st["a_"][:, c0:c0 + 128], in0=st["a_"][:, c0:c0 + 128],
                               in1=strictUb.v(), op=ALU.mult)
                    if kb > 0:
                        P.dve("tensor_tensor", out=ls[:, c0:512], in0=ls[:, c0:512], in1=st["sp_"][:, c0:512], op=ALU.add)

                def s3(st):
                    c0, kb = st["c0"], st["kb"]
                    P.mm(pnum[0:64, c0:512], Vall[:, kb, h * 64:(h + 1) * 64], st["a_"][:, c0:512],
                         start=False, stop=(kb == 0))

                ns = len(steps)
                s1(steps[0])
                for n in range(ns):
                    if n + 1 < ns:
                        s1(steps[n + 1])
                    s2(steps[n])
                    if n >= 1:
                        s3(steps[n - 1])
                s3(steps[ns - 1])
                P.dve("tensor_copy", out=ot[ub].v(), in_=pnum[0:64, :])
                P.dma(rows_view(XT, h * 64, 64)[:, j * 512:(j + 1) * 512], ot[ub].v(), lane="sp", final=True)


def build_SB(nc):
    P = Prog(nc)
    hT = P.dram("hT", [D, S], F32, "ExternalInput")
    grp = dict(wq=P.dram("wq", [D, 512], F32, "ExternalInput").v(), wk=P.dram("wk", [D, 512], F32, "ExternalInput").v(),
               wv=P.dram("wv", [D, 512], F32, "ExternalInput").v(), XT=P.dram("XT", [512, S], F32, "ExternalOutput").v())
    body_SB(P, hT.v(), [grp])
    P.emit()
    P.close()
    return nc


import math

D = 1024
S = 4096
NSB = S // 128
NH = 8
CNEG = -math.exp(-0.5)
GN_EPS = 64e-5


def body_RW(P, hT, muT, w1, a1, g1, groups, nsb=NSB):
    DEBUG = False
    dbg = None

    def dump(view, idx):
        pass

    def dump2(view, idx, rows, cols):
        pass

    def sb_(shape, dt, name):
        return P.sbuf(shape, dt, name)

    mut = sb_([128, 8, 6], F32, "mut")
    omut = sb_([128, 8, 6], F32, "omut")
    Wa = {}
    Wb = {}
    for nm, ncol in (("r", 512), ("k", 512), ("v", 512), ("w1", 64), ("a1", 64), ("g1", 128)):
        Wa[nm] = sb_([128, 8, ncol], BF16, "Wa_" + nm)
        Wb[nm] = sb_([128, 8, ncol], BF16, "Wb_" + nm)
    w2b = sb_([64, 512], BF16, "w2b")
    a2b = sb_([64, 512], BF16, "a2b")
    g2b = sb_([128, 512], BF16, "g2b")
    vt = [sb_([128, 512], F32, "vec%d" % i) for i in range(7)]
    W0, A0, KK_, KA_, GNG, GNB, RK = vt

    wstl = [sb_([128, 512], F32, "wsl%d" % i) for i in range(4)]
    P.dma(mut.v(), muT.v(), lane="sp")
    P.dve("tensor_scalar", out=omut.v(), in0=mut.v(), scalar1=-1.0, scalar2=1.0, op0=ALU.mult, op1=ALU.add)

    identf = sb_([128, 128], F32, "identf")
    P.op("pool", "memset", extra_writes=[identf], ap=identf.v().ap, constant=1.0)
    P.pool("affine_select", out=identf.v(), in_=identf.v(), pattern=[[-1, 128]],
           compare_op=ALU.is_equal, fill=0.0, base=0, channel_multiplier=1)
    BD = sb_([128, 128], F32, "BD")
    bd3 = BD.v().rearrange("p (c i) -> p c i", c=4)
    P.op("pool", "memset", extra_writes=[BD], ap=BD.v().ap, constant=1.0)
    P.pool("affine_select", out=bd3, in_=bd3, pattern=[[-32, 4], [0, 32]],
           compare_op=ALU.is_ge, fill=0.0, base=0, channel_multiplier=1)
    P.pool("affine_select", out=bd3, in_=bd3, pattern=[[32, 4], [0, 32]],
           compare_op=ALU.is_ge, fill=0.0, base=31, channel_multiplier=-1)
    mAB = sb_([128, 512], F32, "mAB")
    mC = sb_([128, 256], F32, "mC")
    triBD = sb_([128, 128], F32, "triBD")
    blkBD = sb_([128, 128], F32, "blkBD")
    P.pool("affine_select", out=mAB[:, 0:128], in_=BD.v(), pattern=[[1, 128]],
           compare_op=ALU.is_gt, fill=0.0, base=0, channel_multiplier=-1)
    P.pool("affine_select", out=mAB[:, 128:256], in_=BD.v(), pattern=[[1, 128]],
           compare_op=ALU.is_ge, fill=0.0, base=0, channel_multiplier=-1)
    P.pool("tensor_copy", out=mAB[:, 256:512], in_=mAB[:, 0:256])
    P.pool("affine_select", out=mC[:, 0:128], in_=BD.v(), pattern=[[-1, 128]],
           compare_op=ALU.is_gt, fill=0.0, base=0, channel_multiplier=1)
    P.pool("tensor_copy", out=mC[:, 128:256], in_=mC[:, 0:128])
    P.pool("tensor_scalar", out=triBD.v(), in0=mAB[:, 128:256], scalar1=CNEG, scalar2=None, op0=ALU.mult)
    P.pool("tensor_scalar", out=blkBD.v(), in0=BD.v(), scalar1=CNEG, scalar2=None, op0=ALU.mult)
    RMexp = sb_([128, 4, 64], F32, "RMexp")
    P.op("pool", "memset", extra_writes=[RMexp], ap=RMexp.v().ap, constant=1.0)
    P.pool("affine_select", out=RMexp.v(), in_=RMexp.v(), pattern=[[-32, 4], [0, 64]],
           compare_op=ALU.is_ge, fill=0.0, base=0, channel_multiplier=1)
    P.pool("affine_select", out=RMexp.v(), in_=RMexp.v(), pattern=[[32, 4], [0, 64]],
           compare_op=ALU.is_ge, fill=0.0, base=31, channel_multiplier=-1)
    CM = sb_([64, 4, 128], F32, "CM")
    cm4 = CM.v().rearrange("p c (d i) -> p c d i", d=4)
    P.op("pool", "memset", extra_writes=[CM], ap=CM.v().ap, constant=1.0)
    P.pool("affine_select", out=cm4, in_=cm4, pattern=[[1, 4], [-1, 4], [0, 32]],
           compare_op=ALU.is_equal, fill=0.0, base=0, channel_multiplier=0)
    Sel = sb_([128, 4], F32, "Sel")
    P.op("pool", "memset", extra_writes=[Sel], ap=Sel.v().ap, constant=1.0)
    P.pool("affine_select", out=Sel.v(), in_=Sel.v(), pattern=[[-32, 4]],
           compare_op=ALU.is_equal, fill=0.0, base=0, channel_multiplier=1)

    hg = [sb_([128, 8, 513], BF16, "hg%d" % i) for i in range(1)]
    th = [sb_([64, 512], BF16, "th%d" % i) for i in range(1)]
    xa = [sb_([64, 512], BF16, "xa%d" % i) for i in range(1)]
    sgT = [sb_([128, 512], BF16, "sgT%d" % i) for i in range(1)]

    def f32t(name, n=1):
        return [sb_([128, 512], F32, "%s%d" % (name, i)) for i in range(n)]
    r_t = f32t("r_t", 1)
    k_t = f32t("k_t", 1)
    V_t = f32t("V_t", 1)
    sgd = f32t("sgd", 1)
    lwc = f32t("lwc", 1)
    tmpA = f32t("tmpA", 1)
    tmpB = f32t("tmpB", 1)
    a_t = f32t("a_t", 1)
    kk_t = f32t("kk_t", 1)
    kp_t = f32t("kp_t", 1)
    ka_t = a_t
    E2 = f32t("E2", 1)
    E4 = sgd
    At = [wstl[0]]
    Bt = [wstl[1]]
    Kt = [wstl[2]]
    Rt = [wstl[3]]
    Bh = f32t("Bh", 1)
    Kh = f32t("Kh", 1)
    E5 = f32t("E5", 1)
    g_t = f32t("g_t", 1)
    bon = f32t("bon", 1)
    Vm = [sb_([128, 8, 4, 64], F32, "Vm%d" % i) for i in range(1)]
    ss8 = sb_([128, 8], F32, "ss8")
    rk8 = sb_([128, 8], F32, "rk8")
    TT = [sb_([64, 512], BF16, "TT%d" % i) for i in range(2)]
    PP = [[sb_([128, 256], BF16, "PP%d_%d" % (i, j)) for j in range(2)] for i in range(2)]
    XX = [[sb_([128, 192], BF16, "XX%d_%d" % (i, j)) for j in range(2)] for i in range(2)]
    XF32 = [sb_([128, 192], F32, "XF32_%d" % i) for i in range(2)]
    KrKh = [sb_([128, 192], F32, "KrKh%d" % i) for i in range(2)]
    AkaT = [sb_([128, 128], F32, "AkaT%d" % i) for i in range(2)]
    G1 = [sb_([64, 128], F32, "G1_%d" % i) for i in range(2)]
    G1pad = [[sb_([64, 4, 128], F32, "G1pad%d_%d" % (i, h)) for h in range(NH)] for i in range(1)]
    G2H2 = [sb_([128, 192], F32, "G2H2_%d" % i) for i in range(2)]
    X2m = [sb_([128, 4, 64], F32, "X2m%d" % i) for i in range(2)]
    WCT = [sb_([64, 4], F32, "WCT%d" % i) for i in range(2)]
    H1 = [[sb_([64, 4, 64], F32, "H1_%d_%d" % (i, h)) for h in range(NH)] for i in range(1)]
    SvT = [sb_([64, 4, NH, 64], F32, "SvT%d" % i) for i in range(1)]
    ST = [sb_([64, NH, 64], F32, "ST%d" % i) for i in range(2)]
    y_t = f32t("y_t", 1)
    s1 = sb_([128, 8], F32, "s1")
    s2 = sb_([128, 8], F32, "s2")
    o_t = f32t("o_t", 1)
    ysq = o_t

    psG = [P.psum([128, 512], F32, "psG%d" % i) for i in range(2)]
    psY = [P.psum([128, 512], F32, "psY%d" % i) for i in range(1)]
    psS = P.psum([128, 512], F32, "psS")
    B0, B1, B2, B3 = [P.psum([128, 512], F32, "B%d" % i) for i in range(4)]


    def bc8(tile8):
        return View(tile8, tile8.tensor[:, :].unsqueeze(2).to_broadcast([128, 8, 64]))

    def v3(view):
        return view.rearrange("p (h j) -> p h j", h=8)

    gi = 0
    for grp in groups:
        wr, wk, wv, w2, a2, g2, vecs = (grp[n] for n in ("wr", "wk", "wv", "w2", "a2", "g2", "vecs"))
        for ci, (nm, src, ncol) in enumerate((("r", wr, 512), ("k", wk, 512), ("v", wv, 512),
                                              ("w1", w1, 64), ("a1", a1, 64), ("g1", g1, 128))):
            for k in range(8):
                wk_ = wstl[(ci * 8 + k) % 4]
                P.dma(wk_[:, 0:ncol], src[k * 128:(k + 1) * 128, :], lane="sp")
                P.dve("tensor_scalar", out=Wb[nm][:, k, :], in0=wk_[:, 0:ncol], scalar1=mut[:, k, ci:ci + 1],
                      scalar2=None, op0=ALU.mult)
                P.act("mul", out=Wa[nm][:, k, :], in_=wk_[:, 0:ncol], mul=omut[:, k, ci:ci + 1])
        P.dma(w2b.v(), w2, lane="pool")
        P.dma(a2b.v(), a2, lane="pool")
        P.dma(g2b.v(), g2, lane="pool")
        for i in range(7):
            P.dma(vt[i].v(), View(vecs[i], vecs[i].tensor.broadcast_to([128, 512])), lane="sp")
        P.op("pool", "memset", extra_writes=[ST[0]], ap=ST[0].v().ap, constant=0.0)
        st_cur = 0
        for sb in range(nsb):
            q = sb % 4
            g = sb // 4
            gb = 0
            pb = 0
            yb = 0
            t0 = sb * 128
            if q == 0:
                hgt = hg[gb]
                for k in range(8):
                    if g == 0:
                        P.op("pool", "memset", extra_writes=[hgt], ap=hgt[:, k, 0:1].ap, constant=0.0)
                        P.dma(hgt[:, k, 1:513], hT[k * 128:(k + 1) * 128, 0:512], lane="pool")
                    else:
                        P.dma(hgt[:, k, 0:513], hT[k * 128:(k + 1) * 128, g * 512 - 1:(g + 1) * 512], lane="pool")
                for nm, dst, fn, rows in (("w1", th[gb], AF.Tanh, 64), ("a1", xa[gb], None, 64), ("g1", sgT[gb], AF.Sigmoid, 128)):
                    pp = psG[gi % 2]
                    gi += 1
                    for k in range(8):
                        P.mm(pp[0:rows, :], Wa[nm][:, k, :], hgt[:, k, 1:513], start=(k == 0), stop=False)
                        P.mm(pp[0:rows, :], Wb[nm][:, k, :], hgt[:, k, 0:512], start=False, stop=(k == 7))
                    if fn is None:
                        P.dve("tensor_copy", out=dst.v(), in_=pp[0:rows, :])
                    else:
                        P.act("activation", out=dst.v(), in_=pp[0:rows, :], func=fn)
            hgt = hg[gb]
            cur = lambda k: hgt[:, k, 1 + q * 128:1 + (q + 1) * 128]
            prv = lambda k: hgt[:, k, q * 128:(q + 1) * 128]

            def proj(nm):
                nonlocal gi
                pp = psG[gi % 2]
                gi += 1
                for k in range(8):
                    P.mm(pp.v(), cur(k), Wa[nm][:, k, :], start=(k == 0), stop=False)
                    P.mm(pp.v(), prv(k), Wb[nm][:, k, :], start=False, stop=(k == 7))
                return pp

            def nextps():
                nonlocal gi
                pp = psG[gi % 2]
                gi += 1
                return pp
            pp = proj("r")
            P.act("copy", out=r_t[pb].v(), in_=pp.v())
            pp = proj("k")
            P.act("copy", out=k_t[0].v(), in_=pp.v())
            pp = proj("v")
            P.act("copy", out=V_t[pb].v(), in_=pp.v())
            if sb == 0:
                dump(r_t[0].v(), 0); dump(k_t[0].v(), 1); dump(V_t[0].v(), 2)
            pp = nextps()
            P.mm(pp.v(), th[gb][:, q * 128:(q + 1) * 128], w2b.v())
            P.dve("tensor_tensor", out=tmpA[0].v(), in0=pp.v(), in1=W0.v(), op=ALU.add)
            P.act("activation", out=sgd[0].v(), in_=tmpA[0].v(), func=AF.Sigmoid)
            if sb == 0:
                dump(sgd[0].v(), 3)
            pp = nextps()
            P.mm(pp.v(), xa[gb][:, q * 128:(q + 1) * 128], a2b.v())
            P.dve("tensor_tensor", out=tmpA[0].v(), in0=pp.v(), in1=A0.v(), op=ALU.add)
            P.act("activation", out=a_t[0].v(), in_=tmpA[0].v(), func=AF.Sigmoid)
            pp = nextps()
            P.mm(pp.v(), sgT[gb][:, q * 128:(q + 1) * 128], g2b.v())
            P.act("copy", out=g_t[pb].v(), in_=pp.v())
            pl = nextps()
            P.mm(pl.v(), triBD.v(), sgd[0].v())
            P.act("copy", out=lwc[0].v(), in_=pl.v())
            if sb == 0:
                dump(lwc[0].v(), 4); dump(a_t[0].v(), 5); dump(g_t[0].v(), 6)
            pe_ = nextps()
            P.mm(pe_.v(), blkBD.v(), sgd[0].v())
            P.act("activation", out=E5[pb].v(), in_=pe_.v(), func=AF.Exp)
            P.dve("tensor_tensor", out=tmpB[0].v(), in0=pe_.v(), in1=lwc[0].v(), op=ALU.subtract)
            P.dve("scalar_tensor_tensor", out=tmpA[0].v(), in0=sgd[0].v(), scalar=-CNEG, in1=lwc[0].v(),
                  op0=ALU.mult, op1=ALU.add)
            P.act("activation", out=tmpA[0].v(), in_=tmpA[0].v(), func=AF.Exp)
            P.act("activation", out=E4[0].v(), in_=tmpB[0].v(), func=AF.Exp)
            P.act("activation", out=E2[0].v(), in_=lwc[0].v(), func=AF.Exp, scale=-1.0)
            P.act("activation", out=lwc[0].v(), in_=lwc[0].v(), func=AF.Exp)
            P.dve("tensor_tensor", out=kk_t[0].v(), in0=k_t[0].v(), in1=KK_.v(), op=ALU.mult)
            P.dve("tensor_tensor", out=tmpB[0].v(), in0=kk_t[0].v(), in1=kk_t[0].v(), op=ALU.mult)
            P.dve("tensor_reduce", out=ss8.v(), in_=v3(tmpB[0].v()), axis=AX.X, op=ALU.add)
            P.act("activation", out=ss8.v(), in_=ss8.v(), func=AF.Sqrt)
            P.dve("tensor_scalar", out=ss8.v(), in0=ss8.v(), scalar1=1e-12, scalar2=None, op0=ALU.max)
            P.dve("reciprocal", out=ss8.v(), in_=ss8.v())
            P.dve("tensor_tensor", out=v3(kk_t[0].v()), in0=v3(kk_t[0].v()), in1=bc8(ss8), op=ALU.mult)
            P.dve("scalar_tensor_tensor", out=kp_t[0].v(), in0=a_t[0].v(), scalar=-1.0, in1=KA_.v(),
                   op0=ALU.add, op1=ALU.mult)
            P.dve("scalar_tensor_tensor", out=kp_t[0].v(), in0=kp_t[0].v(), scalar=1.0, in1=k_t[0].v(),
                   op0=ALU.add, op1=ALU.mult)
            P.dve("tensor_tensor", out=ka_t[0].v(), in0=kk_t[0].v(), in1=a_t[0].v(), op=ALU.mult)
            P.dve("scalar_tensor_tensor", out=At[pb].v(), in0=kk_t[0].v(), scalar=-1.0, in1=tmpA[0].v(),
                  op0=ALU.mult, op1=ALU.mult)
            P.dve("tensor_tensor", out=Bt[pb].v(), in0=ka_t[0].v(), in1=E2[0].v(), op=ALU.mult)
            P.dve("tensor_tensor", out=Kt[pb].v(), in0=kp_t[0].v(), in1=E2[0].v(), op=ALU.mult)
            P.dve("tensor_tensor", out=Rt[pb].v(), in0=r_t[pb].v(), in1=lwc[0].v(), op=ALU.mult)
            P.dve("tensor_tensor", out=Bh[pb].v(), in0=ka_t[0].v(), in1=E4[0].v(), op=ALU.mult)
            P.dve("tensor_tensor", out=Kh[pb].v(), in0=kp_t[0].v(), in1=E4[0].v(), op=ALU.mult)
            if sb == 0:
                dump(At[0].v(), 7); dump(Bt[0].v(), 8); dump(Kt[0].v(), 9); dump(Rt[0].v(), 10)
                dump(Bh[0].v(), 11); dump(Kh[0].v(), 12); dump(E5[0].v(), 13); dump(kk_t[0].v(), 14); dump(kp_t[0].v(), 15)
            for c in range(4):
                P.act("mul", out=Vm[pb][:, :, c, :], in_=v3(V_t[pb].v()), mul=RMexp[:, c, 0:1])
            P.dve("tensor_tensor", out=tmpB[0].v(), in0=r_t[pb].v(), in1=kp_t[0].v(), op=ALU.mult)
            P.dve("tensor_tensor", out=tmpB[0].v(), in0=tmpB[0].v(), in1=RK.v(), op=ALU.mult)
            P.dve("tensor_reduce", out=rk8.v(), in_=v3(tmpB[0].v()), axis=AX.X, op=ALU.add)
            P.dve("tensor_tensor", out=v3(bon[pb].v()), in0=v3(V_t[pb].v()), in1=bc8(rk8), op=ALU.mult)

            pY = psY[yb]
            def head_gen(h, BX, BY):
                u = h % 2
                hs = slice(h * 64, (h + 1) * 64)
                P.pe("transpose", out=BX[0:64, 0:128], in_=At[pb][:, hs], identity=identf.v())
                P.pe("transpose", out=BX[0:64, 128:256], in_=Rt[pb][:, hs], identity=identf.v())
                P.pe("transpose", out=BX[0:64, 256:384], in_=Bt[pb][:, hs], identity=identf.v())
                P.pe("transpose", out=BX[0:64, 384:512], in_=Kt[pb][:, hs], identity=identf.v())
                P.act("copy", out=TT[u][:, 0:256], in_=BX[0:64, 0:256])
                P.dve("tensor_copy", out=TT[u][:, 256:512], in_=BX[0:64, 256:512])
                yield
                P.mm(BY[:, 0:256], TT[u][:, 256:384], TT[u][:, 0:256])
                P.mm(BY[:, 256:512], TT[u][:, 384:512], TT[u][:, 0:256])
                P.mm(BX[:, 0:256], TT[u][:, 0:128], TT[u][:, 256:512])
                P.mm(BX[0:64, 256:260], E5[pb][:, hs], Sel.v())
                pk = PP[u][0]
                xx = XX[u][0]
                P.dve("tensor_tensor", out=pk[:, 0:128], in0=BY[:, 0:128], in1=mAB[:, 0:128], op=ALU.mult)
                P.dve("tensor_tensor", out=xx[:, 0:128], in0=BY[:, 128:256], in1=mAB[:, 128:256], op=ALU.mult)
                P.dve("tensor_tensor", out=KrKh[u][:, 0:128], in0=BY[:, 384:512], in1=mAB[:, 128:256], op=ALU.mult)
                P.act("copy", out=WCT[u].v(), in_=BX[0:64, 256:260])
                P.dve("tensor_tensor", out=pk[:, 128:256], in0=BX[:, 0:128], in1=mC[:, 0:128], op=ALU.mult)
                P.dve("tensor_tensor", out=AkaT[u].v(), in0=BX[:, 128:256], in1=mC[:, 128:256], op=ALU.mult)
                P.act("copy", out=xx[:, 128:192], in_=Bh[pb][:, hs])
                P.act("copy", out=KrKh[u][:, 128:192], in_=Kh[pb][:, hs])
                yield
                cp = 0
                for lev in range(5):
                    pk = PP[u][cp]
                    xx = XX[u][cp]
                    xn = XX[u][1 - cp]
                    P.mm(BX[:, 0:192], pk[:, 128:256], xx.v())
                    if lev < 4:
                        pn = PP[u][1 - cp]
                        P.mm(BY[:, 0:128], pk[:, 128:256], pk[:, 0:128])
                        P.mm(BY[:, 128:256], pk[:, 0:128], pk[:, 128:256])
                    P.dve("tensor_tensor", out=xn.v(), in0=BX[:, 0:192], in1=xx.v(), op=ALU.add)
                    if lev < 4:
                        P.act("copy", out=pn.v(), in_=BY[:, 0:256])
                    cp = 1 - cp
                    yield
                P.act("copy", out=XF32[u].v(), in_=XX[u][cp].v())
                xf = XF32[u]
                P.mm(BX[0:64, 256:384], At[pb][:, hs], xf[:, 0:128])
                P.mm(BY[:, 0:192], AkaT[u].v(), xf.v())
                P.dve("tensor_tensor", out=X2m[u].v(),
                       in0=View(xf, xf.tensor[:, 128:192].unsqueeze(1).to_broadcast([128, 4, 64])),
                       in1=RMexp.v(), op=ALU.mult)
                P.dve("tensor_tensor", out=G1[u].v(), in0=BX[0:64, 256:384], in1=TT[u][:, 128:256], op=ALU.add)
                P.dve("tensor_tensor", out=G2H2[u].v(), in0=BY[:, 0:192], in1=KrKh[u].v(), op=ALU.add)
                P.dve("tensor_tensor", out=G1pad[pb][h].v(),
                       in0=View(G1[u], G1[u].tensor[:, :].unsqueeze(1).to_broadcast([64, 4, 128])),
                       in1=CM.v(), op=ALU.mult)
                yield
                P.mm(BX[0:64, 0:256], At[pb][:, hs], X2m[u].v().rearrange("p c j -> p (c j)"))
                P.mm(pY[:, hs], G2H2[u][:, 0:128], V_t[pb][:, hs], start=(h == 0), stop=False)
                P.mm(BY[0:64, 256:512], G2H2[u][:, 128:192], Vm[pb][:, h, :, :].rearrange("p c i -> p (c i)"))
                for c in range(4):
                    P.dve("scalar_tensor_tensor", out=H1[pb][h][:, c, :], in0=identf[0:64, 0:64],
                          scalar=WCT[u][:, c:c + 1], in1=BX[0:64, c * 64:(c + 1) * 64],
                          op0=ALU.mult, op1=ALU.add)
                P.act("copy", out=SvT[pb][:, :, h, :], in_=BY[0:64, 256:512].rearrange("p (c i) -> p c i", c=4))

            banks = [(B0, B1), (B2, B3)]
            active = [head_gen(0, *banks[0]), head_gen(1, *banks[1])]
            next_h = 2
            while any(g is not None for g in active):
                for slot in range(2):
                    g = active[slot]
                    if g is None:
                        continue
                    try:
                        next(g)
                    except StopIteration:
                        if next_h < NH:
                            assert next_h % 2 == slot
                            active[slot] = head_gen(next_h, *banks[slot])
                            next_h += 1
                            next(active[slot])
                        else:
                            active[slot] = None

            for c in range(4):
                stc = ST[st_cur]
                stn = ST[1 - st_cur]
                for h in range(NH):
                    hs = slice(h * 64, (h + 1) * 64)
                    P.mm(pY[:, hs], G1pad[pb][h][:, c, :], stc[:, h, :], start=False, stop=(c == 3))
                    P.mm(psS[0:64, hs], H1[pb][h][:, c, :], stc[:, h, :])
                P.dve("tensor_tensor", out=stn.v().rearrange("p h i -> p (h i)"), in0=psS[0:64, :],
                      in1=SvT[pb][:, c, :, :].rearrange("p h i -> p (h i)"), op=ALU.add)
                st_cur = 1 - st_cur

            P.act("copy", out=y_t[0].v(), in_=pY.v())
            if sb == 0:
                dump(y_t[0].v(), 16); dump(bon[0].v(), 17)
                dump(ST[st_cur].v().rearrange("p h i -> p (h i)"), 18) if False else None
            P.dve("tensor_reduce", out=s1.v(), in_=v3(y_t[0].v()), axis=AX.X, op=ALU.add)
            P.dve("tensor_tensor", out=ysq[0].v(), in0=y_t[0].v(), in1=y_t[0].v(), op=ALU.mult)
            P.dve("tensor_reduce", out=s2.v(), in_=v3(ysq[0].v()), axis=AX.X, op=ALU.add)
            P.dve("tensor_scalar", out=s1.v(), in0=s1.v(), scalar1=1.0 / 64, scalar2=None, op0=ALU.mult)
            P.dve("tensor_scalar", out=s2.v(), in0=s2.v(), scalar1=1.0 / 64, scalar2=GN_EPS, op0=ALU.mult, op1=ALU.add)
            P.dve("tensor_tensor", out=rk8.v(), in0=s1.v(), in1=s1.v(), op=ALU.mult)
            P.dve("tensor_tensor", out=s2.v(), in0=s2.v(), in1=rk8.v(), op=ALU.subtract)
            P.act("activation", out=s2.v(), in_=s2.v(), func=AF.Sqrt)
            P.dve("reciprocal", out=s2.v(), in_=s2.v())
            P.dve("tensor_tensor", out=v3(y_t[0].v()), in0=v3(y_t[0].v()), in1=bc8(s1), op=ALU.subtract)
            P.dve("tensor_tensor", out=v3(y_t[0].v()), in0=v3(y_t[0].v()), in1=bc8(s2), op=ALU.mult)
            P.dve("tensor_tensor", out=y_t[0].v(), in0=y_t[0].v(), in1=GNG.v(), op=ALU.mult)
            P.dve("tensor_tensor", out=y_t[0].v(), in0=y_t[0].v(), in1=GNB.v(), op=ALU.add)
            P.dve("tensor_tensor", out=y_t[0].v(), in0=y_t[0].v(), in1=bon[pb].v(), op=ALU.add)
            P.dve("tensor_tensor", out=o_t[pb].v(), in0=y_t[0].v(), in1=g_t[pb].v(), op=ALU.mult)
            if "X" in grp:
                P.dma(grp["X"][t0:t0 + 128, :], o_t[pb].v(), lane="sp", final=True)
            else:
                pto = psG[gi % 2]
                gi += 1
                for k4 in range(4):
                    P.pe("transpose", out=pto[:, k4 * 128:(k4 + 1) * 128], in_=o_t[pb][:, k4 * 128:(k4 + 1) * 128],
                         identity=identf.v())
                P.act("copy", out=y_t[0].v(), in_=pto.v())
                for k4 in range(4):
                    P.dma(rows_view(grp["XT"], k4 * 128, 128)[:, t0:t0 + 128], y_t[0][:, k4 * 128:(k4 + 1) * 128],
                          lane="sp", final=True)


def build_RW(nc, nsb=NSB):
    P = Prog(nc)
    hT = P.dram("hT", [D, S], F32, "ExternalInput")
    muT = P.dram("muT", [128, 8, 6], F32, "ExternalInput")
    wr = P.dram("wr", [D, 512], F32, "ExternalInput")
    wk = P.dram("wk", [D, 512], F32, "ExternalInput")
    wv = P.dram("wv", [D, 512], F32, "ExternalInput")
    w1 = P.dram("w1", [D, 64], F32, "ExternalInput")
    a1 = P.dram("a1", [D, 64], F32, "ExternalInput")
    g1 = P.dram("g1", [D, 128], F32, "ExternalInput")
    w2 = P.dram("w2", [64, 512], F32, "ExternalInput")
    a2 = P.dram("a2", [64, 512], F32, "ExternalInput")
    g2 = P.dram("g2", [128, 512], F32, "ExternalInput")
    vecs = P.dram("vecs", [7, 512], F32, "ExternalInput")
    X = P.dram("X", [S, 512], F32, "ExternalOutput")
    grp = dict(wr=wr.v(), wk=wk.v(), wv=wv.v(), w2=w2.v(), a2=a2.v(), g2=g2.v(),
               vecs=[vecs[i:i + 1, :] for i in range(7)], X=X.v())
    body_RW(P, hT.v(), muT.v(), w1.v(), a1.v(), g1.v(), [grp], nsb)
    P.emit()
    P.close()
    return nc


_S, _D, _H = 4096, 1024, 2048
_PAIRS = [[0, 1], [2, 3], [4, 5], [6, 7]]

_IN2 = dict(
    x_half=[_H, _D], x_T=[_D, _S], sel=[128, 2],
    ln1_g=[4, _D], ln1_b=[4, _D], ln2_g=[4, _D], ln2_b=[4, _D],
    fox_wq=[_D, 512], fox_wk=[_D, 512], fox_wv=[_D, 512], fox_wog=[_D, 512], fox_wf=[_D, 8], fox_bf=[1, 8],
    fox_w_out=[_D, _D],
    gm_w_in=[_D, 2048], gm_b_in=[1, 2048], gm_ln_g=[1, _D], gm_ln_b=[1, _D],
    gm_wsT=[128, 8, 128], gm_bsT=[128, 8], gm_w_out=[_D, _D],
    sb_wq=[_D, 512], sb_wk=[_D, 512], sb_wv=[_D, 512], sb_w_out=[_D, _D],
    rw_muT=[128, 8, 6], rw_wr=[_D, 512], rw_wk=[_D, 512], rw_wv=[_D, 512], rw_w1=[_D, 64], rw_a1=[_D, 64],
    rw_g1=[_D, 128], rw_w2=[64, 512], rw_a2=[64, 512], rw_g2=[128, 512], rw_vecs=[7, 512], rw_w_out=[_D, _D],
    router_w=[_D, 16], router_b=[1, 16],
    moe_w_gate=[4, 16, _D, 512], moe_w_up=[4, 16, _D, 512], moe_w_down=[4, 16, 512, _D],
)


def build_fused2(nc, layers=(0, 1, 2, 3)):
    P = Prog(nc)
    I = {k: P.dram(k, shp, F32, "ExternalInput").v() for k, shp in _IN2.items()}
    out = P.dram("out", [_H, _D], F32, "ExternalOutput").v()
    xgi = [P.dram("xgi%d" % j, [128, _S], F32, "Internal").v() for j in range(4)]
    xgo = [P.dram("xgo%d" % j, [256, _S], F32, "Internal").v() for j in range(4)]
    xg_in = RowBlocks(xgi, 128)
    xblocks = [xgo[k % 4][(k // 4) * 128:(k // 4 + 1) * 128, :] for k in range(8)]
    h_d = P.dram("h_d", [_H, _D], F32, "Internal").v()
    hTo = [P.dram("hTo%d" % j, [256, _H], F32, "Internal").v() for j in range(4)]
    hTg = [P.dram("hTg%d" % j, [512, _H], F32, "Internal").v() for j in range(4)]
    hT_own = RowBlocks(hTo, 256)
    xT_own = P.dram("xT_own", [_D, _H], F32, "Internal").v()
    hT_full = P.dram("hT_full", [_D, _S], F32, "Internal").v()

    def gather_x():
        P.barrier()
        for j in range(4):
            P.all_gather(xgo[j], xgi[j], _PAIRS)

    def gather_h():
        P.barrier()
        for j in range(4):
            P.all_gather(hTg[j], hTo[j], _PAIRS)
        for j in range(4):
            for r in range(2):
                P.dma(hT_full[j * 256:(j + 1) * 256, r * _H:(r + 1) * _H], hTg[j][r * 256:(r + 1) * 256, :], lane="sp")

    def t_stage(L, wout, first, last, blend):
        io = dict(hin=(I["x_half"] if first else h_d), wout=wout,
                  ln=[I["ln1_g"][L:L + 1, :], I["ln1_b"][L:L + 1, :], I["ln2_g"][L:L + 1, :], I["ln2_b"][L:L + 1, :]],
                  rw=I["router_w"], rb=I["router_b"], wg=I["moe_w_gate"][L], wu=I["moe_w_up"][L],
                  wd=I["moe_w_down"][L], hout=(out if last else h_d))
        if blend:
            io["xT_blend"] = (xblocks, I["sel"])
        else:
            io["xT"] = xT_own
        if not last:
            io["houtT"] = hT_own
        P.begin_stage()
        body_T(P, io, _H)
        if (not last) and L != 0:
            gather_h()
        P.end_stage(last=last)

    nl = len(layers)
    for li, L in enumerate(layers):
        first, last = (li == 0), (li == nl - 1)
        hT = I["x_T"] if first else hT_full
        P.begin_stage()
        if L == 0:
            grp = dict(wq=I["fox_wq"], wk=I["fox_wk"], wv=I["fox_wv"], wog=I["fox_wog"], wf=I["fox_wf"],
                       bf=I["fox_bf"], XT=xg_in)
            body_FOX(P, hT, [grp])
            gather_x()
            wout = I["fox_w_out"]
        elif L == 1:
            body_GM(P, dict(hT=(hT_own if not first else None), w_in=I["gm_w_in"], b_in=I["gm_b_in"],
                            ln=[I["gm_ln_g"], I["gm_ln_b"]], wsT=I["gm_wsT"], bsT=I["gm_bsT"], XT=xT_own), _H)
            wout = I["gm_w_out"]
        elif L == 2:
            body_SB(P, hT, [dict(wq=I["sb_wq"], wk=I["sb_wk"], wv=I["sb_wv"], XT=xg_in)])
            gather_x()
            wout = I["sb_w_out"]
        else:
            grp = dict(wr=I["rw_wr"], wk=I["rw_wk"], wv=I["rw_wv"], w2=I["rw_w2"], a2=I["rw_a2"], g2=I["rw_g2"],
                       vecs=[I["rw_vecs"][i:i + 1, :] for i in range(7)], XT=xg_in)
            body_RW(P, hT, I["rw_muT"], I["rw_w1"], I["rw_a1"], I["rw_g1"], [grp])
            gather_x()
            wout = I["rw_w_out"]
        P.end_stage()
        t_stage(L, wout, first, last, blend=(L != 1))
    P.close()
    return nc


def fused2_inputs(inp, c):
    b, r = c // 2, c % 2
    f = lambda a: np.ascontiguousarray(a, dtype=np.float32)
    cs = slice(r * 512, (r + 1) * 512)
    m = dict(x_half=f(inp["x"][b][r * _H:(r + 1) * _H]), x_T=f(inp["x"][b].T))
    sel = np.zeros((128, 2), np.float32)
    sel[:, r] = 1.0
    m["sel"] = sel
    for k in ("ln1_g", "ln1_b", "ln2_g", "ln2_b", "router_w", "moe_w_gate", "moe_w_up", "moe_w_down"):
        m[k] = f(inp[k])
    m["router_b"] = f(inp["router_b"]).reshape(1, 16)
    W = inp["fox_w_in"][0]
    m["fox_wq"] = f(W[:, 0:1024][:, cs]); m["fox_wk"] = f(W[:, 1024:2048][:, cs]); m["fox_wv"] = f(W[:, 2048:3072][:, cs])
    m["fox_wog"] = f(W[:, 3088:4112][:, cs]); m["fox_wf"] = f(W[:, 3072 + r * 8:3072 + (r + 1) * 8])
    m["fox_bf"] = f(inp["fox_b_f"][0][r * 8:(r + 1) * 8]).reshape(1, 8)
    for k in ("fox_w_out", "gm_w_in", "gm_w_out", "sb_w_out", "rw_w1", "rw_a1", "rw_g1", "rw_w_out"):
        m[k] = f(inp[k][0])
    m["gm_b_in"] = f(inp["gm_b_in"][0]).reshape(1, -1)
    m["gm_ln_g"] = f(inp["gm_ln_g"][0]).reshape(1, -1); m["gm_ln_b"] = f(inp["gm_ln_b"][0]).reshape(1, -1)
    m["gm_wsT"] = f(inp["gm_w_s"][0].transpose(2, 0, 1)); m["gm_bsT"] = f(inp["gm_b_s"][0].T)
    W = inp["sb_w_in"][0]
    m["sb_wq"] = f(W[:, 0:1024][:, cs]); m["sb_wk"] = f(W[:, 1024:2048][:, cs]); m["sb_wv"] = f(W[:, 2048:3072][:, cs])
    m["rw_muT"] = f(inp["rw_mu"][0].T.reshape(8, 128, 6).transpose(1, 0, 2))
    W = inp["rw_w_rkv"][0]
    m["rw_wr"] = f(W[0][:, cs]); m["rw_wk"] = f(W[1][:, cs]); m["rw_wv"] = f(W[2][:, cs])
    m["rw_w2"] = f(inp["rw_w2"][0][:, cs]); m["rw_a2"] = f(inp["rw_a2"][0][:, cs]); m["rw_g2"] = f(inp["rw_g2"][0][:, cs])
    m["rw_vecs"] = f(np.stack([inp["rw_w0"][0][cs], inp["rw_a0"][0][cs], inp["rw_k_k"][0][cs], inp["rw_k_a"][0][cs],
                               inp["rw_gn_g"][0][cs], inp["rw_gn_b"][0][cs], inp["rw_r_k"][0].reshape(-1)[cs]]))
    return m


from concourse.bass_utils import run_bass_kernel_spmd


def kernel(**inp):
    inp = {k: np.asarray(v) for k, v in inp.items()}
    nc = bass.Bass("TRN2", target_bir_lowering=False, num_devices=8)
    build_fused2(nc)
    maps = [fused2_inputs(inp, c) for c in range(8)]
    res = run_bass_kernel_spmd(nc, maps, core_ids=list(range(8))).results
    out = np.stack([np.concatenate([res[2 * b]["out"], res[2 * b + 1]["out"]], 0) for b in range(4)], 0)
    return out.astype(np.float32)
```

```python
import numpy as np
import concourse.bass as bass
import concourse.mybir as mybir
from contextlib import ExitStack

F32 = mybir.dt.float32
BF16 = mybir.dt.bfloat16
AF = mybir.ActivationFunctionType
ALU = mybir.AluOpType
AX = mybir.AxisListType

SAME_SYNC_DEFAULT = True
ENGS = ["pe", "dve", "act", "pool", "sp"]
_WRITE_KEYS = ("out", "accum_out", "out_max", "out_indices")


class Tile:
    def __init__(self, tensor, name):
        self.tensor = tensor
        self.name = name
        self.is_psum = False
        self.last_w = None
        self.readers = {}

    def __getitem__(self, idx):
        return View(self, self.tensor[idx])

    def v(self):
        return View(self, self.tensor[:])


class View:
    def __init__(self, tile, ap):
        if isinstance(tile, View):
            tile = tile.tile
        self.tile = tile
        self.ap = ap

    @property
    def tensor(self):
        return self.ap

    def v(self):
        return self

    def __getitem__(self, idx):
        return View(self.tile, self.ap[idx])

    def rearrange(self, s, **kw):
        return View(self.tile, self.ap.rearrange(s, **kw))

    def bitcast(self, dt):
        return View(self.tile, self.ap.bitcast(dt))

    def to_broadcast(self, shape):
        return View(self.tile, self.ap.to_broadcast(shape))

    def broadcast_to(self, shape):
        return View(self.tile, self.ap.broadcast_to(shape))

    def unsqueeze(self, a):
        return View(self.tile, self.ap.unsqueeze(a))

    def partition_broadcast(self, n):
        return View(self.tile, self.ap.partition_broadcast(n))


class RowBlocks:
    def __init__(self, views, bs):
        self.views, self.bs = views, bs

    def rows(self, r0, n):
        blk = self.views[r0 // self.bs]
        o = r0 % self.bs
        assert o + n <= self.bs
        return blk[o:o + n, :]


def rows_view(X, r0, n):
    return X.rows(r0, n) if isinstance(X, RowBlocks) else X[r0:r0 + n, :]


class Prog:
    def __init__(self, nc, same_engine_sync=SAME_SYNC_DEFAULT, n_dma_sems=12):
        self.nc = nc
        self.es = ExitStack()
        self.ops = {e: [] for e in ENGS}
        self.count = {e: 0 for e in ENGS}
        self.waited = {e: {} for e in ENGS}
        self.same_engine_sync = same_engine_sync
        self.sems = {}
        for e in ENGS:
            self.sems[("eng", e)] = self.es.enter_context(nc.semaphore("s_" + e))
        self.n_dma_sems = n_dma_sems
        self.dma_k = {}
        for lane in ("sp", "pool", "act"):
            self.dma_k[lane] = 0
            for i in range(n_dma_sems):
                self.sems[("dma", lane, i)] = self.es.enter_context(
                    nc.semaphore("d_%s_%d" % (lane, i)))
        self.sems[("cc",)] = self.es.enter_context(nc.semaphore("s_cc"))
        self.cc_count = 0
        self.n_tiles = 0
        self.final_tokens = []
        self.ses = None
        self.stage_id = 0

    def begin_stage(self):
        self.ses = ExitStack()
        self.stage_id += 1

    def end_stage(self, last=False):
        self.barrier()
        self.emit(last=last)
        self.ses.close()
        self.ses = None

    def sbuf(self, shape, dt, name=None):
        self.n_tiles += 1
        name = name or ("t%d" % self.n_tiles)
        if self.ses is not None:
            name = "s%d_%s" % (self.stage_id, name)
        t = (self.ses or self.es).enter_context(self.nc.sbuf_tensor(name, list(shape), dt))
        return Tile(t, name)

    def psum(self, shape, dt, name=None):
        self.n_tiles += 1
        name = name or ("p%d" % self.n_tiles)
        if self.ses is not None:
            name = "s%d_%s" % (self.stage_id, name)
        t = (self.ses or self.es).enter_context(self.nc.psum_tensor(name, list(shape), dt))
        tl = Tile(t, name)
        tl.is_psum = True
        return tl

    def dram(self, name, shape, dt, kind):
        t = self.nc.dram_tensor(name, list(shape), dt, kind=kind)
        return Tile(t.ap(), name)

    def subtile(self, tile, idx, name=None):
        return Tile(tile.tensor[idx], name or (tile.name + "_sub"))

    def _deps(self, eng, reads, writes, skip_same=False):
        deps = {}

        def add(tok):
            if tok is None:
                return
            k, v = tok
            if deps.get(k, 0) < v:
                deps[k] = v

        for t in reads:
            add(t.last_w)
        for t in writes:
            add(t.last_w)
            for k, v in t.readers.items():
                add((k, v))
        out = []
        for k, v in deps.items():
            if k == ("eng", eng):
                if skip_same:
                    continue
            if self.waited[eng].get(k, 0) >= v:
                continue
            self.waited[eng][k] = v
            out.append((k, v))
        return out

    def _commit(self, tok, reads, writes):
        k, v = tok
        for t in writes:
            t.last_w = tok
            t.readers = {}
        for t in reads:
            if t in writes:
                continue
            if t.readers.get(k, 0) < v:
                t.readers[k] = v

    def _split(self, kwargs):
        reads, writes, real = [], [], {}
        for k, a in kwargs.items():
            if isinstance(a, View):
                (writes if k in _WRITE_KEYS else reads).append(a.tile)
                real[k] = a.ap
            else:
                real[k] = a
        return reads, writes, real

    def op(self, eng, method, extra_reads=(), extra_writes=(), **kwargs):
        reads, writes, real = self._split(kwargs)
        reads += [v.tile if isinstance(v, View) else v for v in extra_reads]
        writes += [v.tile if isinstance(v, View) else v for v in extra_writes]
        if eng != "pe":
            writes += [t for t in reads if t.is_psum and t not in writes]
        waits = self._deps(eng, reads, writes,
                           skip_same=(eng == "pe" or not self.same_engine_sync))
        self.count[eng] += 1
        tok = (("eng", eng), self.count[eng])
        self.ops[eng].append((waits, method, real, tok, 1))
        self._commit(tok, reads, writes)
        return tok

    def dve(self, method, **kw):
        return self.op("dve", method, **kw)

    def act(self, method, **kw):
        return self.op("act", method, **kw)

    def pool(self, method, **kw):
        return self.op("pool", method, **kw)

    def pe(self, method, **kw):
        return self.op("pe", method, **kw)

    def mm(self, out, lhsT, rhs, start=True, stop=True, **kw):
        return self.op("pe", "matmul", out=out, lhsT=lhsT, rhs=rhs, start=start, stop=stop, **kw)

    def dma(self, out, in_, lane="sp", final=False, **kw):
        reads, writes = [in_.tile], [out.tile]
        k = self.dma_k[lane]
        self.dma_k[lane] += 1
        si = k % self.n_dma_sems
        semkey = ("dma", lane, si)
        waits = self._deps(lane, reads, writes)
        prev = 16 * (k // self.n_dma_sems)
        if prev > 0 and self.waited[lane].get(semkey, 0) < prev:
            self.waited[lane][semkey] = prev
            waits.append((semkey, prev))
        tok = (semkey, prev + 16)
        real = dict(out=out.ap, in_=in_.ap, **kw)
        self.ops[lane].append((waits, "dma_start", real, tok, 16))
        self._commit(tok, reads, writes)
        if final:
            self.final_tokens.append(tok)
        return tok

    def all_gather(self, out, in_, groups, inc=1):
        lane = "pool"
        reads, writes = [in_.tile], [out.tile]
        waits = self._deps(lane, reads, writes)
        self.cc_count += inc
        tok = (("cc",), self.cc_count)
        real = dict(kind="AllGather", op=ALU.bypass, replica_groups=groups, ins=[in_.ap], outs=[out.ap])
        self.ops[lane].append((waits, "collective_compute", real, tok, inc))
        self._commit(tok, reads, writes)
        return tok

    def barrier(self):
        allk = {}
        for e in ENGS:
            if self.count[e] > 0:
                allk[("eng", e)] = self.count[e]
        for lane in ("sp", "pool", "act"):
            k = self.dma_k[lane]
            for i in range(self.n_dma_sems):
                n = (k - i + self.n_dma_sems - 1) // self.n_dma_sems if k > i else 0
                if n > 0:
                    allk[("dma", lane, i)] = 16 * n
        if self.cc_count > 0:
            allk[("cc",)] = self.cc_count
        for e in ENGS:
            waits = []
            for k, v in allk.items():
                if self.waited[e].get(k, 0) >= v:
                    continue
                self.waited[e][k] = v
                waits.append((k, v))
            if waits:
                self.ops[e].append((waits, None, None, None, 0))

    def emit(self, last=True):
        nc = self.nc
        fin = []
        seen = {}
        for k, v in self.final_tokens:
            if seen.get(k, 0) < v:
                seen[k] = v
        fin = list(seen.items())
        engobj = {"pe": "tensor", "dve": "vector", "act": "scalar", "pool": "gpsimd", "sp": "sync"}
        with nc.Block() as block:
            for e in ENGS:
                ops = self.ops[e]
                extra = fin if (e == "sp" and last) else []

                def body(eng, ops=ops, extra=extra):
                    for waits, method, real, tok, inc in ops:
                        for k, v in waits:
                            eng.wait_ge(self.sems[k], v)
                        if method is None:
                            continue
                        ins = getattr(eng, method)(**real)
                        ins.then_inc(self.sems[tok[0]], inc)
                    for k, v in extra:
                        eng.wait_ge(self.sems[k], v)

                if not ops and not extra:
                    continue
                getattr(block, engobj[e])(body)
        self.ops = {e: [] for e in ENGS}

    def close(self):
        self.es.close()


D = 1024
NE = 16
DE = 512
ALPHA = 8 ** 0.25
LN_EPS = 1e-5


def layer_norm_tile(P, x, out, g_t, b_t, stats, mv, rstd, tmp, eps=LN_EPS):
    for c in range(2):
        P.dve("bn_stats", out=stats[:, c, :], in_=x[:, c * 512:(c + 1) * 512])
    P.dve("bn_aggr", out=mv.v(), in_=stats.v())
    P.dve("tensor_scalar", out=rstd.v(), in0=mv[:, 1:2], scalar1=eps, scalar2=None, op0=ALU.add)
    P.act("activation", out=rstd.v(), in_=rstd.v(), func=AF.Sqrt)
    P.dve("reciprocal", out=rstd.v(), in_=rstd.v())
    P.dve("tensor_scalar", out=out, in0=x, scalar1=mv[:, 0:1], scalar2=rstd[:, 0:1],
          op0=ALU.subtract, op1=ALU.mult)
    P.dve("tensor_tensor", out=out, in0=out, in1=g_t, op=ALU.mult)
    P.dve("tensor_tensor", out=out, in0=out, in1=b_t, op=ALU.add)


def body_T(P, io, TOK=2048, stop=99, sub=99):
    nc = P.nc
    NT = TOK // 128
    NC4 = TOK // 512
    hin, xT, wout, rw, rb = io["hin"], io.get("xT"), io["wout"], io["rw"], io["rb"]
    wg, wu, wd, hout = io["wg"], io["wu"], io["wd"], io["hout"]
    lnrows = io["ln"]
    houtT = io.get("houtT")

    acc = P.sbuf([128, NT, D], F32, "acc")
    acc_t = [P.subtile(acc, (slice(None), t, slice(None)), "acc%d" % t) for t in range(NT)]
    h1T = P.sbuf([128, 8, TOK], BF16, "h1T")
    arena = P.sbuf([128, 24576], BF16, "arena")
    lnt = [P.sbuf([128, D], F32, "lnt%d" % i) for i in range(4)]
    rwt = P.sbuf([128, 8, NE], F32, "rwt")
    rbt = P.sbuf([128, NE], F32, "rbt")
    ident = P.sbuf([128, 128], F32, "ident")
    hin_t = [P.sbuf([128, D], F32, "hin0")] * 2
    h1_t = [P.sbuf([128, D], F32, "h1_%d" % i) for i in range(2)]
    h1Tf = [P.sbuf([128, 8, 128], F32, "h1Tf0")] * 2
    tmp_t = [None, None]
    stats = [P.sbuf([128, 2, 6], F32, "st%d" % i) for i in range(2)]
    mv = [P.sbuf([128, 2], F32, "mv%d" % i) for i in range(2)]
    rstd = [P.sbuf([128, 1], F32, "rstd%d" % i) for i in range(2)]
    scores = P.sbuf([128, NT, NE], F32, "scores")
    comb = P.sbuf([128, NT, NE], F32, "comb")
    r_sel = P.sbuf([128, NT, NE], F32, "r_sel")
    r_cnt = P.sbuf([128, NT, NE], F32, "r_cnt")
    r_tmp = P.sbuf([128, NT, NE], F32, "r_tmp")
    r_gs = P.sbuf([128, NT, 4], F32, "r_gs")
    r_gm = P.sbuf([128, NT], F32, "r_gm")
    r_gmask = P.sbuf([128, NT, 4], F32, "r_gmask")
    r_den = P.sbuf([128, NT], F32, "r_den")
    heT = [[P.sbuf([128, 512], BF16, "heT%d_%d" % (b, f)) for f in range(4)] for b in range(2)]
    sg = [P.sbuf([128, 512], F32, "sg%d" % i) for i in range(2)]
    ps = [P.psum([128, 512], F32, "ps%d" % i) for i in range(8)]

    ar = arena.tensor
    woutb = Tile(ar[:, 0:8192].rearrange("p (k f) -> p k f", k=8), "woutb")
    xTb = Tile(ar[:, 8192:8192 + 8 * TOK].rearrange("p (k t) -> p k t", k=8), "xTb")
    wbuf = []
    for b in range(2):
        o = b * 12288
        wbuf.append(dict(
            g=Tile(ar[:, o:o + 4096].rearrange("p (k f) -> p k f", k=8), "wg%d" % b),
            u=Tile(ar[:, o + 4096:o + 8192].rearrange("p (k f) -> p k f", k=8), "wu%d" % b),
            d=Tile(ar[:, o + 8192:o + 12288].rearrange("p (k f) -> p k f", k=4), "wd%d" % b)))

    for i in range(4):
        P.dma(lnt[i].v(), View(lnrows[i], lnrows[i].tensor.broadcast_to([128, D])), lane="sp")
    P.dma(rwt.v(), rw.v().rearrange("(k p) e -> p k e", p=128), lane="sp")
    P.dma(rbt.v(), View(rb, rb.tensor[0:1, :].broadcast_to([128, NE])), lane="sp")
    P.op("pool", "memset", extra_writes=[ident], ap=ident.v().ap, constant=1.0)
    P.pool("affine_select", out=ident.v(), in_=ident.v(), pattern=[[-1, 128]],
           compare_op=ALU.is_equal, fill=0.0, base=0, channel_multiplier=1)
    P.dma(woutb.v(), wout.v().rearrange("(k p) f -> p k f", p=128), lane="pool")
    if "xT_blend" in io:
        xfull, sel = io["xT_blend"]
        selt = P.sbuf([128, 2], F32, "selt")
        stg = [P.sbuf([128, TOK], BF16, "stg%d" % i) for i in range(2)]
        P.dma(selt.v(), sel, lane="sp")
        for k in range(8):
            P.dma(stg[0].v(), xfull[k][:, 0:TOK], lane="pool")
            P.dma(stg[1].v(), xfull[k][:, TOK:2 * TOK], lane="pool")
            P.dve("tensor_scalar", out=stg[0].v(), in0=stg[0].v(), scalar1=selt[:, 0:1], scalar2=None, op0=ALU.mult)
            P.dve("scalar_tensor_tensor", out=xTb[:, k, :], in0=stg[1].v(), scalar=selt[:, 1:2], in1=stg[0].v(),
                  op0=ALU.mult, op1=ALU.add)
    else:
        for k in range(8):
            P.dma(xTb[:, k, :], xT[k * 128:(k + 1) * 128, :], lane="pool")

    for tt in range(NT):
        b = tt % 2
        if sub < 99 and tt > 0:
            break
        P.dma(hin_t[b].v(), hin[tt * 128:(tt + 1) * 128, :], lane="sp")
        pm = [ps[(2 * tt) % 4], ps[(2 * tt) % 4 + 1]]
        for half in range(2):
            for k in range(8):
                P.mm(pm[half].v(), xTb[:, k, tt * 128:(tt + 1) * 128],
                     woutb[:, k, half * 512:(half + 1) * 512], start=(k == 0), stop=(k == 7))
        if sub == 1:
            break
        for half in range(2):
            P.dve("scalar_tensor_tensor", out=hin_t[b][:, half * 512:(half + 1) * 512],
                  in0=hin_t[b][:, half * 512:(half + 1) * 512], scalar=ALPHA, in1=pm[half].v(),
                  op0=ALU.mult, op1=ALU.add)
        if sub == 2:
            break
        layer_norm_tile(P, hin_t[b].v(), h1_t[b].v(), lnt[0].v(), lnt[1].v(),
                        stats[b], mv[b], rstd[b], tmp_t[b])
        if sub == 3:
            break
        P.act("mul", out=acc_t[tt].v(), in_=h1_t[b].v(), mul=ALPHA)
        pt = [ps[4 + (2 * tt) % 4], ps[4 + (2 * tt) % 4 + 1]]
        for k in range(8):
            P.pe("transpose", out=pt[k // 4][:, (k % 4) * 128:(k % 4 + 1) * 128],
                 in_=h1_t[b][:, k * 128:(k + 1) * 128], identity=ident.v())
        if sub == 4:
            break
        for hf in range(2):
            P.act("copy", out=h1T[:, hf * 4:(hf + 1) * 4, tt * 128:(tt + 1) * 128],
                  in_=pt[hf].v().rearrange("p (k t) -> p k t", k=4))
            P.dve("tensor_copy", out=h1Tf[b][:, hf * 4:(hf + 1) * 4, :],
                  in_=pt[hf].v().rearrange("p (k t) -> p k t", k=4))
        if sub == 5:
            break
        pr = pm[0]
        for k in range(8):
            P.mm(pr[:, 0:NE], h1Tf[b][:, k, :], rwt[:, k, :], start=(k == 0), stop=(k == 7))
        P.act("activation", out=scores[:, tt, :], in_=pr[:, 0:NE], func=AF.Sigmoid)

    def v4(t):
        return t.v().rearrange("p t (g i) -> p t g i", g=4)
    P.dve("tensor_tensor", out=r_sel.v(), in0=scores.v(),
          in1=View(rbt, rbt.tensor[:, :].unsqueeze(1).to_broadcast([128, NT, NE])), op=ALU.add)
    P.op("dve", "memset", extra_writes=[r_cnt], ap=r_cnt.v().ap, constant=0.0)
    for j in range(4):
        selj = View(r_sel, v4(r_sel).ap[:, :, :, j:j + 1].to_broadcast([128, NT, 4, 4]))
        P.dve("tensor_tensor", out=v4(r_tmp), in0=selj, in1=v4(r_sel), op=ALU.is_gt)
        P.dve("tensor_tensor", out=r_cnt.v(), in0=r_cnt.v(), in1=r_tmp.v(), op=ALU.add)
    P.dve("tensor_single_scalar", out=r_cnt.v(), in_=r_cnt.v(), scalar=1.5, op=ALU.is_lt)
    P.dve("tensor_tensor", out=r_tmp.v(), in0=r_sel.v(), in1=r_cnt.v(), op=ALU.mult)
    P.dve("tensor_reduce", out=r_gs.v(), in_=v4(r_tmp), axis=AX.X, op=ALU.add)
    P.dve("tensor_reduce", out=r_gm.v(), in_=r_gs.v(), axis=AX.X, op=ALU.max)
    P.dve("tensor_tensor", out=r_gmask.v(), in0=r_gs.v(),
          in1=View(r_gm, r_gm.tensor[:, :].unsqueeze(2).to_broadcast([128, NT, 4])), op=ALU.is_ge)
    P.dve("tensor_tensor", out=v4(r_cnt), in0=v4(r_cnt),
          in1=View(r_gmask, r_gmask.tensor[:, :, :].unsqueeze(3).to_broadcast([128, NT, 4, 4])),
          op=ALU.mult)
    P.dve("tensor_tensor", out=r_tmp.v(), in0=scores.v(), in1=r_cnt.v(), op=ALU.mult)
    P.dve("tensor_reduce", out=r_den.v(), in_=r_tmp.v(), axis=AX.X, op=ALU.add)
    P.dve("reciprocal", out=r_den.v(), in_=r_den.v())
    P.dve("tensor_tensor", out=comb.v(), in0=r_tmp.v(),
          in1=View(r_den, r_den.tensor[:, :].unsqueeze(2).to_broadcast([128, NT, NE])), op=ALU.mult)

    P.barrier()

    it = 0
    for e in range(NE):
        wb = wbuf[e % 2]
        P.dma(wb["g"].v(), wg[e].rearrange("(k p) f -> p k f", p=128), lane="pool")
        P.dma(wb["u"].v(), wu[e].rearrange("(k p) f -> p k f", p=128), lane="pool")
        P.dma(wb["d"].v(), wd[e].rearrange("(k p) f -> p k f", p=128), lane="pool")
        for tc in range(NC4):
            hb = heT[it % 2]
            for ft in range(4):
                pg = ps[(2 * (it * 4 + ft)) % 4]
                pu = ps[(2 * (it * 4 + ft)) % 4 + 1]
                for k in range(8):
                    P.mm(pg.v(), wb["g"][:, k, ft * 128:(ft + 1) * 128],
                         h1T[:, k, tc * 512:(tc + 1) * 512], start=(k == 0), stop=(k == 7))
                for k in range(8):
                    P.mm(pu.v(), wb["u"][:, k, ft * 128:(ft + 1) * 128],
                         h1T[:, k, tc * 512:(tc + 1) * 512], start=(k == 0), stop=(k == 7))
                s_ = sg[(it * 4 + ft) % 2]
                P.act("activation", out=s_.v(), in_=pg.v(), func=AF.Silu)
                P.dve("tensor_tensor", out=hb[ft].v(), in0=s_.v(), in1=pu.v(), op=ALU.mult)
            for t4 in range(4):
                tt = tc * 4 + t4
                for half in range(2):
                    py = ps[4 + (2 * (it * 4 + t4)) % 4 + half]
                    for ft in range(4):
                        P.mm(py.v(), hb[ft][:, t4 * 128:(t4 + 1) * 128],
                             wb["d"][:, ft, half * 512:(half + 1) * 512], start=(ft == 0), stop=(ft == 3))
                    P.dve("scalar_tensor_tensor", out=acc_t[tt][:, half * 512:(half + 1) * 512],
                          in0=py.v(), scalar=comb[:, tt, e:e + 1],
                          in1=acc_t[tt][:, half * 512:(half + 1) * 512], op0=ALU.mult, op1=ALU.add)
            it += 1

    for tt in range(NT):
        b = tt % 2
        layer_norm_tile(P, acc_t[tt].v(), h1_t[b].v(), lnt[2].v(), lnt[3].v(),
                        stats[b], mv[b], rstd[b], tmp_t[b])
        P.dma(hout[tt * 128:(tt + 1) * 128, :], h1_t[b].v(), lane="sp", final=True)
        if houtT is not None:
            ptt = [ps[(2 * tt) % 4], ps[(2 * tt) % 4 + 1]]
            for k in range(8):
                P.pe("transpose", out=ptt[k // 4][:, (k % 4) * 128:(k % 4 + 1) * 128],
                     in_=h1_t[b][:, k * 128:(k + 1) * 128], identity=ident.v())
            for hf in range(2):
                P.act("copy", out=h1Tf[b][:, hf * 4:(hf + 1) * 4, :],
                      in_=ptt[hf].v().rearrange("p (k t) -> p k t", k=4))
            if isinstance(houtT, RowBlocks):
                assert houtT.bs == 256
                for j2 in range(4):
                    P.dma(houtT.views[j2][:, tt * 128:(tt + 1) * 128].rearrange("(k p) t -> p k t", p=128),
                          h1Tf[b][:, 2 * j2:2 * j2 + 2, :], lane="sp", final=True)
            else:
                P.dma(houtT[:, tt * 128:(tt + 1) * 128].rearrange("(k p) t -> p k t", p=128), h1Tf[b].v(),
                      lane="sp", final=True)


def build_T(nc, TOK=2048):
    P = Prog(nc)
    io = dict(hin=P.dram("hin", [TOK, D], F32, "ExternalInput"), xT=P.dram("xT", [D, TOK], F32, "ExternalInput"),
              wout=P.dram("wout", [D, D], F32, "ExternalInput"), rw=P.dram("rw", [D, NE], F32, "ExternalInput"),
              rb=P.dram("rb", [1, NE], F32, "ExternalInput"), wg=P.dram("wg", [NE, D, DE], F32, "ExternalInput"),
              wu=P.dram("wu", [NE, D, DE], F32, "ExternalInput"), wd=P.dram("wd", [NE, DE, D], F32, "ExternalInput"),
              hout=P.dram("hout", [TOK, D], F32, "ExternalOutput"))
    lnp = P.dram("lnp", [4, D], F32, "ExternalInput")
    io["ln"] = [lnp[i:i + 1, :] for i in range(4)]
    body_T(P, io, TOK)
    P.emit()
    P.close()
    return nc


D = 1024


def body_GM(P, io, TOK=2048):
    NT = TOK // 128
    hT, w_in, b_in, wsT, bsT = io["hT"], io["w_in"], io["b_in"], io["wsT"], io["bsT"]
    lnrows = io["ln"]

    hTb = P.sbuf([128, 8, TOK], BF16, "hTb")
    winb = P.sbuf([128, 8, 2 * D], BF16, "winb")
    bint = P.sbuf([128, 2 * D], F32, "bint")
    lnt = [P.sbuf([128, D], F32, "lnt%d" % i) for i in range(2)]
    wst = P.sbuf([128, 8, 128], F32, "wst")
    wsb = P.sbuf([128, 8, 128], BF16, "wsb")
    bst = P.sbuf([128, 8], F32, "bst")
    zt = [P.sbuf([128, 2 * D], F32, "z%d" % i) for i in range(2)]
    vn = [P.sbuf([128, D], F32, "vn%d" % i) for i in range(2)]
    vnb = [P.sbuf([128, D], BF16, "vnb%d" % i) for i in range(2)]
    yt = [P.sbuf([128, D], F32, "y%d" % i) for i in range(2)]
    stats = [P.sbuf([128, 2, 6], F32, "st%d" % i) for i in range(2)]
    mv = [P.sbuf([128, 2], F32, "mv%d" % i) for i in range(2)]
    rstd = [P.sbuf([128, 1], F32, "rstd%d" % i) for i in range(2)]
    ps = [P.psum([128, 512], F32, "ps%d" % i) for i in range(8)]
    identf = P.sbuf([128, 128], F32, "identf")
    yT = [P.sbuf([128, 8, 128], F32, "yT%d" % i) for i in range(2)]
    P.op("pool", "memset", extra_writes=[identf], ap=identf.v().ap, constant=1.0)
    P.pool("affine_select", out=identf.v(), in_=identf.v(), pattern=[[-1, 128]],
           compare_op=ALU.is_equal, fill=0.0, base=0, channel_multiplier=1)

    for k in range(8):
        for c2 in range(max(1, TOK // 2048)):
            w_ = min(TOK, 2048)
            P.dma(hTb[:, k, c2 * w_:(c2 + 1) * w_], rows_view(hT, k * 128, 128)[:, c2 * w_:(c2 + 1) * w_], lane="pool")
        P.dma(winb[:, k, 0:1024], w_in[k * 128:(k + 1) * 128, 0:1024], lane="pool")
        P.dma(winb[:, k, 1024:2048], w_in[k * 128:(k + 1) * 128, 1024:2048], lane="pool")
    P.dma(bint.v(), View(b_in, b_in.tensor[0:1, :].broadcast_to([128, 2 * D])), lane="sp")
    for i in range(2):
        P.dma(lnt[i].v(), View(lnrows[i], lnrows[i].tensor.broadcast_to([128, D])), lane="sp")
    P.dma(wst.v(), wsT.v(), lane="sp")
    P.dma(bst.v(), bsT.v(), lane="sp")
    P.pool("affine_select", out=wst.v(), in_=wst.v(), pattern=[[0, 8], [1, 128]],
           compare_op=ALU.is_ge, fill=0.0, base=0, channel_multiplier=-1)
    P.pool("tensor_copy", out=wsb.v(), in_=wst.v())

    for tt in range(NT):
        b = tt % 2
        for cb in range(4):
            pz = ps[cb]
            for k in range(8):
                P.mm(pz.v(), hTb[:, k, tt * 128:(tt + 1) * 128], winb[:, k, cb * 512:(cb + 1) * 512],
                     start=(k == 0), stop=(k == 7))
            P.dve("tensor_tensor", out=zt[b][:, cb * 512:(cb + 1) * 512], in0=pz.v(),
                  in1=bint[:, cb * 512:(cb + 1) * 512], op=ALU.add)
        P.act("activation", out=zt[b].v(), in_=zt[b].v(), func=AF.Gelu)
        layer_norm_tile(P, zt[b][:, D:2 * D], vn[b].v(), lnt[0].v(), lnt[1].v(), stats[b], mv[b], rstd[b], None)
        P.act("copy", out=vnb[b].v(), in_=vn[b].v())
        for g in range(8):
            psv = ps[4 + g // 4]
            P.mm(psv[:, (g % 4) * 128:(g % 4 + 1) * 128], wsb[:, g, :], vnb[b][:, g * 128:(g + 1) * 128])
        for g in range(8):
            psv = ps[4 + g // 4]
            P.dve("scalar_tensor_tensor", out=yt[b][:, g * 128:(g + 1) * 128],
                  in0=psv[:, (g % 4) * 128:(g % 4 + 1) * 128], scalar=bst[:, g:g + 1],
                  in1=zt[b][:, g * 128:(g + 1) * 128], op0=ALU.add, op1=ALU.mult)
        if "X" in io:
            P.dma(io["X"][tt * 128:(tt + 1) * 128, :], yt[b].v(), lane="sp", final=True)
        else:
            for k in range(8):
                P.pe("transpose", out=ps[6 + k // 4][:, (k % 4) * 128:(k % 4 + 1) * 128],
                     in_=yt[b][:, k * 128:(k + 1) * 128], identity=identf.v())
            for hf in range(2):
                P.act("copy", out=yT[b][:, hf * 4:(hf + 1) * 4, :],
                      in_=ps[6 + hf].v().rearrange("p (k t) -> p k t", k=4))
            P.dma(io["XT"][:, tt * 128:(tt + 1) * 128].rearrange("(k p) t -> p k t", p=128), yT[b].v(),
                  lane="sp", final=True)


def build_GM(nc, TOK=2048):
    P = Prog(nc)
    lnp = P.dram("lnp", [2, D], F32, "ExternalInput")
    io = dict(hT=P.dram("hT", [D, TOK], F32, "ExternalInput").v(), w_in=P.dram("w_in", [D, 2 * D], F32, "ExternalInput").v(),
              b_in=P.dram("b_in", [1, 2 * D], F32, "ExternalInput").v(), ln=[lnp[i:i + 1, :] for i in range(2)],
              wsT=P.dram("wsT", [128, 8, 128], F32, "ExternalInput").v(), bsT=P.dram("bsT", [128, 8], F32, "ExternalInput").v(),
              X=P.dram("X", [TOK, D], F32, "ExternalOutput").v())
    body_GM(P, io, TOK)
    P.emit()
    P.close()
    return nc


D = 1024
S = 4096
NB = S // 128
NQ = S // 512
NH = 8


def body_FOX(P, hT, groups):
    hTb = P.sbuf([128, 8, S], BF16, "hTb")
    wqb = P.sbuf([128, 8, 512], BF16, "wqb")
    wkb = P.sbuf([128, 8, 512], BF16, "wkb")
    wvb = P.sbuf([128, 8, 512], BF16, "wvb")
    wogb = P.sbuf([128, 8, 512], BF16, "wogb")
    wfb = P.sbuf([128, 8, NH], BF16, "wfb")
    bft = P.sbuf([128, NH], F32, "bft")
    Vall = P.sbuf([128, NB, 512], BF16, "Vall")
    QT = [P.sbuf([128, S], BF16, "QT%d" % i) for i in range(2)]
    KT = [P.sbuf([128, S], BF16, "KT%d" % i) for i in range(2)]
    OG = [P.sbuf([64, S], BF16, "OG%d" % i) for i in range(2)]
    triU = P.sbuf([128, 128], F32, "triU")
    triUb = P.sbuf([128, 128], BF16, "triUb")
    onesf = P.sbuf([128, 128], F32, "onesf")
    Vaug = [P.sbuf([128, NB, 128], BF16, "Vaug0")] * 2
    Sh = P.sbuf([128, 64], F32, "Sh")
    rd = P.sbuf([128, 512], F32, "rd")
    logf = P.sbuf([128, NB, NH], F32, "logf")
    negcin = P.sbuf([128, NB, NH], F32, "negcin")
    Rb = P.sbuf([128, NB + 1, NH], F32, "Rb")
    negR = P.sbuf([128, NB + 1, NH], F32, "negR")
    biasq = [P.sbuf([128, NB], F32, "biasq%d" % i) for i in range(2)]
    Pt = [P.sbuf([128, 512], BF16, "P%d" % i) for i in range(4)]
    rden = [P.sbuf([64, 512], F32, "rden%d" % i) for i in range(2)]
    ot = [P.sbuf([64, 512], F32, "ot%d" % i) for i in range(2)]
    ps = [P.psum([128, 512], F32, "ps%d" % i) for i in range(8)]

    for k in range(8):
        for c4 in range(4):
            P.dma(hTb[:, k, c4 * 1024:(c4 + 1) * 1024], hT[k * 128:(k + 1) * 128, c4 * 1024:(c4 + 1) * 1024], lane="pool")
    P.op("pool", "memset", extra_writes=[triU], ap=triU.v().ap, constant=1.0)
    P.pool("affine_select", out=triU.v(), in_=triU.v(), pattern=[[1, 128]],
           compare_op=ALU.is_ge, fill=0.0, base=0, channel_multiplier=-1)
    P.pool("tensor_copy", out=triUb.v(), in_=triU.v())
    P.op("pool", "memset", extra_writes=[onesf], ap=onesf.v().ap, constant=1.0)
    P.op("pool", "memset", extra_writes=[Vaug[0]], ap=Vaug[0].v().ap, constant=1.0)
    P.op("pool", "memset", extra_writes=[rd], ap=rd.v().ap, constant=0.0)
    for t_ in (QT, KT):
        P.op("pool", "memset", extra_writes=[t_[0]], ap=t_[0][64:128, :].ap, constant=0.0)
        P.op("pool", "memset", extra_writes=[t_[1]], ap=t_[1][0:64, :].ap, constant=0.0)
    P.op("pool", "memset", extra_writes=[Sh], ap=Sh.v().ap, constant=1.0)
    P.pool("affine_select", out=Sh.v(), in_=Sh.v(), pattern=[[-1, 64]],
           compare_op=ALU.is_equal, fill=0.0, base=-64, channel_multiplier=1)

    unit = 0
    sc = 0
    for grp in groups:
        wq, wk, wv, wog, wf, bf, XT = (grp[n] for n in ("wq", "wk", "wv", "wog", "wf", "bf", "XT"))
        for wsrc, wdst in ((wq, wqb), (wk, wkb), (wv, wvb), (wog, wogb)):
            P.dma(wdst.v(), wsrc.rearrange("(k p) f -> p k f", p=128), lane="pool")
        P.dma(wfb.v(), wf.rearrange("(k p) f -> p k f", p=128), lane="pool")
        P.dma(bft.v(), View(bf, bf.tensor.broadcast_to([128, NH])), lane="sp")
        pf = ps[0]
        for kb in range(NB):
            for k in range(8):
                P.mm(pf[:, kb * NH:(kb + 1) * NH], hTb[:, k, kb * 128:(kb + 1) * 128], wfb[:, k, :],
                     start=(k == 0), stop=(k == 7))
        lf2 = logf.v().rearrange("p b h -> p (b h)")
        P.dve("tensor_tensor", out=logf.v(), in0=pf[:, 0:NB * NH].rearrange("p (b h) -> p b h", h=NH),
              in1=View(bft, bft.tensor[:, :].unsqueeze(1).to_broadcast([128, NB, NH])), op=ALU.add)
        P.act("activation", out=lf2, in_=lf2, func=AF.Exp, scale=-1.0)
        P.act("activation", out=lf2, in_=lf2, func=AF.Ln, bias=1.0)
        P.dve("tensor_scalar", out=lf2, in0=lf2, scalar1=-1.0, scalar2=None, op0=ALU.mult)
        pc = ps[1]
        P.mm(pc[:, 0:NB * NH], triU.v(), lf2)
        P.dve("tensor_scalar", out=negcin.v().rearrange("p b h -> p (b h)"), in0=pc[:, 0:NB * NH],
              scalar1=-1.0, scalar2=None, op0=ALU.mult)
        pT = ps[2]
        P.mm(pT[:, 0:NB * NH], onesf.v(), lf2)
        P.op("dve", "memset", extra_writes=[Rb], ap=Rb[:, 0, :].ap, constant=0.0)
        for m in range(NB):
            P.dve("tensor_tensor", out=Rb[:, m + 1, :], in0=Rb[:, m, :], in1=pT[:, m * NH:(m + 1) * NH], op=ALU.add)
        P.dve("tensor_scalar", out=negR.v(), in0=Rb.v(), scalar1=-1.0, scalar2=None, op0=ALU.mult)

        for kb in range(NB):
            pv = ps[4 + kb % 4]
            for k in range(8):
                P.mm(pv.v(), hTb[:, k, kb * 128:(kb + 1) * 128], wvb[:, k, :], start=(k == 0), stop=(k == 7))
            if kb % 2 == 0:
                P.dve("tensor_copy", out=Vall[:, kb, :], in_=pv.v())
            else:
                P.act("copy", out=Vall[:, kb, :], in_=pv.v())

        for h in range(NH):
            hb = h % 2
            for j in range(NQ):
                if hb == 0:
                    for (wsrc, dsts) in ((wqb, QT), (wkb, KT)):
                        pp = ps[sc % 4]
                        sc += 1
                        for k in range(8):
                            P.mm(pp.v(), wsrc[:, k, h * 64:(h + 2) * 64], hTb[:, k, j * 512:(j + 1) * 512],
                                 start=(k == 0), stop=(k == 7))
                        P.dve("tensor_copy", out=dsts[0][0:64, j * 512:(j + 1) * 512], in_=pp[0:64, :])
                        P.act("copy", out=dsts[1][64:128, j * 512:(j + 1) * 512], in_=pp[64:128, :])
                pp = ps[sc % 4]
                sc += 1
                for k in range(8):
                    P.mm(pp[0:64, :], wogb[:, k, h * 64:(h + 1) * 64], hTb[:, k, j * 512:(j + 1) * 512],
                         start=(k == 0), stop=(k == 7))
                P.act("activation", out=OG[hb][:, j * 512:(j + 1) * 512], in_=pp[0:64, :], func=AF.Sigmoid)
            P.dve("tensor_copy", out=Vaug[hb][:, :, 0:64], in_=Vall[:, :, h * 64:(h + 1) * 64])
            for j in range(NQ):
                ub = unit % 2
                unit += 1
                nk = 4 * j + 4
                bq = biasq[ub]
                P.dve("scalar_tensor_tensor", out=bq[:, 0:nk], in0=negR[:, 0:nk, h], scalar=Rb[:, 4 * j, h:h + 1],
                      in1=negcin[:, 0:nk, h], op0=ALU.add, op1=ALU.add)
                pnum = ps[4 + 2 * ub]
                pden = ps[5 + 2 * ub]
                pend = []
                for kb in range(nk):
                    i = kb - 4 * j
                    c0 = 128 * i if i > 0 else 0
                    pS = ps[sc % 4]
                    pt_ = Pt[sc % 4]
                    sc += 1
                    P.mm(pS[:, c0:512], KT[hb][:, kb * 128:(kb + 1) * 128], QT[hb][:, j * 512 + c0:(j + 1) * 512])
                    P.act("activation", out=pt_[:, c0:512], in_=pS[:, c0:512], func=AF.Exp,
                          scale=0.125, bias=bq[:, kb:kb + 1])
                    if i >= 0:
                        P.dve("tensor_tensor", out=pt_[:, c0:c0 + 128], in0=pt_[:, c0:c0 + 128],
                              in1=triUb.v(), op=ALU.mult)
                    def pv(kb=kb, c0=c0, pt_=pt_):
                        P.mm(pnum[:, c0:512], Vaug[hb][:, kb, :], pt_[:, c0:512],
                             start=(kb == 0), stop=(kb == nk - 1))
                    pend.append(pv)
                    if len(pend) > 3:
                        pend.pop(0)()
                while pend:
                    pend.pop(0)()
                P.dve("reciprocal", out=rd[64:128, :], in_=pnum[64:128, :])
                P.mm(pden[0:64, :], Sh.v(), rd.v())
                P.act("copy", out=rden[ub].v(), in_=pden[0:64, :])
                P.dve("tensor_tensor", out=ot[ub].v(), in0=pnum[0:64, :], in1=rden[ub].v(), op=ALU.mult)
                P.dve("tensor_tensor", out=ot[ub].v(), in0=ot[ub].v(), in1=OG[hb][:, j * 512:(j + 1) * 512], op=ALU.mult)
                P.dma(rows_view(XT, h * 64, 64)[:, j * 512:(j + 1) * 512], ot[ub].v(), lane="sp", final=True)


def build_FOX(nc):
    P = Prog(nc)
    hT = P.dram("hT", [D, S], F32, "ExternalInput")
    grp = dict(wq=P.dram("wq", [D, 512], F32, "ExternalInput").v(), wk=P.dram("wk", [D, 512], F32, "ExternalInput").v(),
               wv=P.dram("wv", [D, 512], F32, "ExternalInput").v(), wog=P.dram("wog", [D, 512], F32, "ExternalInput").v(),
               wf=P.dram("wf", [D, NH], F32, "ExternalInput").v(), bf=P.dram("bf", [1, NH], F32, "ExternalInput").v(),
               XT=P.dram("XT", [512, S], F32, "ExternalOutput").v())
    body_FOX(P, hT.v(), [grp])
    P.emit()
    P.close()
    return nc


D = 1024
S = 4096
NB = S // 128
NQ = S // 512
NH = 8


def body_SB(P, hT, groups):
    hTb = P.sbuf([128, 8, S], BF16, "hTb")
    wqb = P.sbuf([128, 8, 512], BF16, "wqb")
    wkb = P.sbuf([128, 8, 512], BF16, "wkb")
    wvb = P.sbuf([128, 8, 512], BF16, "wvb")
    Vall = P.sbuf([128, NB, 512], BF16, "Vall")
    QT = [P.sbuf([128, S], BF16, "QT%d" % i) for i in range(2)]
    KT = [P.sbuf([128, S], BF16, "KT%d" % i) for i in range(2)]
    tmpf = P.sbuf([128, 128], F32, "tmpf")
    strictUb = P.sbuf([128, 128], BF16, "strictUb")
    negTriLb = P.sbuf([128, 128], BF16, "negTriLb")
    negones = P.sbuf([128, 128], BF16, "negones")
    zerosb = P.sbuf([128, 128], BF16, "zerosb")
    et = [P.sbuf([128, 512], F32, "et%d" % i) for i in range(2)]
    spb = [P.sbuf([128, 512], BF16, "spb%d" % i) for i in range(3)]
    At = [P.sbuf([128, 512], BF16, "At%d" % i) for i in range(3)]
    Lsum = [P.sbuf([128, 512], BF16, "Lsum%d" % i) for i in range(2)]
    ot = [P.sbuf([128, 512], F32, "ot%d" % i) for i in range(2)]
    ps = [P.psum([128, 512], F32, "ps%d" % i) for i in range(8)]

    for k in range(8):
        for c4 in range(4):
            P.dma(hTb[:, k, c4 * 1024:(c4 + 1) * 1024], hT[k * 128:(k + 1) * 128, c4 * 1024:(c4 + 1) * 1024], lane="pool")
    P.op("pool", "memset", extra_writes=[tmpf], ap=tmpf.v().ap, constant=1.0)
    P.pool("affine_select", out=tmpf.v(), in_=tmpf.v(), pattern=[[1, 128]],
           compare_op=ALU.is_gt, fill=0.0, base=0, channel_multiplier=-1)
    P.pool("tensor_copy", out=strictUb.v(), in_=tmpf.v())
    P.op("pool", "memset", extra_writes=[tmpf], ap=tmpf.v().ap, constant=-1.0)
    P.pool("affine_select", out=tmpf.v(), in_=tmpf.v(), pattern=[[-1, 128]],
           compare_op=ALU.is_ge, fill=0.0, base=0, channel_multiplier=1)
    P.pool("tensor_copy", out=negTriLb.v(), in_=tmpf.v())
    P.op("pool", "memset", extra_writes=[negones], ap=negones.v().ap, constant=-1.0)
    P.op("pool", "memset", extra_writes=[zerosb], ap=zerosb.v().ap, constant=0.0)
    for t_ in (QT, KT):
        P.op("pool", "memset", extra_writes=[t_[0]], ap=t_[0][64:128, :].ap, constant=0.0)
        P.op("pool", "memset", extra_writes=[t_[1]], ap=t_[1][0:64, :].ap, constant=0.0)

    unit = 0
    sc = 0
    for grp in groups:
        wq, wk, wv, XT = (grp[n] for n in ("wq", "wk", "wv", "XT"))
        for wsrc, wdst in ((wq, wqb), (wk, wkb), (wv, wvb)):
            P.dma(wdst.v(), wsrc.rearrange("(k p) f -> p k f", p=128), lane="pool")
        for kb in range(NB):
            pv = ps[4 + kb % 4]
            for k in range(8):
                P.mm(pv.v(), hTb[:, k, kb * 128:(kb + 1) * 128], wvb[:, k, :], start=(k == 0), stop=(k == 7))
            if kb % 2 == 0:
                P.dve("tensor_copy", out=Vall[:, kb, :], in_=pv.v())
            else:
                P.act("copy", out=Vall[:, kb, :], in_=pv.v())

        for h in range(NH):
            hb = h % 2
            if hb == 0:
                for j in range(NQ):
                    for (wsrc, dsts, scl) in ((wqb, QT, 0.125), (wkb, KT, 1.0)):
                        pp = ps[sc % 2]
                        sc += 1
                        for k in range(8):
                            P.mm(pp.v(), wsrc[:, k, h * 64:(h + 2) * 64], hTb[:, k, j * 512:(j + 1) * 512],
                                 start=(k == 0), stop=(k == 7))
                        P.dve("tensor_scalar", out=dsts[0][0:64, j * 512:(j + 1) * 512], in0=pp[0:64, :],
                              scalar1=scl, scalar2=None, op0=ALU.mult)
                        P.act("mul", out=dsts[1][64:128, j * 512:(j + 1) * 512], in_=pp[64:128, :], mul=scl)
            for j in range(NQ):
                ub = unit % 2
                unit += 1
                nk = 4 * j + 4
                pnum = ps[4 + ub]
                ls = Lsum[ub]
                P.op("pool", "memset", extra_writes=[ls], ap=ls.v().ap, constant=0.0)
                P.mm(pnum.v(), zerosb.v(), hTb[:, 0, 0:512], start=True, stop=False)
                steps = []
                for kb in range(nk - 1, -1, -1):
                    i = kb - 4 * j
                    c0 = 128 * i if i > 0 else 0
                    steps.append(dict(kb=kb, i=i, c0=c0, pA=ps[sc % 4], pB=ps[sc % 4], e_=et[sc % 2],
                                      sp_=spb[sc % 3], a_=At[sc % 3],
                                      kT=KT[hb][:, kb * 128:(kb + 1) * 128],
                                      qT=QT[hb][:, j * 512 + c0:(j + 1) * 512]))
                    sc += 1

                def s1(st):
                    c0 = st["c0"]
                    P.mm(st["pA"][:, c0:512], st["kT"], st["qT"], start=True, stop=False)
                    P.act("activation", out=st["e_"][:, c0:512], in_=st["pA"][:, c0:512], func=AF.Exp)
                    P.act("activation", out=st["sp_"][:, c0:512], in_=st["e_"][:, c0:512], func=AF.Ln, bias=1.0)
                    if st["i"] >= 0:
                        P.pool("tensor_tensor", out=st["sp_"][:, c0:c0 + 128], in0=st["sp_"][:, c0:c0 + 128],
                               in1=strictUb.v(), op=ALU.mult)

                def s2(st):
                    c0, kb = st["c0"], st["kb"]
                    first = (kb == nk - 1)
                    P.mm(st["pB"][:, c0:512], negTriLb.v(), st["sp_"][:, c0:512], start=False, stop=first)
                    if not first:
                        P.mm(st["pB"][:, c0:512], negones.v(), ls[:, c0:512], start=False, stop=True)
                    P.act("activation", out=st["a_"][:, c0:512], in_=st["pB"][:, c0:512], func=AF.Exp)
                    if st["i"] >= 0:
                        P.pool("tensor_tensor", out=st["a_"][:, c0:c0 + 128], in0=st["a_"][:, c0:c0 + 128],
                               in1=strictUb.v(), op=ALU.mult)
                    if kb > 0:
                        P.dve("tensor_tensor", out=ls[:, c0:512], in0=ls[:, c0:512], in1=st["sp_"][:, c0:512], op=ALU.add)

                def s3(st):
                    c0, kb = st["c0"], st["kb"]
                    P.mm(pnum[:, c0:512], Vall[:, kb, (h - hb) * 64:(h - hb + 2) * 64], st["a_"][:, c0:512],
                         start=False, stop=(kb == 0))

                ns = len(steps)
                s1(steps[0])
                for n in range(ns):
                    if n + 1 < ns:
                        s1(steps[n + 1])
                    s2(steps[n])
                    if n >= 1:
                        s3(steps[n - 1])
                s3(steps[ns - 1])
                r0 = hb * 64
                P.dve("tensor_copy", out=ot[ub][r0:r0 + 64, :], in_=pnum[r0:r0 + 64, :])
                P.dma(rows_view(XT, h * 64, 64)[:, j * 512:(j + 1) * 512], ot[ub][r0:r0 + 64, :], lane="sp", final=True)


def build_SB(nc):
    P = Prog(nc)
    hT = P.dram("hT", [D, S], F32, "ExternalInput")
    grp = dict(wq=P.dram("wq", [D, 512], F32, "ExternalInput").v(), wk=P.dram("wk", [D, 512], F32, "ExternalInput").v(),
               wv=P.dram("wv", [D, 512], F32, "ExternalInput").v(), XT=P.dram("XT", [512, S], F32, "ExternalOutput").v())
    body_SB(P, hT.v(), [grp])
    P.emit()
    P.close()
    return nc


import math

D = 1024
S = 4096
NSB = S // 128
NH = 8
CNEG = -math.exp(-0.5)
GN_EPS = 64e-5


def body_RW(P, hT, muT, w1, a1, g1, groups, nsb=NSB):
    DEBUG = False
    dbg = None

    def dump(view, idx):
        pass

    def dump2(view, idx, rows, cols):
        pass

    def sb_(shape, dt, name):
        return P.sbuf(shape, dt, name)

    mut = sb_([128, 8, 6], F32, "mut")
    omut = sb_([128, 8, 6], F32, "omut")
    Wa = {}
    Wb = {}
    for nm, ncol in (("r", 512), ("k", 512), ("v", 512), ("w1", 64), ("a1", 64), ("g1", 128)):
        Wa[nm] = sb_([128, 8, ncol], BF16, "Wa_" + nm)
        Wb[nm] = sb_([128, 8, ncol], BF16, "Wb_" + nm)
    w2b = sb_([64, 512], BF16, "w2b")
    a2b = sb_([64, 512], BF16, "a2b")
    g2b = sb_([128, 512], BF16, "g2b")
    vt = [sb_([128, 512], F32, "vec%d" % i) for i in range(7)]
    W0, A0, KK_, KA_, GNG, GNB, RK = vt

    wstl = [sb_([128, 512], F32, "wsl%d" % i) for i in range(4)]
    P.dma(mut.v(), muT.v(), lane="sp")
    P.dve("tensor_scalar", out=omut.v(), in0=mut.v(), scalar1=-1.0, scalar2=1.0, op0=ALU.mult, op1=ALU.add)

    identf = sb_([128, 128], F32, "identf")
    P.op("pool", "memset", extra_writes=[identf], ap=identf.v().ap, constant=1.0)
    P.pool("affine_select", out=identf.v(), in_=identf.v(), pattern=[[-1, 128]],
           compare_op=ALU.is_equal, fill=0.0, base=0, channel_multiplier=1)
    BD = sb_([128, 128], F32, "BD")
    bd3 = BD.v().rearrange("p (c i) -> p c i", c=4)
    P.op("pool", "memset", extra_writes=[BD], ap=BD.v().ap, constant=1.0)
    P.pool("affine_select", out=bd3, in_=bd3, pattern=[[-32, 4], [0, 32]],
           compare_op=ALU.is_ge, fill=0.0, base=0, channel_multiplier=1)
    P.pool("affine_select", out=bd3, in_=bd3, pattern=[[32, 4], [0, 32]],
           compare_op=ALU.is_ge, fill=0.0, base=31, channel_multiplier=-1)
    mAB = sb_([128, 512], F32, "mAB")
    mC = sb_([128, 256], F32, "mC")
    triBD = sb_([128, 128], F32, "triBD")
    blkBD = sb_([128, 128], F32, "blkBD")
    P.pool("affine_select", out=mAB[:, 0:128], in_=BD.v(), pattern=[[1, 128]],
           compare_op=ALU.is_gt, fill=0.0, base=0, channel_multiplier=-1)
    P.pool("affine_select", out=mAB[:, 128:256], in_=BD.v(), pattern=[[1, 128]],
           compare_op=ALU.is_ge, fill=0.0, base=0, channel_multiplier=-1)
    P.pool("tensor_copy", out=mAB[:, 256:512], in_=mAB[:, 0:256])
    P.pool("affine_select", out=mC[:, 0:128], in_=BD.v(), pattern=[[-1, 128]],
           compare_op=ALU.is_gt, fill=0.0, base=0, channel_multiplier=1)
    P.pool("tensor_copy", out=mC[:, 128:256], in_=mC[:, 0:128])
    P.pool("tensor_scalar", out=triBD.v(), in0=mAB[:, 128:256], scalar1=CNEG, scalar2=None, op0=ALU.mult)
    P.pool("tensor_scalar", out=blkBD.v(), in0=BD.v(), scalar1=CNEG, scalar2=None, op0=ALU.mult)
    RMexp = sb_([128, 4, 64], F32, "RMexp")
    P.op("pool", "memset", extra_writes=[RMexp], ap=RMexp.v().ap, constant=1.0)
    P.pool("affine_select", out=RMexp.v(), in_=RMexp.v(), pattern=[[-32, 4], [0, 64]],
           compare_op=ALU.is_ge, fill=0.0, base=0, channel_multiplier=1)
    P.pool("affine_select", out=RMexp.v(), in_=RMexp.v(), pattern=[[32, 4], [0, 64]],
           compare_op=ALU.is_ge, fill=0.0, base=31, channel_multiplier=-1)
    CM = sb_([64, 4, 128], F32, "CM")
    cm4 = CM.v().rearrange("p c (d i) -> p c d i", d=4)
    P.op("pool", "memset", extra_writes=[CM], ap=CM.v().ap, constant=1.0)
    P.pool("affine_select", out=cm4, in_=cm4, pattern=[[1, 4], [-1, 4], [0, 32]],
           compare_op=ALU.is_equal, fill=0.0, base=0, channel_multiplier=0)
    Sel = sb_([128, 4], F32, "Sel")
    P.op("pool", "memset", extra_writes=[Sel], ap=Sel.v().ap, constant=1.0)
    P.pool("affine_select", out=Sel.v(), in_=Sel.v(), pattern=[[-32, 4]],
           compare_op=ALU.is_equal, fill=0.0, base=0, channel_multiplier=1)

    hg = [sb_([128, 8, 513], BF16, "hg%d" % i) for i in range(1)]
    th = [sb_([64, 512], BF16, "th%d" % i) for i in range(1)]
    xa = [sb_([64, 512], BF16, "xa%d" % i) for i in range(1)]
    sgT = [sb_([128, 512], BF16, "sgT%d" % i) for i in range(1)]

    def f32t(name, n=1):
        return [sb_([128, 512], F32, "%s%d" % (name, i)) for i in range(n)]
    r_t = f32t("r_t", 1)
    k_t = f32t("k_t", 1)
    V_t = f32t("V_t", 1)
    sgd = f32t("sgd", 1)
    lwc = f32t("lwc", 1)
    tmpA = f32t("tmpA", 1)
    tmpB = f32t("tmpB", 1)
    a_t = f32t("a_t", 1)
    kk_t = f32t("kk_t", 1)
    kp_t = f32t("kp_t", 1)
    ka_t = a_t
    E2 = f32t("E2", 1)
    E4 = sgd
    At = [wstl[0]]
    Bt = [wstl[1]]
    Kt = [wstl[2]]
    Rt = [wstl[3]]
    Bh = f32t("Bh", 1)
    Kh = f32t("Kh", 1)
    E5 = f32t("E5", 1)
    g_t = f32t("g_t", 1)
    bon = f32t("bon", 1)
    Vm = [sb_([128, 8, 4, 64], F32, "Vm%d" % i) for i in range(1)]
    ss8 = sb_([128, 8], F32, "ss8")
    rk8 = sb_([128, 8], F32, "rk8")
    TT = [sb_([64, 512], BF16, "TT%d" % i) for i in range(2)]
    PP = [[sb_([128, 256], BF16, "PP%d_%d" % (i, j)) for j in range(2)] for i in range(2)]
    XX = [[sb_([128, 192], BF16, "XX%d_%d" % (i, j)) for j in range(2)] for i in range(2)]
    XF32 = [sb_([128, 192], F32, "XF32_%d" % i) for i in range(2)]
    KrKh = [sb_([128, 192], F32, "KrKh%d" % i) for i in range(2)]
    AkaT = [sb_([128, 128], F32, "AkaT%d" % i) for i in range(2)]
    G1 = [sb_([64, 128], F32, "G1_%d" % i) for i in range(2)]
    G1pad = [[sb_([64, 4, 128], F32, "G1pad%d_%d" % (i, h)) for h in range(NH)] for i in range(1)]
    G2H2 = [sb_([128, 192], F32, "G2H2_%d" % i) for i in range(2)]
    X2m = [sb_([128, 4, 64], F32, "X2m%d" % i) for i in range(2)]
    WCT = [sb_([64, 4], F32, "WCT%d" % i) for i in range(2)]
    H1 = [[sb_([64, 4, 64], F32, "H1_%d_%d" % (i, h)) for h in range(NH)] for i in range(1)]
    SvT = [sb_([64, 4, NH, 64], F32, "SvT%d" % i) for i in range(1)]
    ST = [sb_([64, NH, 64], F32, "ST%d" % i) for i in range(2)]
    y_t = f32t("y_t", 1)
    s1 = sb_([128, 8], F32, "s1")
    s2 = sb_([128, 8], F32, "s2")
    o_t = f32t("o_t", 1)
    ysq = o_t

    psG = [P.psum([128, 512], F32, "psG%d" % i) for i in range(2)]
    psY = [P.psum([128, 512], F32, "psY%d" % i) for i in range(1)]
    psS = P.psum([128, 512], F32, "psS")
    B0, B1, B2, B3 = [P.psum([128, 512], F32, "B%d" % i) for i in range(4)]


    def bc8(tile8):
        return View(tile8, tile8.tensor[:, :].unsqueeze(2).to_broadcast([128, 8, 64]))

    def v3(view):
        return view.rearrange("p (h j) -> p h j", h=8)

    gi = 0
    for grp in groups:
        wr, wk, wv, w2, a2, g2, vecs = (grp[n] for n in ("wr", "wk", "wv", "w2", "a2", "g2", "vecs"))
        for ci, (nm, src, ncol) in enumerate((("r", wr, 512), ("k", wk, 512), ("v", wv, 512),
                                              ("w1", w1, 64), ("a1", a1, 64), ("g1", g1, 128))):
            for k in range(8):
                wk_ = wstl[(ci * 8 + k) % 4]
                P.dma(wk_[:, 0:ncol], src[k * 128:(k + 1) * 128, :], lane="sp")
                P.dve("tensor_scalar", out=Wb[nm][:, k, :], in0=wk_[:, 0:ncol], scalar1=mut[:, k, ci:ci + 1],
                      scalar2=None, op0=ALU.mult)
                P.act("mul", out=Wa[nm][:, k, :], in_=wk_[:, 0:ncol], mul=omut[:, k, ci:ci + 1])
        P.dma(w2b.v(), w2, lane="pool")
        P.dma(a2b.v(), a2, lane="pool")
        P.dma(g2b.v(), g2, lane="pool")
        for i in range(7):
            P.dma(vt[i].v(), View(vecs[i], vecs[i].tensor.broadcast_to([128, 512])), lane="sp")
        P.op("pool", "memset", extra_writes=[ST[0]], ap=ST[0].v().ap, constant=0.0)
        st_cur = 0
        for sb in range(nsb):
            q = sb % 4
            g = sb // 4
            gb = 0
            pb = 0
            yb = 0
            t0 = sb * 128
            if q == 0:
                hgt = hg[gb]
                for k in range(8):
                    if g == 0:
                        P.op("pool", "memset", extra_writes=[hgt], ap=hgt[:, k, 0:1].ap, constant=0.0)
                        P.dma(hgt[:, k, 1:513], hT[k * 128:(k + 1) * 128, 0:512], lane="pool")
                    else:
                        P.dma(hgt[:, k, 0:513], hT[k * 128:(k + 1) * 128, g * 512 - 1:(g + 1) * 512], lane="pool")
                for nm, dst, fn, rows in (("w1", th[gb], AF.Tanh, 64), ("a1", xa[gb], None, 64), ("g1", sgT[gb], AF.Sigmoid, 128)):
                    pp = psG[gi % 2]
                    gi += 1
                    for k in range(8):
                        P.mm(pp[0:rows, :], Wa[nm][:, k, :], hgt[:, k, 1:513], start=(k == 0), stop=False)
                        P.mm(pp[0:rows, :], Wb[nm][:, k, :], hgt[:, k, 0:512], start=False, stop=(k == 7))
                    if fn is None:
                        P.dve("tensor_copy", out=dst.v(), in_=pp[0:rows, :])
                    else:
                        P.act("activation", out=dst.v(), in_=pp[0:rows, :], func=fn)
            hgt = hg[gb]
            cur = lambda k: hgt[:, k, 1 + q * 128:1 + (q + 1) * 128]
            prv = lambda k: hgt[:, k, q * 128:(q + 1) * 128]

            def proj(nm):
                nonlocal gi
                pp = psG[gi % 2]
                gi += 1
                for k in range(8):
                    P.mm(pp.v(), cur(k), Wa[nm][:, k, :], start=(k == 0), stop=False)
                    P.mm(pp.v(), prv(k), Wb[nm][:, k, :], start=False, stop=(k == 7))
                return pp

            def nextps():
                nonlocal gi
                pp = psG[gi % 2]
                gi += 1
                return pp
            pp = proj("r")
            P.act("copy", out=r_t[pb].v(), in_=pp.v())
            pp = proj("k")
            P.act("copy", out=k_t[0].v(), in_=pp.v())
            pp = proj("v")
            P.act("copy", out=V_t[pb].v(), in_=pp.v())
            if sb == 0:
                dump(r_t[0].v(), 0); dump(k_t[0].v(), 1); dump(V_t[0].v(), 2)
            pp = nextps()
            P.mm(pp.v(), th[gb][:, q * 128:(q + 1) * 128], w2b.v())
            P.dve("tensor_tensor", out=tmpA[0].v(), in0=pp.v(), in1=W0.v(), op=ALU.add)
            P.act("activation", out=sgd[0].v(), in_=tmpA[0].v(), func=AF.Sigmoid)
            if sb == 0:
                dump(sgd[0].v(), 3)
            pp = nextps()
            P.mm(pp.v(), xa[gb][:, q * 128:(q + 1) * 128], a2b.v())
            P.dve("tensor_tensor", out=tmpA[0].v(), in0=pp.v(), in1=A0.v(), op=ALU.add)
            P.act("activation", out=a_t[0].v(), in_=tmpA[0].v(), func=AF.Sigmoid)
            pp = nextps()
            P.mm(pp.v(), sgT[gb][:, q * 128:(q + 1) * 128], g2b.v())
            P.act("copy", out=g_t[pb].v(), in_=pp.v())
            pl = nextps()
            P.mm(pl.v(), triBD.v(), sgd[0].v())
            P.act("copy", out=lwc[0].v(), in_=pl.v())
            if sb == 0:
                dump(lwc[0].v(), 4); dump(a_t[0].v(), 5); dump(g_t[0].v(), 6)
            pe_ = nextps()
            P.mm(pe_.v(), blkBD.v(), sgd[0].v())
            P.act("activation", out=E5[pb].v(), in_=pe_.v(), func=AF.Exp)
            P.dve("tensor_tensor", out=tmpB[0].v(), in0=pe_.v(), in1=lwc[0].v(), op=ALU.subtract)
            P.dve("scalar_tensor_tensor", out=tmpA[0].v(), in0=sgd[0].v(), scalar=-CNEG, in1=lwc[0].v(),
                  op0=ALU.mult, op1=ALU.add)
            P.act("activation", out=tmpA[0].v(), in_=tmpA[0].v(), func=AF.Exp)
            P.act("activation", out=E4[0].v(), in_=tmpB[0].v(), func=AF.Exp)
            P.act("activation", out=E2[0].v(), in_=lwc[0].v(), func=AF.Exp, scale=-1.0)
            P.act("activation", out=lwc[0].v(), in_=lwc[0].v(), func=AF.Exp)
            P.dve("tensor_tensor", out=kk_t[0].v(), in0=k_t[0].v(), in1=KK_.v(), op=ALU.mult)
            P.dve("tensor_tensor", out=tmpB[0].v(), in0=kk_t[0].v(), in1=kk_t[0].v(), op=ALU.mult)
            P.dve("tensor_reduce", out=ss8.v(), in_=v3(tmpB[0].v()), axis=AX.X, op=ALU.add)
            P.act("activation", out=ss8.v(), in_=ss8.v(), func=AF.Sqrt)
            P.dve("tensor_scalar", out=ss8.v(), in0=ss8.v(), scalar1=1e-12, scalar2=None, op0=ALU.max)
            P.dve("reciprocal", out=ss8.v(), in_=ss8.v())
            P.dve("tensor_tensor", out=v3(kk_t[0].v()), in0=v3(kk_t[0].v()), in1=bc8(ss8), op=ALU.mult)
            P.dve("scalar_tensor_tensor", out=kp_t[0].v(), in0=a_t[0].v(), scalar=-1.0, in1=KA_.v(),
                   op0=ALU.add, op1=ALU.mult)
            P.dve("scalar_tensor_tensor", out=kp_t[0].v(), in0=kp_t[0].v(), scalar=1.0, in1=k_t[0].v(),
                   op0=ALU.add, op1=ALU.mult)
            P.dve("tensor_tensor", out=ka_t[0].v(), in0=kk_t[0].v(), in1=a_t[0].v(), op=ALU.mult)
            P.dve("scalar_tensor_tensor", out=At[pb].v(), in0=kk_t[0].v(), scalar=-1.0, in1=tmpA[0].v(),
                  op0=ALU.mult, op1=ALU.mult)
            P.dve("tensor_tensor", out=Bt[pb].v(), in0=ka_t[0].v(), in1=E2[0].v(), op=ALU.mult)
            P.dve("tensor_tensor", out=Kt[pb].v(), in0=kp_t[0].v(), in1=E2[0].v(), op=ALU.mult)
            P.dve("tensor_tensor", out=Rt[pb].v(), in0=r_t[pb].v(), in1=lwc[0].v(), op=ALU.mult)
            P.dve("tensor_tensor", out=Bh[pb].v(), in0=ka_t[0].v(), in1=E4[0].v(), op=ALU.mult)
            P.dve("tensor_tensor", out=Kh[pb].v(), in0=kp_t[0].v(), in1=E4[0].v(), op=ALU.mult)
            if sb == 0:
                dump(At[0].v(), 7); dump(Bt[0].v(), 8); dump(Kt[0].v(), 9); dump(Rt[0].v(), 10)
                dump(Bh[0].v(), 11); dump(Kh[0].v(), 12); dump(E5[0].v(), 13); dump(kk_t[0].v(), 14); dump(kp_t[0].v(), 15)
            for c in range(4):
                P.act("mul", out=Vm[pb][:, :, c, :], in_=v3(V_t[pb].v()), mul=RMexp[:, c, 0:1])
            P.dve("tensor_tensor", out=tmpB[0].v(), in0=r_t[pb].v(), in1=kp_t[0].v(), op=ALU.mult)
            P.dve("tensor_tensor", out=tmpB[0].v(), in0=tmpB[0].v(), in1=RK.v(), op=ALU.mult)
            P.dve("tensor_reduce", out=rk8.v(), in_=v3(tmpB[0].v()), axis=AX.X, op=ALU.add)
            P.dve("tensor_tensor", out=v3(bon[pb].v()), in0=v3(V_t[pb].v()), in1=bc8(rk8), op=ALU.mult)

            pY = psY[yb]
            def head_gen(h, BX, BY):
                u = h % 2
                hs = slice(h * 64, (h + 1) * 64)
                P.pe("transpose", out=BX[0:64, 0:128], in_=At[pb][:, hs], identity=identf.v())
                P.pe("transpose", out=BX[0:64, 128:256], in_=Rt[pb][:, hs], identity=identf.v())
                P.pe("transpose", out=BX[0:64, 256:384], in_=Bt[pb][:, hs], identity=identf.v())
                P.pe("transpose", out=BX[0:64, 384:512], in_=Kt[pb][:, hs], identity=identf.v())
                P.act("copy", out=TT[u][:, 0:256], in_=BX[0:64, 0:256])
                P.dve("tensor_copy", out=TT[u][:, 256:512], in_=BX[0:64, 256:512])
                yield
                P.mm(BY[:, 0:256], TT[u][:, 256:384], TT[u][:, 0:256])
                P.mm(BY[:, 256:512], TT[u][:, 384:512], TT[u][:, 0:256])
                P.mm(BX[:, 0:256], TT[u][:, 0:128], TT[u][:, 256:512])
                P.mm(BX[0:64, 256:260], E5[pb][:, hs], Sel.v())
                pk = PP[u][0]
                xx = XX[u][0]
                P.dve("tensor_tensor", out=pk[:, 0:128], in0=BY[:, 0:128], in1=mAB[:, 0:128], op=ALU.mult)
                P.dve("tensor_tensor", out=xx[:, 0:128], in0=BY[:, 128:256], in1=mAB[:, 128:256], op=ALU.mult)
                P.dve("tensor_tensor", out=KrKh[u][:, 0:128], in0=BY[:, 384:512], in1=mAB[:, 128:256], op=ALU.mult)
                P.act("copy", out=WCT[u].v(), in_=BX[0:64, 256:260])
                P.dve("tensor_tensor", out=pk[:, 128:256], in0=BX[:, 0:128], in1=mC[:, 0:128], op=ALU.mult)
                P.dve("tensor_tensor", out=AkaT[u].v(), in0=BX[:, 128:256], in1=mC[:, 128:256], op=ALU.mult)
                P.act("copy", out=xx[:, 128:192], in_=Bh[pb][:, hs])
                P.act("copy", out=KrKh[u][:, 128:192], in_=Kh[pb][:, hs])
                yield
                cp = 0
                for lev in range(5):
                    pk = PP[u][cp]
                    xx = XX[u][cp]
                    xn = XX[u][1 - cp]
                    P.mm(BX[:, 0:192], pk[:, 128:256], xx.v())
                    if lev < 4:
                        pn = PP[u][1 - cp]
                        P.mm(BY[:, 0:128], pk[:, 128:256], pk[:, 0:128])
                        P.mm(BY[:, 128:256], pk[:, 0:128], pk[:, 128:256])
                    P.dve("tensor_tensor", out=xn.v(), in0=BX[:, 0:192], in1=xx.v(), op=ALU.add)
                    if lev < 4:
                        P.act("copy", out=pn.v(), in_=BY[:, 0:256])
                    cp = 1 - cp
                    yield
                P.act("copy", out=XF32[u].v(), in_=XX[u][cp].v())
                xf = XF32[u]
                P.mm(BX[0:64, 256:384], At[pb][:, hs], xf[:, 0:128])
                P.mm(BY[:, 0:192], AkaT[u].v(), xf.v())
                P.dve("tensor_tensor", out=X2m[u].v(),
                       in0=View(xf, xf.tensor[:, 128:192].unsqueeze(1).to_broadcast([128, 4, 64])),
                       in1=RMexp.v(), op=ALU.mult)
                P.dve("tensor_tensor", out=G1[u].v(), in0=BX[0:64, 256:384], in1=TT[u][:, 128:256], op=ALU.add)
                P.dve("tensor_tensor", out=G2H2[u].v(), in0=BY[:, 0:192], in1=KrKh[u].v(), op=ALU.add)
                P.dve("tensor_tensor", out=G1pad[pb][h].v(),
                       in0=View(G1[u], G1[u].tensor[:, :].unsqueeze(1).to_broadcast([64, 4, 128])),
                       in1=CM.v(), op=ALU.mult)
                yield
                P.mm(BX[0:64, 0:256], At[pb][:, hs], X2m[u].v().rearrange("p c j -> p (c j)"))
                P.mm(pY[:, hs], G2H2[u][:, 0:128], V_t[pb][:, hs], start=(h == 0), stop=False)
                P.mm(BY[0:64, 256:512], G2H2[u][:, 128:192], Vm[pb][:, h, :, :].rearrange("p c i -> p (c i)"))
                for c in range(4):
                    P.dve("scalar_tensor_tensor", out=H1[pb][h][:, c, :], in0=identf[0:64, 0:64],
                          scalar=WCT[u][:, c:c + 1], in1=BX[0:64, c * 64:(c + 1) * 64],
                          op0=ALU.mult, op1=ALU.add)
                P.act("copy", out=SvT[pb][:, :, h, :], in_=BY[0:64, 256:512].rearrange("p (c i) -> p c i", c=4))

            banks = [(B0, B1), (B2, B3)]
            active = [head_gen(0, *banks[0]), head_gen(1, *banks[1])]
            next_h = 2
            while any(g is not None for g in active):
                for slot in range(2):
                    g = active[slot]
                    if g is None:
                        continue
                    try:
                        next(g)
                    except StopIteration:
                        if next_h < NH:
                            assert next_h % 2 == slot
                            active[slot] = head_gen(next_h, *banks[slot])
                            next_h += 1
                            next(active[slot])
                        else:
                            active[slot] = None

            for c in range(4):
                stc = ST[st_cur]
                stn = ST[1 - st_cur]
                for h in range(NH):
                    hs = slice(h * 64, (h + 1) * 64)
                    P.mm(pY[:, hs], G1pad[pb][h][:, c, :], stc[:, h, :], start=False, stop=(c == 3))
                    P.mm(psS[0:64, hs], H1[pb][h][:, c, :], stc[:, h, :])
                P.dve("tensor_tensor", out=stn.v().rearrange("p h i -> p (h i)"), in0=psS[0:64, :],
                      in1=SvT[pb][:, c, :, :].rearrange("p h i -> p (h i)"), op=ALU.add)
                st_cur = 1 - st_cur

            P.act("copy", out=y_t[0].v(), in_=pY.v())
            if sb == 0:
                dump(y_t[0].v(), 16); dump(bon[0].v(), 17)
                dump(ST[st_cur].v().rearrange("p h i -> p (h i)"), 18) if False else None
            P.dve("tensor_reduce", out=s1.v(), in_=v3(y_t[0].v()), axis=AX.X, op=ALU.add)
            P.dve("tensor_tensor", out=ysq[0].v(), in0=y_t[0].v(), in1=y_t[0].v(), op=ALU.mult)
            P.dve("tensor_reduce", out=s2.v(), in_=v3(ysq[0].v()), axis=AX.X, op=ALU.add)
            P.dve("tensor_scalar", out=s1.v(), in0=s1.v(), scalar1=1.0 / 64, scalar2=None, op0=ALU.mult)
            P.dve("tensor_scalar", out=s2.v(), in0=s2.v(), scalar1=1.0 / 64, scalar2=GN_EPS, op0=ALU.mult, op1=ALU.add)
            P.dve("tensor_tensor", out=rk8.v(), in0=s1.v(), in1=s1.v(), op=ALU.mult)
            P.dve("tensor_tensor", out=s2.v(), in0=s2.v(), in1=rk8.v(), op=ALU.subtract)
            P.act("activation", out=s2.v(), in_=s2.v(), func=AF.Sqrt)
            P.dve("reciprocal", out=s2.v(), in_=s2.v())
            P.dve("tensor_tensor", out=v3(y_t[0].v()), in0=v3(y_t[0].v()), in1=bc8(s1), op=ALU.subtract)
            P.dve("tensor_tensor", out=v3(y_t[0].v()), in0=v3(y_t[0].v()), in1=bc8(s2), op=ALU.mult)
            P.dve("tensor_tensor", out=y_t[0].v(), in0=y_t[0].v(), in1=GNG.v(), op=ALU.mult)
            P.dve("tensor_tensor", out=y_t[0].v(), in0=y_t[0].v(), in1=GNB.v(), op=ALU.add)
            P.dve("tensor_tensor", out=y_t[0].v(), in0=y_t[0].v(), in1=bon[pb].v(), op=ALU.add)
            P.dve("tensor_tensor", out=o_t[pb].v(), in0=y_t[0].v(), in1=g_t[pb].v(), op=ALU.mult)
            if "X" in grp:
                P.dma(grp["X"][t0:t0 + 128, :], o_t[pb].v(), lane="sp", final=True)
            else:
                pto = psG[gi % 2]
                gi += 1
                for k4 in range(4):
                    P.pe("transpose", out=pto[:, k4 * 128:(k4 + 1) * 128], in_=o_t[pb][:, k4 * 128:(k4 + 1) * 128],
                         identity=identf.v())
                P.act("copy", out=y_t[0].v(), in_=pto.v())
                for k4 in range(4):
                    P.dma(rows_view(grp["XT"], k4 * 128, 128)[:, t0:t0 + 128], y_t[0][:, k4 * 128:(k4 + 1) * 128],
                          lane="sp", final=True)


def build_RW(nc, nsb=NSB):
    P = Prog(nc)
    hT = P.dram("hT", [D, S], F32, "ExternalInput")
    muT = P.dram("muT", [128, 8, 6], F32, "ExternalInput")
    wr = P.dram("wr", [D, 512], F32, "ExternalInput")
    wk = P.dram("wk", [D, 512], F32, "ExternalInput")
    wv = P.dram("wv", [D, 512], F32, "ExternalInput")
    w1 = P.dram("w1", [D, 64], F32, "ExternalInput")
    a1 = P.dram("a1", [D, 64], F32, "ExternalInput")
    g1 = P.dram("g1", [D, 128], F32, "ExternalInput")
    w2 = P.dram("w2", [64, 512], F32, "ExternalInput")
    a2 = P.dram("a2", [64, 512], F32, "ExternalInput")
    g2 = P.dram("g2", [128, 512], F32, "ExternalInput")
    vecs = P.dram("vecs", [7, 512], F32, "ExternalInput")
    X = P.dram("X", [S, 512], F32, "ExternalOutput")
    grp = dict(wr=wr.v(), wk=wk.v(), wv=wv.v(), w2=w2.v(), a2=a2.v(), g2=g2.v(),
               vecs=[vecs[i:i + 1, :] for i in range(7)], X=X.v())
    body_RW(P, hT.v(), muT.v(), w1.v(), a1.v(), g1.v(), [grp], nsb)
    P.emit()
    P.close()
    return nc


_S, _D, _H = 4096, 1024, 2048
_PAIRS = [[0, 1], [2, 3], [4, 5], [6, 7]]

_IN2 = dict(
    x_half=[_H, _D], x_T=[_D, _S], sel=[128, 2],
    ln1_g=[4, _D], ln1_b=[4, _D], ln2_g=[4, _D], ln2_b=[4, _D],
    fox_wq=[_D, 512], fox_wk=[_D, 512], fox_wv=[_D, 512], fox_wog=[_D, 512], fox_wf=[_D, 8], fox_bf=[1, 8],
    fox_w_out=[_D, _D],
    gm_w_in=[_D, 2048], gm_b_in=[1, 2048], gm_ln_g=[1, _D], gm_ln_b=[1, _D],
    gm_wsT=[128, 8, 128], gm_bsT=[128, 8], gm_w_out=[_D, _D],
    sb_wq=[_D, 512], sb_wk=[_D, 512], sb_wv=[_D, 512], sb_w_out=[_D, _D],
    rw_muT=[128, 8, 6], rw_wr=[_D, 512], rw_wk=[_D, 512], rw_wv=[_D, 512], rw_w1=[_D, 64], rw_a1=[_D, 64],
    rw_g1=[_D, 128], rw_w2=[64, 512], rw_a2=[64, 512], rw_g2=[128, 512], rw_vecs=[7, 512], rw_w_out=[_D, _D],
    router_w=[_D, 16], router_b=[1, 16],
    moe_w_gate=[4, 16, _D, 512], moe_w_up=[4, 16, _D, 512], moe_w_down=[4, 16, 512, _D],
)


def build_fused2(nc, layers=(0, 1, 2, 3)):
    P = Prog(nc)
    I = {k: P.dram(k, shp, F32, "ExternalInput").v() for k, shp in _IN2.items()}
    out = P.dram("out", [_H, _D], F32, "ExternalOutput").v()
    xgi = [P.dram("xgi%d" % j, [128, _S], F32, "Internal").v() for j in range(4)]
    xgo = [P.dram("xgo%d" % j, [256, _S], F32, "Internal").v() for j in range(4)]
    xg_in = RowBlocks(xgi, 128)
    xblocks = [xgo[k % 4][(k // 4) * 128:(k // 4 + 1) * 128, :] for k in range(8)]
    h_d = P.dram("h_d", [_H, _D], F32, "Internal").v()
    hTo = [P.dram("hTo%d" % j, [256, _H], F32, "Internal").v() for j in range(4)]
    hTg = [P.dram("hTg%d" % j, [512, _H], F32, "Internal").v() for j in range(4)]
    hT_own = RowBlocks(hTo, 256)
    xT_own = P.dram("xT_own", [_D, _H], F32, "Internal").v()
    hT_full = P.dram("hT_full", [_D, _S], F32, "Internal").v()

    def gather_x():
        P.barrier()
        for j in range(4):
            P.all_gather(xgo[j], xgi[j], _PAIRS)

    def gather_h():
        P.barrier()
        for j in range(4):
            P.all_gather(hTg[j], hTo[j], _PAIRS)
        for j in range(4):
            for r in range(2):
                P.dma(hT_full[j * 256:(j + 1) * 256, r * _H:(r + 1) * _H], hTg[j][r * 256:(r + 1) * 256, :], lane="sp")

    def t_stage(L, wout, first, last, blend):
        io = dict(hin=(I["x_half"] if first else h_d), wout=wout,
                  ln=[I["ln1_g"][L:L + 1, :], I["ln1_b"][L:L + 1, :], I["ln2_g"][L:L + 1, :], I["ln2_b"][L:L + 1, :]],
                  rw=I["router_w"], rb=I["router_b"], wg=I["moe_w_gate"][L], wu=I["moe_w_up"][L],
                  wd=I["moe_w_down"][L], hout=(out if last else h_d))
        if blend:
            io["xT_blend"] = (xblocks, I["sel"])
        else:
            io["xT"] = xT_own
        if not last:
            io["houtT"] = hT_own
        P.begin_stage()
        body_T(P, io, _H)
        if (not last) and L != 0:
            gather_h()
        P.end_stage(last=last)

    nl = len(layers)
    for li, L in enumerate(layers):
        first, last = (li == 0), (li == nl - 1)
        hT = I["x_T"] if first else hT_full
        P.begin_stage()
        if L == 0:
            grp = dict(wq=I["fox_wq"], wk=I["fox_wk"], wv=I["fox_wv"], wog=I["fox_wog"], wf=I["fox_wf"],
                       bf=I["fox_bf"], XT=xg_in)
            body_FOX(P, hT, [grp])
            gather_x()
            wout = I["fox_w_out"]
        elif L == 1:
            body_GM(P, dict(hT=(hT_own if not first else None), w_in=I["gm_w_in"], b_in=I["gm_b_in"],
                            ln=[I["gm_ln_g"], I["gm_ln_b"]], wsT=I["gm_wsT"], bsT=I["gm_bsT"], XT=xT_own), _H)
            wout = I["gm_w_out"]
        elif L == 2:
            body_SB(P, hT, [dict(wq=I["sb_wq"], wk=I["sb_wk"], wv=I["sb_wv"], XT=xg_in)])
            gather_x()
            wout = I["sb_w_out"]
        else:
            grp = dict(wr=I["rw_wr"], wk=I["rw_wk"], wv=I["rw_wv"], w2=I["rw_w2"], a2=I["rw_a2"], g2=I["rw_g2"],
                       vecs=[I["rw_vecs"][i:i + 1, :] for i in range(7)], XT=xg_in)
            body_RW(P, hT, I["rw_muT"], I["rw_w1"], I["rw_a1"], I["rw_g1"], [grp])
            gather_x()
            wout = I["rw_w_out"]
        P.end_stage()
        t_stage(L, wout, first, last, blend=(L != 1))
    P.close()
    return nc


def fused2_inputs(inp, c):
    b, r = c // 2, c % 2
    f = lambda a: np.ascontiguousarray(a, dtype=np.float32)
    cs = slice(r * 512, (r + 1) * 512)
    m = dict(x_half=f(inp["x"][b][r * _H:(r + 1) * _H]), x_T=f(inp["x"][b].T))
    sel = np.zeros((128, 2), np.float32)
    sel[:, r] = 1.0
    m["sel"] = sel
    for k in ("ln1_g", "ln1_b", "ln2_g", "ln2_b", "router_w", "moe_w_gate", "moe_w_up", "moe_w_down"):
        m[k] = f(inp[k])
    m["router_b"] = f(inp["router_b"]).reshape(1, 16)
    W = inp["fox_w_in"][0]
    m["fox_wq"] = f(W[:, 0:1024][:, cs]); m["fox_wk"] = f(W[:, 1024:2048][:, cs]); m["fox_wv"] = f(W[:, 2048:3072][:, cs])
    m["fox_wog"] = f(W[:, 3088:4112][:, cs]); m["fox_wf"] = f(W[:, 3072 + r * 8:3072 + (r + 1) * 8])
    m["fox_bf"] = f(inp["fox_b_f"][0][r * 8:(r + 1) * 8]).reshape(1, 8)
    for k in ("fox_w_out", "gm_w_in", "gm_w_out", "sb_w_out", "rw_w1", "rw_a1", "rw_g1", "rw_w_out"):
        m[k] = f(inp[k][0])
    m["gm_b_in"] = f(inp["gm_b_in"][0]).reshape(1, -1)
    m["gm_ln_g"] = f(inp["gm_ln_g"][0]).reshape(1, -1); m["gm_ln_b"] = f(inp["gm_ln_b"][0]).reshape(1, -1)
    m["gm_wsT"] = f(inp["gm_w_s"][0].transpose(2, 0, 1)); m["gm_bsT"] = f(inp["gm_b_s"][0].T)
    W = inp["sb_w_in"][0]
    m["sb_wq"] = f(W[:, 0:1024][:, cs]); m["sb_wk"] = f(W[:, 1024:2048][:, cs]); m["sb_wv"] = f(W[:, 2048:3072][:, cs])
    m["rw_muT"] = f(inp["rw_mu"][0].T.reshape(8, 128, 6).transpose(1, 0, 2))
    W = inp["rw_w_rkv"][0]
    m["rw_wr"] = f(W[0][:, cs]); m["rw_wk"] = f(W[1][:, cs]); m["rw_wv"] = f(W[2][:, cs])
    m["rw_w2"] = f(inp["rw_w2"][0][:, cs]); m["rw_a2"] = f(inp["rw_a2"][0][:, cs]); m["rw_g2"] = f(inp["rw_g2"][0][:, cs])
    m["rw_vecs"] = f(np.stack([inp["rw_w0"][0][cs], inp["rw_a0"][0][cs], inp["rw_k_k"][0][cs], inp["rw_k_a"][0][cs],
                               inp["rw_gn_g"][0][cs], inp["rw_gn_b"][0][cs], inp["rw_r_k"][0].reshape(-1)[cs]]))
    return m


from concourse.bass_utils import run_bass_kernel_spmd


def kernel(**inp):
    inp = {k: np.asarray(v) for k, v in inp.items()}
    nc = bass.Bass("TRN2", target_bir_lowering=False, num_devices=8)
    build_fused2(nc)
    maps = [fused2_inputs(inp, c) for c in range(8)]
    res = run_bass_kernel_spmd(nc, maps, core_ids=list(range(8))).results
    out = np.stack([np.concatenate([res[2 * b]["out"], res[2 * b + 1]["out"]], 0) for b in range(4)], 0)
    return out.astype(np.float32)
```
